# Optimizing a Trainium2 kernel written in Bass

```python
import math
import jax, jax.numpy as jnp
from jax import lax
import numpy as np

D_MODEL = 2048
BATCH = 16
SEQ = 2048
DEPTH = 2

MEM_LEN = 256
D_MIX = D_MODEL
D_ATTN = D_MIX // 2
DQ = 64
DV = 2 * DQ
N_HEADS_A = D_ATTN // DV
D_CONV = D_MIX - D_ATTN
CONV_WIDTH = 31
N_BUCKETS = 32
MAX_DISTANCE = 128
N_HEADS_C = 4
DH_C = 128
D_CROSS = N_HEADS_C * DH_C
N_GROUPS = 8
EXPERTS_PER_GROUP = 8
N_EXPERTS = N_GROUPS * EXPERTS_PER_GROUP
TOP_K = 2
D_EXPERT = D_MODEL // 4
Q_BLOCK = 128
MOE_BLOCK = 256
EPS = 1e-6
NEG_INF = -1e30
D_QK = N_HEADS_A * 2 * DQ
D_IN = 2 * D_QK + N_HEADS_A * DV + 2 * D_CONV

kernel_name = 'hybrid_diffattn_conformer_hmoe'


def rmsnorm(x, g):
    xf = x.astype(jnp.float32)
    y = xf * lax.rsqrt(jnp.mean(xf * xf, axis=-1, keepdims=True) + EPS)
    return (y * g.astype(jnp.float32)).astype(x.dtype)


def layernorm(x, g, b):
    xf = x.astype(jnp.float32)
    mu = jnp.mean(xf, axis=-1, keepdims=True)
    var = jnp.mean(jnp.square(xf - mu), axis=-1, keepdims=True)
    y = (xf - mu) * lax.rsqrt(var + EPS)
    return (y * g.astype(jnp.float32) + b.astype(jnp.float32)).astype(x.dtype)


def t5_causal_bucket(dist):
    max_exact = N_BUCKETS // 2
    d_f = jnp.maximum(dist, 1).astype(jnp.float32)
    large = max_exact + (jnp.log(d_f / max_exact) / math.log(MAX_DISTANCE / max_exact)
                         * (N_BUCKETS - max_exact)).astype(jnp.int32)
    large = jnp.minimum(large, N_BUCKETS - 1)
    return jnp.where(dist < max_exact, dist, large)


def diff_attention(q, k, v, lam, lam_init, g_q, g_k, g_subln, rel_bias_dist):
    B, S = q.shape[:2]
    q = rmsnorm(q, g_q) * (DQ ** -0.5)
    k = rmsnorm(k, g_k)
    outs = []
    for i in range(S // Q_BLOCK):
        q0, qe = i * Q_BLOCK, (i + 1) * Q_BLOCK
        s = jnp.einsum('bqhmd,bkhmd->bhmqk', q[:, q0:qe], k[:, :qe]).astype(jnp.float32)
        dist = jnp.arange(q0, qe)[:, None] - jnp.arange(qe)[None, :]
        bias = rel_bias_dist[:, jnp.maximum(dist, 0)].astype(jnp.float32)
        s = jnp.where(dist >= 0, s + bias[None, :, None], NEG_INF)
        p = jax.nn.softmax(s, axis=-1)
        a = (p[:, :, 0] - lam * p[:, :, 1]).astype(v.dtype)
        outs.append(jnp.einsum('bhqk,bkhd->bqhd', a, v[:, :qe]))
    o = jnp.concatenate(outs, axis=1)
    o = rmsnorm(o, g_subln) * (1.0 - lam_init)
    return o.reshape(B, S, N_HEADS_A * DV)


def conformer_conv(c, conv_w, conv_b, ln_g, ln_b):
    a, gate = jnp.split(c, 2, axis=-1)
    u = a * jax.nn.sigmoid(gate)
    u = lax.conv_general_dilated(u, conv_w[:, None, :], window_strides=(1,),
                                 padding=[(CONV_WIDTH - 1, 0)],
                                 dimension_numbers=('NWC', 'WIO', 'NWC'),
                                 feature_group_count=D_CONV) + conv_b
    u = layernorm(u, ln_g, ln_b)
    return jax.nn.silu(u)


def cross_attention(h, mem_n, wq, wkv, g_qc, g_kc, wo):
    B, S, _ = h.shape
    M = mem_n.shape[1]
    q = rmsnorm((h @ wq).reshape(B, S, N_HEADS_C, DH_C), g_qc) * (DH_C ** -0.5)
    kv = (mem_n @ wkv).reshape(B, M, 2, N_HEADS_C, DH_C)
    k = rmsnorm(kv[:, :, 0], g_kc)
    v = kv[:, :, 1]
    s = jnp.einsum('bqhd,bkhd->bhqk', q, k).astype(jnp.float32)
    p = jax.nn.softmax(s, axis=-1).astype(v.dtype)
    o = jnp.einsum('bhqk,bkhd->bqhd', p, v).reshape(B, S, D_CROSS)
    return o @ wo


def hierarchical_route(h, w_group, b_group, w_router, b_router):
    T = h.shape[0]
    gl = (h @ w_group).astype(jnp.float32) + b_group.astype(jnp.float32)
    gp = jax.nn.softmax(gl, axis=-1)
    g_sel = jnp.argmax(gl, axis=-1)
    g_w = jnp.take_along_axis(gp, g_sel[:, None], axis=-1)
    el = ((h @ w_router).astype(jnp.float32) + b_router.astype(jnp.float32))
    el = el.reshape(T, N_GROUPS, EXPERTS_PER_GROUP)
    el = jnp.take_along_axis(el, g_sel[:, None, None], axis=1)[:, 0]
    top_v, top_i = lax.top_k(el, TOP_K)
    gates = (g_w * jax.nn.softmax(top_v, axis=-1)).astype(h.dtype)
    ids = g_sel[:, None].astype(jnp.int32) * EXPERTS_PER_GROUP + top_i.astype(jnp.int32)
    return ids, gates


def moe_ffn(h, ids, gates, w1, w3, w2):
    T, D = h.shape
    A = T * TOP_K
    flat_e = ids.reshape(-1)
    flat_t = jnp.repeat(jnp.arange(T, dtype=jnp.int32), TOP_K)
    flat_g = gates.reshape(-1)
    order = jnp.argsort(flat_e)
    se, st, sg = flat_e[order], flat_t[order], flat_g[order]
    counts = jnp.bincount(flat_e, length=N_EXPERTS)
    start = jnp.cumsum(counts) - counts
    pcounts = (counts + MOE_BLOCK - 1) // MOE_BLOCK * MOE_BLOCK
    pend = jnp.cumsum(pcounts)
    pstart = pend - pcounts
    dest = pstart[se] + (jnp.arange(A) - start[se])
    n_blocks = -(-(A + N_EXPERTS * (MOE_BLOCK - 1)) // MOE_BLOCK)
    P = n_blocks * MOE_BLOCK
    tok_buf = jnp.full((P,), T, dtype=jnp.int32).at[dest].set(st)
    gate_buf = jnp.zeros((P,), h.dtype).at[dest].set(sg)
    blk_expert = jnp.minimum(
        jnp.searchsorted(pend, jnp.arange(n_blocks) * MOE_BLOCK, side='right'), N_EXPERTS - 1)
    h_pad = jnp.concatenate([h, jnp.zeros((1, D), h.dtype)], axis=0)

    def body(acc, xs):
        tok, g, e = xs
        xb = h_pad[tok]
        hid = jax.nn.silu(xb @ w1[e]) * (xb @ w3[e])
        yb = (hid @ w2[e]) * g[:, None]
        return acc.at[tok].add(yb), None

    acc0 = jnp.zeros((T + 1, D), h.dtype)
    acc, _ = lax.scan(body, acc0, (tok_buf.reshape(n_blocks, MOE_BLOCK),
                                   gate_buf.reshape(n_blocks, MOE_BLOCK), blk_expert))
    return acc[:T]


def setup_inputs(seed: int = 0) -> dict:
    key = jax.random.key(seed)
    ks = list(jax.random.split(key, 40))

    def nrm(i, shape, scale):
        return jax.random.normal(ks[i], shape, jnp.float32) * scale

    def gain(i, shape):
        return 1.0 + nrm(i, shape, 0.05)

    L = DEPTH
    return {
        'x': nrm(0, (BATCH, SEQ, D_MODEL), 1.0),
        'mem': nrm(1, (BATCH, MEM_LEN, D_MODEL), 1.0),
        'rel_bias_table': nrm(2, (N_BUCKETS, N_HEADS_A), 0.2),
        'g_mix': gain(3, (L, D_MODEL)),
        'w_in': nrm(4, (L, D_MODEL, D_IN), D_MODEL ** -0.5),
        'g_q': gain(5, (L, DQ)),
        'g_k': gain(6, (L, DQ)),
        'diff_lambda': nrm(7, (L, 4, DQ), 0.1),
        'g_subln': gain(8, (L, DV)),
        'conv_w': nrm(9, (L, CONV_WIDTH, D_CONV), CONV_WIDTH ** -0.5),
        'conv_b': nrm(10, (L, D_CONV), 0.01),
        'conv_ln_g': gain(11, (L, D_CONV)),
        'conv_ln_b': nrm(12, (L, D_CONV), 0.01),
        'w_out': nrm(13, (L, D_MIX, D_MODEL), D_MIX ** -0.5),
        'g_cross': gain(14, (L, D_MODEL)),
        'g_mem': gain(15, (L, D_MODEL)),
        'wq_c': nrm(16, (L, D_MODEL, D_CROSS), D_MODEL ** -0.5),
        'wkv_c': nrm(17, (L, D_MODEL, 2 * D_CROSS), D_MODEL ** -0.5),
        'g_qc': gain(18, (L, DH_C)),
        'g_kc': gain(19, (L, DH_C)),
        'wo_c': nrm(20, (L, D_CROSS, D_MODEL), D_CROSS ** -0.5),
        'g_ffn': gain(21, (L, D_MODEL)),
        'w_group': nrm(22, (L, D_MODEL, N_GROUPS), D_MODEL ** -0.5),
        'b_group': nrm(23, (L, N_GROUPS), 0.01),
        'w_router': nrm(24, (L, D_MODEL, N_EXPERTS), D_MODEL ** -0.5),
        'b_router': nrm(25, (L, N_EXPERTS), 0.01),
        'w1': nrm(26, (L, N_EXPERTS, D_MODEL, D_EXPERT), D_MODEL ** -0.5),
        'w3': nrm(27, (L, N_EXPERTS, D_MODEL, D_EXPERT), D_MODEL ** -0.5),
        'w2': nrm(28, (L, N_EXPERTS, D_EXPERT, D_MODEL), D_EXPERT ** -0.5),
    }


def reference(x, mem, rel_bias_table, g_mix, w_in, g_q, g_k, diff_lambda, g_subln,
              conv_w, conv_b, conv_ln_g, conv_ln_b, w_out, g_cross, g_mem, wq_c, wkv_c,
              g_qc, g_kc, wo_c, g_ffn, w_group, b_group, w_router, b_router, w1, w3, w2):
    B, S, D = x.shape
    rel_bias_dist = rel_bias_table[t5_causal_bucket(jnp.arange(S))].T
    for l in range(DEPTH):
        lam_init = 0.8 - 0.6 * math.exp(-0.3 * l)
        dl = diff_lambda[l].astype(jnp.float32)
        lam = jnp.exp(jnp.sum(dl[0] * dl[1])) - jnp.exp(jnp.sum(dl[2] * dl[3])) + lam_init

        h = rmsnorm(x, g_mix[l])
        proj = h @ w_in[l]
        q = proj[..., :D_QK].reshape(B, S, N_HEADS_A, 2, DQ)
        k = proj[..., D_QK:2 * D_QK].reshape(B, S, N_HEADS_A, 2, DQ)
        v = proj[..., 2 * D_QK:2 * D_QK + N_HEADS_A * DV].reshape(B, S, N_HEADS_A, DV)
        c = proj[..., 2 * D_QK + N_HEADS_A * DV:]
        attn = diff_attention(q, k, v, lam, lam_init, g_q[l], g_k[l], g_subln[l], rel_bias_dist)
        conv = conformer_conv(c, conv_w[l], conv_b[l], conv_ln_g[l], conv_ln_b[l])
        x = x + jnp.concatenate([attn, conv], axis=-1) @ w_out[l]

        h = rmsnorm(x, g_cross[l])
        x = x + cross_attention(h, rmsnorm(mem, g_mem[l]), wq_c[l], wkv_c[l], g_qc[l], g_kc[l], wo_c[l])

        h = rmsnorm(x, g_ffn[l]).reshape(B * S, D)
        ids, gates = hierarchical_route(h, w_group[l], b_group[l], w_router[l], b_router[l])
        x = x + moe_ffn(h, ids, gates, w1[l], w3[l], w2[l]).reshape(B, S, D)
    return x
```

```python
import math
from contextlib import ExitStack

import numpy as np
import concourse.bass as bass
import concourse.mybir as mybir
from concourse.bass_utils import run_bass_kernel_spmd

AF = mybir.ActivationFunctionType
ALU = mybir.AluOpType
AX = mybir.AxisListType
F32 = mybir.dt.float32
BF16 = mybir.dt.bfloat16
I32 = mybir.dt.int32

D = 2048
KC = D // 128
DQ = 64
NH = 8
D_IN = 5120
CW = 31
MEM = 256
NHC = 4
NG = 8
NE = 64
BLK = 256
EPS = 1e-6
NCORES = 8
MASKV = -30000.0
STAGES = "ABCDEF"

_off = {}
_n = 0
for _name, _w in [("gmixT", 16), ("gcrossT", 16), ("gmemT", 16), ("gq2", 1), ("gk2", 1), ("gsub", 1),
                  ("gqc", 1), ("gkc", 1), ("convw", 8 * CW), ("convb", 8), ("lng", 8), ("lnb", 8),
                  ("dl", 4 * DQ), ("brt", 72), ("relc", 8)]:
    _off[_name] = (_n, _w)
    _n += _w
NPAR = _n


class Buf:
    __slots__ = ("t", "w", "r")

    def __init__(self, t):
        self.t = t
        self.w = []
        self.r = []


class KB:
    NRING = 64

    def __init__(self, nc, es):
        self.nc = nc
        self.es = es
        self.eng = {"pe": nc.tensor, "dve": nc.vector, "act": nc.scalar, "pool": nc.gpsimd, "sp": nc.sync}
        self.chan = {}
        self.waited = {e: {} for e in self.eng}
        self.ring = [es.enter_context(nc.semaphore(f"dq{i}")) for i in range(self.NRING)]
        self.ring_cnt = [0] * self.NRING
        self.ring_i = 0
        self.nsem = 0
        self.ninstr = 0

    def _chan(self, e):
        c = self.chan.get(e)
        if c is None or c[2] >= 30000:
            self.nsem += 1
            c = [f"c{e}{self.nsem}", self.es.enter_context(self.nc.semaphore(f"c_{e}_{self.nsem}")), 0]
            self.chan[e] = c
        return c

    def _wait(self, e, toks):
        wd = self.waited[e]
        for (key, sem, v, prod) in toks:
            if prod == e and e == "pe":
                continue
            if wd.get(key, 0) >= v:
                continue
            wd[key] = v
            self.eng[e].wait_ge(sem, v)

    @staticmethod
    def _prune(lst):
        last = {}
        out = []
        for t in lst:
            if t[3] == "dma":
                out.append(t)
            else:
                last[t[3]] = t
        return out + list(last.values())

    def _deps(self, e, reads, writes):
        toks = []
        for b in reads:
            toks += b.w
        for b in writes:
            toks += b.w
            toks += b.r
        self._wait(e, toks)

    def _commit(self, tok, reads, writes):
        for b in reads:
            b.r.append(tok)
            if len(b.r) > 6:
                b.r = self._prune(b.r)
        for b in writes:
            b.w = [tok]
            b.r = []

    def op(self, e, fn, reads=(), writes=()):
        self._deps(e, reads, writes)
        ins = fn(self.eng[e])
        c = self._chan(e)
        c[2] += 1
        ins.then_inc(c[1], 1)
        tok = (c[0], c[1], c[2], e)
        self._commit(tok, reads, writes)
        self.ninstr += 1
        return tok

    def dma(self, q, fn, reads=(), writes=(), accumulate_writes=False):
        if accumulate_writes:
            toks = []
            for b in reads:
                toks += b.w
            for b in writes:
                if b.r:
                    toks += b.w
                    toks += b.r
            self._wait(q, toks)
        else:
            self._deps(q, reads, writes)
        i = self.ring_i
        self.ring_i = (i + 1) % self.NRING
        if self.ring_cnt[i] > 0:
            self._wait(q, [(f"dq{i}", self.ring[i], self.ring_cnt[i], "dma")])
        ins = fn(self.eng[q])
        self.ring_cnt[i] += 16
        ins.then_inc(self.ring[i], 16)
        tok = (f"dq{i}", self.ring[i], self.ring_cnt[i], "dma")
        for b in reads:
            b.r.append(tok)
        for b in writes:
            if accumulate_writes and not b.r:
                b.w.append(tok)
            else:
                b.w = [tok]
                b.r = []
        self.ninstr += 1
        return tok

    def barrier(self):
        toks = [(c[0], c[1], c[2], e) for e, c in self.chan.items() if c[2] > 0]
        toks += [(f"dq{i}", self.ring[i], self.ring_cnt[i], "dma") for i in range(self.NRING) if self.ring_cnt[i] > 0]
        for e in self.eng:
            self._wait(e, toks)

    def drain(self, e, bufs):
        toks = []
        for b in bufs:
            toks += b.w
            toks += b.r
        self._wait(e, toks)


def build_program(L, NSEQ, S, DE, debug=()):
    T = NSEQ * S
    NT = T // 128
    NB = -(-(2 * T + NE * (BLK - 1)) // BLK)
    PR = NB * BLK
    FC = DE // 128
    nc = bass.Bass("TRN2", target_bir_lowering=False)
    es = ExitStack()

    def dram(name, shape, dt, kind="Internal"):
        return nc.dram_tensor(name, list(shape), dt, kind=kind).ap()

    x_in = dram("x", [T, D], F32, "ExternalInput")
    mem_in = dram("mem", [NSEQ * MEM, D], F32, "ExternalInput")
    pars_in = dram("pars", [L, 128, NPAR], F32, "ExternalInput")
    gffn_in = dram("gffnb", [L, 128, D], F32, "ExternalInput")
    biasT_in = dram("biasT", [128, NH * 2, 128], F32, "ExternalInput")
    consts_in = dram("consts", [128, 5, 128], F32, "ExternalInput")
    thr_in = dram("thr", [128, NB], F32, "ExternalInput")
    w_in = dram("w_in", [L, D, D_IN], F32, "ExternalInput")
    w_out = dram("w_out", [L, D, D], F32, "ExternalInput")
    wq_c = dram("wq_c", [L, D, 512], F32, "ExternalInput")
    wkv_c = dram("wkv_c", [L, D, 1024], F32, "ExternalInput")
    wo_c = dram("wo_c", [L, 512, D], F32, "ExternalInput")
    w_rt = dram("w_rt", [L, D, 72], F32, "ExternalInput")
    if "F" in STAGES:
        NCH = KC * DE // 2048
        GK = 2048 // DE
        w1r = [[dram(f"w1r_{l_}_{c}", [NE * 128, 2048], F32, "ExternalInput") for c in range(NCH)] for l_ in range(L)]
        w3r = [[dram(f"w3r_{l_}_{c}", [NE * 128, 2048], F32, "ExternalInput") for c in range(NCH)] for l_ in range(L)]
        w2r = [[dram(f"w2r_{l_}_{c}", [NE * 128, 2048], F32, "ExternalInput") for c in range(FC)] for l_ in range(L)]
    out = dram("out", [T, D], F32, "ExternalOutput")

    xres = dram("xres", [T, D], F32)
    qTs = dram("qTs", [NSEQ, NH, 128, S], BF16)
    kTs = dram("kTs", [NSEQ, NH, 128, S], BF16)
    Vs = dram("Vs", [NSEQ, S, NH * 128], BF16)
    Us = dram("Us", [NSEQ, 8, 128, S], F32)
    mixT = dram("mixT", [16, 128, T], BF16)
    Hrows = dram("Hrows", [T, D], BF16)
    Xs = dram("Xs", [PR, D], BF16)
    Ys = dram("Ys", [PR, D], F32)
    dbg = {}
    for name, shape, dt in debug:
        dbg[name] = dram("dbg_" + name, shape, dt, "ExternalOutput")

    k = KB(nc, es)
    regions = {}
    uid = [0]

    def R(*key):
        b = regions.get(key)
        if b is None:
            b = Buf(None)
            regions[key] = b
        return b

    class Stage:
        def __init__(self):
            self.es = ExitStack()

        def sb(self, name, shape, dt):
            uid[0] += 1
            return Buf(self.es.enter_context(nc.sbuf_tensor(f"{name}_{uid[0]}", list(shape), dt)))

        def close(self):
            k.barrier()
            self.es.close()

    G = Stage()

    def ps(name, shape, dt=F32):
        return Buf(es.enter_context(nc.psum_tensor(name, list(shape), dt)))

    cst = G.sb("cst", [128, 5, 128], F32)
    ident_bf = G.sb("ident_bf", [128, 128], BF16)
    ones_bf = G.sb("ones_bf", [128, 128], BF16)
    ltri_bf = G.sb("ltri_bf", [128, 128], BF16)
    biasT = G.sb("biasT_sb", [128, NH * 2, 128], BF16)
    pars = G.sb("pars_sb", [128, NPAR], F32)
    small = G.sb("small", [128, 16], F32)
    col = [G.sb(f"col{i}", [128, 8], F32) for i in range(4)]
    f512 = [G.sb(f"f512_{i}", [128, 512], F32) for i in range(6)]
    b512 = [G.sb(f"b512_{i}", [128, 512], BF16) for i in range(4)]
    ones_f = cst.t[:, 1, :]
    bd64_f = cst.t[:, 2, :]
    iota_p = cst.t[:, 4, 0:1]
    pT = ps("pT", [128, KC, 128], BF16)
    pA = [ps(f"pA{i}", [128, 512]) for i in range(6)]

    k.dma("sp", lambda e: e.dma_start(out=cst.t[:], in_=consts_in[:, :, :]), writes=[cst])
    k.dma("pool", lambda e: e.dma_start(out=biasT.t[:], in_=biasT_in[:, :, :]), writes=[biasT])
    k.op("dve", lambda e: e.tensor_copy(out=ident_bf.t[:], in_=cst.t[:, 0, :]), reads=[cst], writes=[ident_bf])
    k.op("dve", lambda e: e.tensor_copy(out=ones_bf.t[:], in_=cst.t[:, 1, :]), reads=[cst], writes=[ones_bf])
    k.op("dve", lambda e: e.tensor_copy(out=ltri_bf.t[:], in_=cst.t[:, 3, :]), reads=[cst], writes=[ltri_bf])

    def P(name, j=0, w=1):
        o, _ = _off[name]
        return pars.t[:, o + j:o + j + w]

    cnt = {}

    def nxt(lst, key):
        i = cnt.get(key, 0)
        cnt[key] = i + 1
        return lst[i % len(lst)]

    def rms_tile(W, x_rows, xreg, gcolT, dst, dst_c0, a=1.0 / D):
        xt = nxt(W["xt"], "x")
        k.dma("sp", lambda e: e.dma_start(out=xt.t[:], in_=x_rows), reads=[xreg], writes=[xt])
        c0 = nxt(col, "c")
        junk = W["junk"]
        k.op("act", lambda e: e.activation(out=junk.t[:], in_=xt.t[:], func=AF.Square, accum_out=c0.t[:, 0:1]),
             reads=[xt], writes=[junk, c0])
        k.op("act", lambda e: e.activation(out=c0.t[:, 1:2], in_=c0.t[:, 0:1], func=AF.Sqrt, scale=a, bias=EPS),
             reads=[c0], writes=[c0])
        k.op("dve", lambda e: e.reciprocal(out=c0.t[:, 2:3], in_=c0.t[:, 1:2]), reads=[c0], writes=[c0])
        hb = nxt(W["hb"], "hb")
        k.op("act", lambda e: e.activation(out=hb.t[:], in_=xt.t[:], func=AF.Copy, scale=c0.t[:, 2:3]),
             reads=[xt, c0], writes=[hb])
        for kc in range(KC):
            k.op("pe", lambda e, kc=kc: e.transpose(out=pT.t[:, kc, :], in_=hb.t[:, kc * 128:(kc + 1) * 128],
                                                    identity=ident_bf.t[:]),
                 reads=[hb, ident_bf], writes=[pT])
        k.op("dve", lambda e: e.tensor_tensor(out=dst.t[:, :, dst_c0:dst_c0 + 128], in0=pT.t[:],
                                              in1=gcolT.unsqueeze(2).to_broadcast([128, KC, 128]), op=ALU.mult),
             reads=[pT, pars], writes=[dst])
        return xt

    def load_w(W, dram_view, cols, width=512):
        wb = nxt(W["wb"], "w")
        k.dma("pool", lambda e: e.dma_start(out=wb.t[:, :, 0:width], in_=dram_view[:, :, cols:cols + width]),
              writes=[wb])
        return wb

    def fm_norm(psrc, gcol, a, bconst, n, ones_mat):
        sq = nxt(f512, "f")
        k.op("act", lambda e: e.activation(out=sq.t[:, 0:n], in_=psrc.t[:, 0:n], func=AF.Square), reads=[psrc], writes=[sq])
        pss = nxt(pA, "pa")
        k.op("pe", lambda e: e.matmul(pss.t[:, 0:n], lhsT=ones_mat, rhs=sq.t[:, 0:n], start=True, stop=True),
             reads=[sq, cst], writes=[pss])
        rs = nxt(f512, "f")
        k.op("act", lambda e: e.activation(out=rs.t[:, 0:n], in_=pss.t[:, 0:n], func=AF.Sqrt, scale=a, bias=bconst),
             reads=[pss], writes=[rs])
        k.op("dve", lambda e: e.reciprocal(out=rs.t[:, 0:n], in_=rs.t[:, 0:n]), reads=[rs], writes=[rs])
        ob = nxt(b512, "b")
        k.op("dve", lambda e: e.scalar_tensor_tensor(out=ob.t[:, 0:n], in0=psrc.t[:, 0:n], scalar=gcol, in1=rs.t[:, 0:n],
                                                     op0=ALU.mult, op1=ALU.mult),
             reads=[psrc, rs, pars, small], writes=[ob])
        return ob

    def rmsW(st):
        return {"xt": [st.sb("xt", [128, D], F32) for _ in range(2)],
                "hb": [st.sb("hb", [128, D], BF16) for _ in range(2)],
                "junk": st.sb("junk", [128, D], BF16)}

    x_src = x_in
    x_key = "xin"
    for l in range(L):
        last = (l == L - 1)
        lam_init = 0.8 - 0.6 * math.exp(-0.3 * l)
        LS = Stage()
        k.dma("sp", lambda e: e.dma_start(out=pars.t[:], in_=pars_in[l, :, :]), writes=[pars])
        o_dl = _off["dl"][0]
        tmpc = f512[0]
        for i in range(2):
            k.op("dve", lambda e, i=i: e.tensor_tensor(out=tmpc.t[:, 0:DQ], in0=pars.t[:, o_dl + 2 * i * DQ:o_dl + (2 * i + 1) * DQ],
                                                       in1=pars.t[:, o_dl + (2 * i + 1) * DQ:o_dl + (2 * i + 2) * DQ], op=ALU.mult),
                 reads=[pars], writes=[tmpc])
            k.op("dve", lambda e, i=i: e.tensor_reduce(out=small.t[:, 2 + i:3 + i], in_=tmpc.t[:, 0:DQ], axis=AX.X, op=ALU.add),
                 reads=[tmpc], writes=[small])
        k.op("act", lambda e: e.activation(out=small.t[:, 4:6], in_=small.t[:, 2:4], func=AF.Exp), reads=[small], writes=[small])
        k.op("dve", lambda e: e.scalar_tensor_tensor(out=small.t[:, 0:1], in0=small.t[:, 5:6], scalar=-lam_init, in1=small.t[:, 4:5],
                                                     op0=ALU.add, op1=ALU.subtract), reads=[small], writes=[small])
        k.op("dve", lambda e: e.tensor_scalar(out=small.t[:, 1:2], in0=P("gsub"), scalar1=1.0 - lam_init, scalar2=None, op0=ALU.mult),
             reads=[pars], writes=[small])
        neglam = small.t[:, 0:1]
        gsubs = small.t[:, 1:2]

        if "A" in STAGES:
            st_ = Stage()
            W = rmsW(st_)
            W["wb"] = [st_.sb("wb", [128, KC, 512], BF16) for _ in range(3)]
            hT = st_.sb("hT", [128, KC, S], BF16)
            w_in_v = w_in[l].rearrange("(kc p) n -> p kc n", p=128)
            for s in range(NSEQ):
                for tt in range(S // 128):
                    r0 = s * S + tt * 128
                    rms_tile(W, x_src[r0:r0 + 128, :], R(x_key, r0 // 128), P("gmixT", 0, 16), hT, tt * 128)
                for nb in range(4):
                    wb = load_w(W, w_in_v, nb * 512)
                    isq = nb < 2
                    dstT = qTs if isq else kTs
                    gcol = P("gq2") if isq else P("gk2")
                    a, bc = (1.0, DQ * EPS) if isq else (1.0 / DQ, EPS)
                    for j in range(4):
                        head = (nb % 2) * 4 + j
                        for tb in range(S // 512):
                            pp = nxt(pA, "pa")
                            for kc in range(KC):
                                k.op("pe", lambda e, kc=kc: e.matmul(pp.t[:], lhsT=wb.t[:, kc, j * 128:(j + 1) * 128],
                                                                     rhs=hT.t[:, kc, tb * 512:(tb + 1) * 512],
                                                                     start=(kc == 0), stop=(kc == KC - 1)),
                                     reads=[wb, hT], writes=[pp])
                            ob = fm_norm(pp, gcol, a, bc, 512, bd64_f)
                            k.dma("sp", lambda e: e.dma_start(out=dstT[s, head, :, tb * 512:(tb + 1) * 512], in_=ob.t[:]),
                                  reads=[ob], writes=[R("qk", isq, s, head)], accumulate_writes=True)
                for nb in range(4, 6):
                    wb = load_w(W, w_in_v, nb * 512)
                    for tt in range(S // 128):
                        pp = nxt(pA, "pa")
                        for kc in range(KC):
                            k.op("pe", lambda e, kc=kc: e.matmul(pp.t[:], lhsT=hT.t[:, kc, tt * 128:(tt + 1) * 128], rhs=wb.t[:, kc, :],
                                                                 start=(kc == 0), stop=(kc == KC - 1)),
                                 reads=[wb, hT], writes=[pp])
                        ob = nxt(b512, "b")
                        k.op("act", lambda e: e.activation(out=ob.t[:], in_=pp.t[:], func=AF.Copy), reads=[pp], writes=[ob])
                        k.dma("sp", lambda e: e.dma_start(out=Vs[s, tt * 128:(tt + 1) * 128, (nb - 4) * 512:(nb - 3) * 512], in_=ob.t[:]),
                              reads=[ob], writes=[R("v", s)], accumulate_writes=True)
                for i in range(2):
                    wa = load_w(W, w_in_v, (6 + i) * 512)
                    wg = load_w(W, w_in_v, (8 + i) * 512)
                    for j in range(4):
                        ch = i * 4 + j
                        for tb in range(S // 512):
                            pa_ = nxt(pA, "pa")
                            pg_ = nxt(pA, "pa")
                            for kc in range(KC):
                                k.op("pe", lambda e, kc=kc: e.matmul(pa_.t[:], lhsT=wa.t[:, kc, j * 128:(j + 1) * 128],
                                                                     rhs=hT.t[:, kc, tb * 512:(tb + 1) * 512],
                                                                     start=(kc == 0), stop=(kc == KC - 1)),
                                     reads=[wa, hT], writes=[pa_])
                            for kc in range(KC):
                                k.op("pe", lambda e, kc=kc: e.matmul(pg_.t[:], lhsT=wg.t[:, kc, j * 128:(j + 1) * 128],
                                                                     rhs=hT.t[:, kc, tb * 512:(tb + 1) * 512],
                                                                     start=(kc == 0), stop=(kc == KC - 1)),
                                     reads=[wg, hT], writes=[pg_])
                            sg = nxt(f512, "f")
                            k.op("act", lambda e: e.activation(out=sg.t[:], in_=pg_.t[:], func=AF.Sigmoid), reads=[pg_], writes=[sg])
                            ub = nxt(f512, "f")
                            k.op("dve", lambda e: e.tensor_tensor(out=ub.t[:], in0=pa_.t[:], in1=sg.t[:], op=ALU.mult),
                                 reads=[pa_, sg], writes=[ub])
                            k.dma("sp", lambda e: e.dma_start(out=Us[s, ch, :, tb * 512:(tb + 1) * 512], in_=ub.t[:]),
                                  reads=[ub], writes=[R("u", s, ch)], accumulate_writes=True)
            st_.close()

        if "qT" in dbg and l == 0:
            k.dma("sp", lambda e: e.dma_start(out=dbg["qT"][:, :, :, :], in_=qTs[:, :, :, :]), writes=[R("dbg", "qT")])
            k.dma("sp", lambda e: e.dma_start(out=dbg["kT"][:, :, :, :], in_=kTs[:, :, :, :]), writes=[R("dbg", "kT")])
            k.dma("sp", lambda e: e.dma_start(out=dbg["V"][:, :, :], in_=Vs[:, :, :]), writes=[R("dbg", "V")])
            k.dma("sp", lambda e: e.dma_start(out=dbg["U"][:, :, :, :], in_=Us[:, :, :, :]), writes=[R("dbg", "U")])
            k.barrier()

        if "B" in STAGES:
            st_ = Stage()
            qh2 = [st_.sb("qh", [128, S], BF16) for i in range(2)]
            kh2 = [st_.sb("kh", [128, S], BF16) for i in range(2)]
            vh2 = [st_.sb("vh", [128, S // 128, 128], BF16) for i in range(2)]
            it = 0
            for s in range(NSEQ):
                for h in range(NH):
                    qh, kh, vh = qh2[it % 2], kh2[it % 2], vh2[it % 2]
                    it += 1
                    k.dma("sp", lambda e: e.dma_start(out=qh.t[:], in_=qTs[s, h, :, :]), reads=[R("qk", True, s, h)], writes=[qh])
                    k.dma("sp", lambda e: e.dma_start(out=kh.t[:], in_=kTs[s, h, :, :]), reads=[R("qk", False, s, h)], writes=[kh])
                    k.dma("sp", lambda e: e.dma_start(out=vh.t[:], in_=Vs[s, :, h * 128:(h + 1) * 128].rearrange("(t p) d -> p t d", p=128)),
                          reads=[R("v", s)], writes=[vh])
                    for qc in range(S // 512):
                        OL = []
                        for m in range(2):
                            pO = pA[2 * m]
                            pL = pA[2 * m + 1]
                            OL.append((pO, pL))
                            nk = (qc + 1) * 4
                            for kj in range(nk):
                                q_lo = max(qc * 512, kj * 128)
                                c0 = q_lo - qc * 512
                                sT = nxt(pA[4:6], "pa_s")
                                blist = [dlt for dlt in (0, 1) if qc * 4 <= kj + dlt < (qc + 1) * 4]
                                k.op("pe", lambda e: e.matmul(sT.t[:, c0:512], lhsT=kh.t[m * 64:(m + 1) * 64, kj * 128:(kj + 1) * 128],
                                                              rhs=qh.t[m * 64:(m + 1) * 64, q_lo:(qc + 1) * 512],
                                                              start=True, stop=(len(blist) == 0)),
                                     reads=[kh, qh], writes=[sT])
                                for bi, dlt in enumerate(blist):
                                    cc = (kj + dlt) * 128 - qc * 512
                                    k.op("pe", lambda e, cc=cc, dlt=dlt, bi=bi: e.matmul(
                                        sT.t[:, cc:cc + 128], lhsT=ident_bf.t[:], rhs=biasT.t[:, h * 2 + dlt, :],
                                        start=False, stop=(bi == len(blist) - 1)),
                                         reads=[ident_bf, biasT], writes=[sT])
                                near_end = min((kj + 2) * 128, (qc + 1) * 512) - qc * 512
                                pt = nxt(b512, "b")
                                if near_end > c0:
                                    k.op("act", lambda e: e.activation(out=pt.t[:, c0:near_end], in_=sT.t[:, c0:near_end], func=AF.Exp),
                                         reads=[sT], writes=[pt])
                                if near_end < 512:
                                    lo = max(near_end, c0)
                                    k.op("act", lambda e: e.activation(out=pt.t[:, lo:512], in_=sT.t[:, lo:512], func=AF.Exp,
                                                                       bias=P("relc", h)),
                                         reads=[sT, pars], writes=[pt])
                                k.op("pe", lambda e: e.matmul(pO.t[:, c0:512], lhsT=vh.t[:, kj, :], rhs=pt.t[:, c0:512],
                                                              start=(kj == 0), stop=(kj == nk - 1)),
                                     reads=[vh, pt], writes=[pO])
                                k.op("pe", lambda e: e.matmul(pL.t[:, c0:512], lhsT=ones_bf.t[:], rhs=pt.t[:, c0:512],
                                                              start=(kj == 0), stop=(kj == nk - 1)),
                                     reads=[ones_bf, pt], writes=[pL])
                        tms = []
                        for m in range(2):
                            pO, pL = OL[m]
                            r_ = nxt(f512, "f")
                            k.op("dve", lambda e: e.reciprocal(out=r_.t[:], in_=pL.t[:]), reads=[pL], writes=[r_])
                            t_ = nxt(f512, "f")
                            k.op("dve", lambda e: e.tensor_tensor(out=t_.t[:], in0=pO.t[:], in1=r_.t[:], op=ALU.mult),
                                 reads=[pO, r_], writes=[t_])
                            tms.append(t_)
                        a_ = nxt(f512, "f")
                        k.op("dve", lambda e: e.scalar_tensor_tensor(out=a_.t[:], in0=tms[1].t[:], scalar=neglam, in1=tms[0].t[:],
                                                                     op0=ALU.mult, op1=ALU.add),
                             reads=[tms[0], tms[1], small], writes=[a_])
                        ob = fm_norm(a_, gsubs, 1.0 / 128, EPS, 512, ones_f)
                        c_lo = s * S + qc * 512
                        k.dma("sp", lambda e: e.dma_start(out=mixT[h, :, c_lo:c_lo + 512], in_=ob.t[:]),
                              reads=[ob], writes=[R("mix", c_lo // 512)], accumulate_writes=True)
            st_.close()

        if "C" in STAGES:
            st_ = Stage()
            Ubuf = st_.sb("Ubuf", [128, 32 + S], F32)
            Yc = [st_.sb("Yc", [128, S], F32) for j in range(8)]
            mean = st_.sb("mean", [128, 512], F32)
            msq = st_.sb("msq", [128, 512], F32)
            var = st_.sb("var", [128, 512], F32)
            k.op("dve", lambda e: e.memset(Ubuf.t[:, 0:32], 0.0), writes=[Ubuf])
            o_cw = _off["convw"][0]
            for s in range(NSEQ):
                for j in range(8):
                    k.dma("sp", lambda e: e.dma_start(out=Ubuf.t[:, 32:32 + S], in_=Us[s, j, :, :]), reads=[R("u", s, j)], writes=[Ubuf])
                    k.op("dve", lambda e: e.tensor_scalar(out=Yc[j].t[:], in0=Ubuf.t[:, 2:2 + S], scalar1=pars.t[:, o_cw + j * CW:o_cw + j * CW + 1],
                                                          scalar2=P("convb", j), op0=ALU.mult, op1=ALU.add),
                         reads=[Ubuf, pars], writes=[Yc[j]])
                    for tap in range(1, CW):
                        k.op("dve", lambda e, tap=tap: e.scalar_tensor_tensor(
                            out=Yc[j].t[:], in0=Ubuf.t[:, 2 + tap:2 + tap + S], scalar=pars.t[:, o_cw + j * CW + tap:o_cw + j * CW + tap + 1],
                            in1=Yc[j].t[:], op0=ALU.mult, op1=ALU.add),
                             reads=[Ubuf, pars], writes=[Yc[j]])
                for tb in range(S // 512):
                    cs = slice(tb * 512, (tb + 1) * 512)
                    pM = nxt(pA, "pa")
                    pQ = nxt(pA, "pa")
                    for j in range(8):
                        k.op("pe", lambda e, j=j: e.matmul(pM.t[:], lhsT=ones_f, rhs=Yc[j].t[:, cs], start=(j == 0), stop=(j == 7)),
                             reads=[cst, Yc[j]], writes=[pM])
                    for j in range(8):
                        sq = nxt(f512, "f")
                        k.op("act", lambda e, j=j: e.activation(out=sq.t[:], in_=Yc[j].t[:, cs], func=AF.Square), reads=[Yc[j]], writes=[sq])
                        k.op("pe", lambda e, j=j: e.matmul(pQ.t[:], lhsT=ones_f, rhs=sq.t[:], start=(j == 0), stop=(j == 7)),
                             reads=[cst, sq], writes=[pQ])
                    k.op("act", lambda e: e.activation(out=mean.t[:], in_=pM.t[:], func=AF.Copy, scale=1.0 / 1024), reads=[pM], writes=[mean])
                    k.op("act", lambda e: e.activation(out=msq.t[:], in_=mean.t[:], func=AF.Square), reads=[mean], writes=[msq])
                    k.op("dve", lambda e: e.scalar_tensor_tensor(out=var.t[:], in0=pQ.t[:], scalar=1.0 / 1024, in1=msq.t[:],
                                                                 op0=ALU.mult, op1=ALU.subtract), reads=[pQ, msq], writes=[var])
                    k.op("act", lambda e: e.activation(out=var.t[:], in_=var.t[:], func=AF.Sqrt, scale=1.0, bias=EPS), reads=[var], writes=[var])
                    k.op("dve", lambda e: e.reciprocal(out=var.t[:], in_=var.t[:]), reads=[var], writes=[var])
                    for j in range(8):
                        t_ = nxt(f512, "f")
                        k.op("dve", lambda e, j=j: e.tensor_tensor(out=t_.t[:], in0=Yc[j].t[:, cs], in1=mean.t[:], op=ALU.subtract),
                             reads=[Yc[j], mean], writes=[t_])
                        k.op("dve", lambda e: e.tensor_tensor(out=t_.t[:], in0=t_.t[:], in1=var.t[:], op=ALU.mult),
                             reads=[var], writes=[t_])
                        ob = nxt(b512, "b")
                        k.op("act", lambda e, j=j: e.activation(out=ob.t[:], in_=t_.t[:], func=AF.Silu, scale=P("lng", j), bias=P("lnb", j)),
                             reads=[t_, pars], writes=[ob])
                        c_lo = s * S + tb * 512
                        k.dma("sp", lambda e, j=j: e.dma_start(out=mixT[8 + j, :, c_lo:c_lo + 512], in_=ob.t[:]),
                              reads=[ob], writes=[R("mix", c_lo // 512)], accumulate_writes=True)
            st_.close()

        if "mixT" in dbg and l == 0:
            k.dma("sp", lambda e: e.dma_start(out=dbg["mixT"][:, :, :], in_=mixT[:, :, :]), writes=[R("dbg", "mixT")])
            k.barrier()

        if "D" in STAGES:
            st_ = Stage()
            wo_v = w_out[l].rearrange("(kc p) n -> p kc n", p=128)
            wfull = st_.sb("wfull", [128, KC, D], BF16)
            mb2 = [st_.sb("mb", [128, KC, 512], BF16) for _ in range(2)]
            xt2 = [st_.sb("xt", [128, D], F32) for _ in range(2)]
            xo2 = [st_.sb("xo", [128, D], F32) for _ in range(2)]
            for nb in range(4):
                k.dma("pool", lambda e, nb=nb: e.dma_start(out=wfull.t[:, :, nb * 512:(nb + 1) * 512], in_=wo_v[:, :, nb * 512:(nb + 1) * 512]),
                      writes=[wfull], accumulate_writes=True)
            mix_v = mixT.rearrange("c p t -> p c t")
            for tb in range(T // 512):
                mb = nxt(mb2, "mb")
                k.dma("sp", lambda e: e.dma_start(out=mb.t[:], in_=mix_v[:, :, tb * 512:(tb + 1) * 512]), reads=[R("mix", tb)], writes=[mb])
                for tt in range(4):
                    r0 = tb * 512 + tt * 128
                    xt = nxt(xt2, "x")
                    k.dma("sp", lambda e: e.dma_start(out=xt.t[:], in_=x_src[r0:r0 + 128, :]), reads=[R(x_key, r0 // 128)], writes=[xt])
                    xo = nxt(xo2, "xo")
                    for nb in range(4):
                        pp = nxt(pA, "pa")
                        for c in range(KC):
                            k.op("pe", lambda e, c=c: e.matmul(pp.t[:], lhsT=mb.t[:, c, tt * 128:(tt + 1) * 128],
                                                               rhs=wfull.t[:, c, nb * 512:(nb + 1) * 512],
                                                               start=(c == 0), stop=(c == KC - 1)),
                                 reads=[mb, wfull], writes=[pp])
                        k.op("dve", lambda e, nb=nb: e.tensor_tensor(out=xo.t[:, nb * 512:(nb + 1) * 512], in0=pp.t[:],
                                                                     in1=xt.t[:, nb * 512:(nb + 1) * 512], op=ALU.add),
                             reads=[pp, xt], writes=[xo])
                    k.dma("sp", lambda e: e.dma_start(out=xres[r0:r0 + 128, :], in_=xo.t[:]), reads=[xo], writes=[R("xres", r0 // 128)])
            st_.close()
            x_src = xres
            x_key = "xres"

        if "x1" in dbg and l == 0:
            k.dma("sp", lambda e: e.dma_start(out=dbg["x1"][:, :], in_=xres[:, :]), writes=[R("dbg", "x1")])
            k.barrier()

        if "E" in STAGES:
            st_ = Stage()
            W = rmsW(st_)
            W["wb"] = [st_.sb("wb", [128, KC, 512], BF16) for _ in range(2)]
            xo2 = [st_.sb("xo", [128, D], F32) for _ in range(2)]
            memT = st_.sb("memT", [128, KC, MEM], BF16)
            kcT = st_.sb("kcT", [128, NHC, MEM], BF16)
            vcs = st_.sb("vcs", [128, 2, 512], BF16)
            ocT = st_.sb("ocT", [128, NHC, 512], BF16)
            hTb = st_.sb("hTb", [128, KC, 512], BF16)
            wq_sb = st_.sb("wq_sb", [128, KC, 512], BF16)
            wo_sb = st_.sb("wo_sb", [128, NHC, D], BF16)
            wq_v = wq_c[l].rearrange("(kc p) n -> p kc n", p=128)
            wkv_v = wkv_c[l].rearrange("(kc p) n -> p kc n", p=128)
            woc_v = wo_c[l].rearrange("(kc p) n -> p kc n", p=128)
            k.dma("pool", lambda e: e.dma_start(out=wq_sb.t[:], in_=wq_v[:, :, :]), writes=[wq_sb])
            k.dma("pool", lambda e: e.dma_start(out=wo_sb.t[:], in_=woc_v[:, :, :]), writes=[wo_sb])
            for s in range(NSEQ):
                for mt in range(2):
                    r0 = s * MEM + mt * 128
                    rms_tile(W, mem_in[r0:r0 + 128, :], R("memin"), P("gmemT", 0, 16), memT, mt * 128)
                wk_ = load_w(W, wkv_v, 0)
                for h in range(NHC):
                    pp = nxt(pA, "pa")
                    for kc in range(KC):
                        k.op("pe", lambda e, kc=kc: e.matmul(pp.t[:, 0:MEM], lhsT=wk_.t[:, kc, h * 128:(h + 1) * 128], rhs=memT.t[:, kc, :],
                                                             start=(kc == 0), stop=(kc == KC - 1)), reads=[wk_, memT], writes=[pp])
                    ob = fm_norm(pp, P("gkc"), 1.0 / 128, EPS, MEM, ones_f)
                    k.op("dve", lambda e: e.tensor_copy(out=kcT.t[:, h, :], in_=ob.t[:, 0:MEM]), reads=[ob], writes=[kcT])
                wv_ = load_w(W, wkv_v, 512)
                for mt in range(2):
                    pp = nxt(pA, "pa")
                    for kc in range(KC):
                        k.op("pe", lambda e, kc=kc: e.matmul(pp.t[:], lhsT=memT.t[:, kc, mt * 128:(mt + 1) * 128], rhs=wv_.t[:, kc, :],
                                                             start=(kc == 0), stop=(kc == KC - 1)), reads=[wv_, memT], writes=[pp])
                    k.op("act", lambda e: e.activation(out=vcs.t[:, mt, :], in_=pp.t[:], func=AF.Copy), reads=[pp], writes=[vcs])
                for tb in range(S // 512):
                    for tt in range(4):
                        r0 = s * S + tb * 512 + tt * 128
                        rms_tile(W, xres[r0:r0 + 128, :], R("xres", r0 // 128), P("gcrossT", 0, 16), hTb, tt * 128)
                    for h in range(NHC):
                        pp = nxt(pA, "pa")
                        for kc in range(KC):
                            k.op("pe", lambda e, kc=kc: e.matmul(pp.t[:], lhsT=wq_sb.t[:, kc, h * 128:(h + 1) * 128], rhs=hTb.t[:, kc, :],
                                                                 start=(kc == 0), stop=(kc == KC - 1)), reads=[wq_sb, hTb], writes=[pp])
                        qn = fm_norm(pp, P("gqc"), 1.0, 128 * EPS, 512, ones_f)
                        pO = nxt(pA, "pa")
                        pL = nxt(pA, "pa")
                        for mt in range(2):
                            sT = nxt(pA, "pa")
                            k.op("pe", lambda e: e.matmul(sT.t[:], lhsT=kcT.t[:, h, mt * 128:(mt + 1) * 128], rhs=qn.t[:], start=True, stop=True),
                                 reads=[kcT, qn], writes=[sT])
                            pt = nxt(b512, "b")
                            k.op("act", lambda e: e.activation(out=pt.t[:], in_=sT.t[:], func=AF.Exp), reads=[sT], writes=[pt])
                            k.op("pe", lambda e: e.matmul(pO.t[:], lhsT=vcs.t[:, mt, h * 128:(h + 1) * 128], rhs=pt.t[:], start=(mt == 0), stop=(mt == 1)),
                                 reads=[vcs, pt], writes=[pO])
                            k.op("pe", lambda e: e.matmul(pL.t[:], lhsT=ones_bf.t[:], rhs=pt.t[:], start=(mt == 0), stop=(mt == 1)),
                                 reads=[ones_bf, pt], writes=[pL])
                        r_ = nxt(f512, "f")
                        k.op("dve", lambda e: e.reciprocal(out=r_.t[:], in_=pL.t[:]), reads=[pL], writes=[r_])
                        k.op("dve", lambda e: e.tensor_tensor(out=ocT.t[:, h, :], in0=pO.t[:], in1=r_.t[:], op=ALU.mult),
                             reads=[pO, r_], writes=[ocT])
                    for tt in range(4):
                        r0 = s * S + tb * 512 + tt * 128
                        xt = nxt(W["xt"], "x")
                        k.dma("sp", lambda e: e.dma_start(out=xt.t[:], in_=xres[r0:r0 + 128, :]), reads=[R("xres", r0 // 128)], writes=[xt])
                        xo = nxt(xo2, "xo")
                        for nb in range(4):
                            pp = nxt(pA, "pa")
                            for h in range(NHC):
                                k.op("pe", lambda e, h=h: e.matmul(pp.t[:], lhsT=ocT.t[:, h, tt * 128:(tt + 1) * 128],
                                                                   rhs=wo_sb.t[:, h, nb * 512:(nb + 1) * 512],
                                                                   start=(h == 0), stop=(h == NHC - 1)), reads=[ocT, wo_sb], writes=[pp])
                            k.op("dve", lambda e, nb=nb: e.tensor_tensor(out=xo.t[:, nb * 512:(nb + 1) * 512], in0=pp.t[:],
                                                                         in1=xt.t[:, nb * 512:(nb + 1) * 512], op=ALU.add),
                                 reads=[pp, xt], writes=[xo])
                        k.dma("sp", lambda e: e.dma_start(out=xres[r0:r0 + 128, :], in_=xo.t[:]), reads=[xo], writes=[R("xres", r0 // 128)])
            st_.close()

        if "x2" in dbg and l == 0:
            k.dma("sp", lambda e: e.dma_start(out=dbg["x2"][:, :], in_=xres[:, :]), writes=[R("dbg", "x2")])
            k.barrier()

        if "F" in STAGES:
            gates = LS.sb("gates", [128, NT, 2], F32)
            desti = LS.sb("desti", [128, NT * 2], I32)
            widx = LS.sb("widx", [128, NB], I32)
            st_ = Stage()
            xt2 = [st_.sb("xt", [128, D], F32) for _ in range(2)]
            hb2 = [st_.sb("hb", [128, D], BF16) for _ in range(2)]
            junk = st_.sb("junk", [128, D], BF16)
            gffn = st_.sb("gffn", [128, D], F32)
            wr = st_.sb("wr", [128, KC, 72], BF16)
            hTt = st_.sb("hTt", [128, KC, 128], BF16)
            OH = st_.sb("OH", [128, NT, 2, NE], F32)
            RANK = st_.sb("RANK", [128, NT, NE], F32)
            cum = st_.sb("cum", [128, NE], F32)
            rt = [st_.sb("rt", [128, 128], F32) for i in range(6)]
            Mb = st_.sb("Mb", [128, NE], BF16)
            destf = st_.sb("destf", [128, NT * 2], F32)
            pcs = [st_.sb("pcs", [128, NE], F32) for i in range(3)]
            pci = st_.sb("pci", [128, NE], I32)
            thr = st_.sb("thr_sb", [128, NB], F32)
            cmpb = st_.sb("cmpb", [128, NB, NE], F32)
            bef = st_.sb("bef", [128, NB], F32)
            k.dma("sp", lambda e: e.dma_start(out=thr.t[:], in_=thr_in[:, :]), writes=[thr])
            k.dma("sp", lambda e: e.dma_start(out=gffn.t[:], in_=gffn_in[l, :, :]), writes=[gffn])
            k.dma("pool", lambda e: e.dma_start(out=wr.t[:], in_=w_rt[l].rearrange("(kc p) n -> p kc n", p=128)), writes=[wr])
            k.op("dve", lambda e: e.memset(cum.t[:], 0.0), writes=[cum])
            o_brt = _off["brt"][0]
            for tt in range(NT):
                r0 = tt * 128
                xt = nxt(xt2, "x")
                k.dma("sp", lambda e: e.dma_start(out=xt.t[:], in_=xres[r0:r0 + 128, :]), reads=[R("xres", tt)], writes=[xt])
                c0 = nxt(col, "c")
                k.op("act", lambda e: e.activation(out=junk.t[:], in_=xt.t[:], func=AF.Square, accum_out=c0.t[:, 0:1]),
                     reads=[xt], writes=[junk, c0])
                k.op("act", lambda e: e.activation(out=c0.t[:, 1:2], in_=c0.t[:, 0:1], func=AF.Sqrt, scale=1.0 / D, bias=EPS), reads=[c0], writes=[c0])
                k.op("dve", lambda e: e.reciprocal(out=c0.t[:, 2:3], in_=c0.t[:, 1:2]), reads=[c0], writes=[c0])
                hb = nxt(hb2, "hb")
                k.op("dve", lambda e: e.scalar_tensor_tensor(out=hb.t[:], in0=xt.t[:], scalar=c0.t[:, 2:3], in1=gffn.t[:],
                                                             op0=ALU.mult, op1=ALU.mult), reads=[xt, c0, gffn], writes=[hb])
                k.dma("sp", lambda e: e.dma_start(out=Hrows[r0:r0 + 128, :], in_=hb.t[:]), reads=[hb], writes=[R("hrows", tt)])
                for kc in range(KC):
                    k.op("pe", lambda e, kc=kc: e.transpose(out=pT.t[:, kc, :], in_=hb.t[:, kc * 128:(kc + 1) * 128], identity=ident_bf.t[:]),
                         reads=[hb, ident_bf], writes=[pT])
                k.op("act", lambda e: e.activation(out=hTt.t[:], in_=pT.t[:], func=AF.Copy), reads=[pT], writes=[hTt])
                pp = nxt(pA, "pa")
                for kc in range(KC):
                    k.op("pe", lambda e, kc=kc: e.matmul(pp.t[:, 0:72], lhsT=hTt.t[:, kc, :], rhs=wr.t[:, kc, :], start=(kc == 0), stop=(kc == KC - 1)),
                         reads=[hTt, wr], writes=[pp])
                lg, w1_, w2_, w3_, w4_, w5_ = rt
                k.op("dve", lambda e: e.tensor_tensor(out=lg.t[:, 0:72], in0=pp.t[:, 0:72], in1=pars.t[:, o_brt:o_brt + 72], op=ALU.add),
                     reads=[pp, pars], writes=[lg])
                k.op("dve", lambda e: e.tensor_reduce(out=w1_.t[:, 0:1], in_=lg.t[:, 0:8], axis=AX.X, op=ALU.max), reads=[lg], writes=[w1_])
                k.op("dve", lambda e: e.tensor_scalar(out=w2_.t[:, 0:8], in0=lg.t[:, 0:8], scalar1=w1_.t[:, 0:1], scalar2=None, op0=ALU.is_equal),
                     reads=[lg, w1_], writes=[w2_])
                k.op("dve", lambda e: e.tensor_scalar(out=w1_.t[:, 1:2], in0=w1_.t[:, 0:1], scalar1=-1.0, scalar2=None, op0=ALU.mult),
                     reads=[w1_], writes=[w1_])
                k.op("act", lambda e: e.activation(out=w2_.t[:, 8:16], in_=lg.t[:, 0:8], func=AF.Exp, bias=w1_.t[:, 1:2], accum_out=w1_.t[:, 2:3]),
                     reads=[lg, w1_], writes=[w2_, w1_])
                k.op("dve", lambda e: e.reciprocal(out=w1_.t[:, 3:4], in_=w1_.t[:, 2:3]), reads=[w1_], writes=[w1_])
                k.op("dve", lambda e: e.tensor_tensor(out=w3_.t[:, 0:64].rearrange("p (g j) -> p g j", j=8),
                                                      in0=lg.t[:, 8:72].rearrange("p (g j) -> p g j", j=8),
                                                      in1=w2_.t[:, 0:8].unsqueeze(2).to_broadcast([128, 8, 8]), op=ALU.mult),
                     reads=[lg, w2_], writes=[w3_])
                k.op("dve", lambda e: e.tensor_reduce(out=w4_.t[:, 0:8], in_=w3_.t[:, 0:64].rearrange("p (g j) -> p j g", j=8), axis=AX.X, op=ALU.add),
                     reads=[w3_], writes=[w4_])
                k.op("dve", lambda e: e.tensor_reduce(out=w1_.t[:, 4:5], in_=w4_.t[:, 0:8], axis=AX.X, op=ALU.max), reads=[w4_], writes=[w1_])
                k.op("dve", lambda e: e.tensor_scalar(out=w4_.t[:, 8:16], in0=w4_.t[:, 0:8], scalar1=w1_.t[:, 4:5], scalar2=None, op0=ALU.is_equal),
                     reads=[w4_, w1_], writes=[w4_])
                k.op("dve", lambda e: e.scalar_tensor_tensor(out=w4_.t[:, 16:24], in0=w4_.t[:, 8:16], scalar=-1e30, in1=w4_.t[:, 0:8],
                                                             op0=ALU.mult, op1=ALU.add), reads=[w4_], writes=[w4_])
                k.op("dve", lambda e: e.tensor_reduce(out=w1_.t[:, 5:6], in_=w4_.t[:, 16:24], axis=AX.X, op=ALU.max), reads=[w4_], writes=[w1_])
                k.op("dve", lambda e: e.tensor_scalar(out=w4_.t[:, 24:32], in0=w4_.t[:, 16:24], scalar1=w1_.t[:, 5:6], scalar2=None, op0=ALU.is_equal),
                     reads=[w4_, w1_], writes=[w4_])
                k.op("dve", lambda e: e.tensor_tensor(out=w1_.t[:, 6:7], in0=w1_.t[:, 5:6], in1=w1_.t[:, 4:5], op=ALU.subtract), reads=[w1_], writes=[w1_])
                k.op("act", lambda e: e.activation(out=w1_.t[:, 7:8], in_=w1_.t[:, 6:7], func=AF.Exp), reads=[w1_], writes=[w1_])
                k.op("dve", lambda e: e.tensor_scalar(out=w1_.t[:, 8:9], in0=w1_.t[:, 7:8], scalar1=1.0, scalar2=None, op0=ALU.add), reads=[w1_], writes=[w1_])
                k.op("dve", lambda e: e.reciprocal(out=w1_.t[:, 9:10], in_=w1_.t[:, 8:9]), reads=[w1_], writes=[w1_])
                k.op("dve", lambda e: e.tensor_tensor(out=gates.t[:, tt, 0:1], in0=w1_.t[:, 3:4], in1=w1_.t[:, 9:10], op=ALU.mult), reads=[w1_], writes=[gates])
                k.op("dve", lambda e: e.tensor_tensor(out=gates.t[:, tt, 1:2], in0=gates.t[:, tt, 0:1], in1=w1_.t[:, 7:8], op=ALU.mult), reads=[w1_], writes=[gates])
                for kk in range(2):
                    k.op("dve", lambda e, kk=kk: e.tensor_tensor(
                        out=OH.t[:, tt, kk, :].rearrange("p (g j) -> p g j", j=8),
                        in0=w2_.t[:, 0:8].unsqueeze(2).to_broadcast([128, 8, 8]),
                        in1=w4_.t[:, 8 + 16 * kk:16 + 16 * kk].unsqueeze(1).to_broadcast([128, 8, 8]), op=ALU.mult),
                         reads=[w2_, w4_], writes=[OH])
                k.op("dve", lambda e: e.tensor_tensor(out=Mb.t[:], in0=OH.t[:, tt, 0, :], in1=OH.t[:, tt, 1, :], op=ALU.add), reads=[OH], writes=[Mb])
                pr = nxt(pA, "pa")
                k.op("pe", lambda e: e.matmul(pr.t[:, 0:NE], lhsT=ltri_bf.t[:], rhs=Mb.t[:], start=True, stop=True), reads=[ltri_bf, Mb], writes=[pr])
                k.op("dve", lambda e: e.tensor_tensor(out=RANK.t[:, tt, :], in0=pr.t[:, 0:NE], in1=cum.t[:], op=ALU.add), reads=[pr, cum], writes=[RANK])
                pc_ = nxt(pA, "pa")
                k.op("pe", lambda e: e.matmul(pc_.t[:, 0:NE], lhsT=ones_bf.t[:], rhs=Mb.t[:], start=True, stop=True), reads=[ones_bf, Mb], writes=[pc_])
                k.op("dve", lambda e: e.tensor_tensor(out=cum.t[:], in0=cum.t[:], in1=pc_.t[:, 0:NE], op=ALU.add), reads=[pc_], writes=[cum])
            k.op("dve", lambda e: e.tensor_scalar(out=pcs[0].t[:], in0=cum.t[:], scalar1=float(BLK - 1), scalar2=None, op0=ALU.add), reads=[cum], writes=[pcs[0]])
            k.op("dve", lambda e: e.tensor_copy(out=pci.t[:], in_=pcs[0].t[:]), reads=[pcs[0]], writes=[pci])
            sh = int(math.log2(BLK))
            k.op("dve", lambda e: e.tensor_single_scalar(out=pci.t[:], in_=pci.t[:], scalar=sh, op=ALU.arith_shift_right), writes=[pci])
            k.op("dve", lambda e: e.tensor_single_scalar(out=pci.t[:], in_=pci.t[:], scalar=sh, op=ALU.logical_shift_left), writes=[pci])
            k.op("dve", lambda e: e.tensor_copy(out=pcs[0].t[:], in_=pci.t[:]), reads=[pci], writes=[pcs[0]])
            k.op("dve", lambda e: e.tensor_copy(out=pcs[1].t[:], in_=pcs[0].t[:]), reads=[pcs[0]], writes=[pcs[1]])
            cur, oth = pcs[1], pcs[2]
            stp = 1
            while stp < NE:
                k.op("dve", lambda e, stp=stp, cur=cur, oth=oth: e.tensor_copy(out=oth.t[:, 0:stp], in_=cur.t[:, 0:stp]), reads=[cur], writes=[oth])
                k.op("dve", lambda e, stp=stp, cur=cur, oth=oth: e.tensor_tensor(out=oth.t[:, stp:NE], in0=cur.t[:, stp:NE], in1=cur.t[:, 0:NE - stp], op=ALU.add),
                     reads=[cur], writes=[oth])
                cur, oth = oth, cur
                stp *= 2
            pend = cur
            pstart = oth
            k.op("dve", lambda e: e.tensor_tensor(out=pstart.t[:], in0=pend.t[:], in1=pcs[0].t[:], op=ALU.subtract), reads=[pend, pcs[0]], writes=[pstart])
            for tt in range(NT):
                k.op("dve", lambda e, tt=tt: e.tensor_tensor(out=RANK.t[:, tt, :], in0=RANK.t[:, tt, :], in1=pstart.t[:], op=ALU.add), reads=[pstart], writes=[RANK])
                for kk in range(2):
                    k.op("dve", lambda e, tt=tt, kk=kk: e.tensor_tensor(out=rt[2].t[:, 0:NE], in0=RANK.t[:, tt, :], in1=OH.t[:, tt, kk, :], op=ALU.mult),
                         reads=[RANK, OH], writes=[rt[2]])
                    k.op("dve", lambda e, tt=tt, kk=kk: e.tensor_reduce(out=destf.t[:, tt * 2 + kk:tt * 2 + kk + 1], in_=rt[2].t[:, 0:NE], axis=AX.X, op=ALU.add),
                         reads=[rt[2]], writes=[destf])
            k.op("dve", lambda e: e.tensor_copy(out=desti.t[:], in_=destf.t[:]), reads=[destf], writes=[desti])
            k.op("dve", lambda e: e.tensor_tensor(out=cmpb.t[:], in0=pend.t[:].unsqueeze(1).to_broadcast([128, NB, NE]),
                                                  in1=thr.t[:].unsqueeze(2).to_broadcast([128, NB, NE]), op=ALU.is_le), reads=[pend, thr], writes=[cmpb])
            k.op("dve", lambda e: e.tensor_reduce(out=bef.t[:], in_=cmpb.t[:], axis=AX.X, op=ALU.add), reads=[cmpb], writes=[bef])
            k.op("dve", lambda e: e.tensor_scalar(out=bef.t[:], in0=bef.t[:], scalar1=float(NE - 1), scalar2=128.0, op0=ALU.min, op1=ALU.mult), writes=[bef])
            k.op("dve", lambda e: e.tensor_scalar(out=bef.t[:], in0=bef.t[:], scalar1=iota_p, scalar2=None, op0=ALU.add), reads=[cst], writes=[bef])
            k.op("dve", lambda e: e.tensor_copy(out=widx.t[:], in_=bef.t[:]), reads=[bef], writes=[widx])

            k.op("dve", lambda e: e.memset(junk.t[:], 0.0), writes=[junk])
            RPP = PR // 128
            zc = 24
            xs_v = Xs.rearrange("(p r) d -> p r d", p=128)
            for z0 in range(0, RPP, zc):
                zn = min(zc, RPP - z0)
                k.dma("sp", lambda e, z0=z0, zn=zn: e.dma_start(out=xs_v[:, z0:z0 + zn, :],
                                                                 in_=junk.t[:].unsqueeze(1).to_broadcast([128, zn, D])),
                      reads=[junk], writes=[R("xs")], accumulate_writes=True)
            k.barrier()
            for tt in range(NT):
                hb = nxt(hb2, "hb")
                k.dma("sp", lambda e: e.dma_start(out=hb.t[:], in_=Hrows[tt * 128:(tt + 1) * 128, :]), reads=[R("hrows", tt)], writes=[hb])
                for kk in range(2):
                    k.dma("pool", lambda e, kk=kk: e.indirect_dma_start(
                        out=Xs[:, :], out_offset=bass.IndirectOffsetOnAxis(ap=desti.t[:, tt * 2 + kk:tt * 2 + kk + 1], axis=0),
                        in_=hb.t[:], in_offset=None), reads=[hb, desti], writes=[R("xs")], accumulate_writes=True)

            if "moe" in dbg and l == 0:
                k.dma("sp", lambda e: e.dma_start(out=dbg["moe"][:, 0:NT * 2], in_=destf.t[:]), reads=[destf], writes=[R("dbg", "moe")])
                k.dma("sp", lambda e: e.dma_start(out=dbg["moe"][:, NT * 2:NT * 2 + NB], in_=bef.t[:]), reads=[bef], writes=[R("dbg", "moe2")])
                k.dma("sp", lambda e: e.dma_start(out=dbg["moe"][:, NT * 2 + NB:NT * 4 + NB], in_=gates.t[:].rearrange("p t k -> p (t k)")),
                      reads=[gates], writes=[R("dbg", "moe3")])
            st_.close()

            st_ = Stage()
            yo2 = [st_.sb("yo", [128, D], F32) for _ in range(2)]
            xsb = [st_.sb("xsb", [128, 2, D], BF16) for i in range(2)]
            xTb = st_.sb("xTb", [128, KC, BLK], BF16)
            hid = st_.sb("hid", [128, FC, BLK], BF16)
            w1b = [st_.sb("w1b", [128, KC, DE], BF16) for i in range(2)]
            w3b = [st_.sb("w3b", [128, KC, DE], BF16) for i in range(2)]
            w2b = [st_.sb("w2b", [128, FC, D], BF16) for i in range(2)]

            def load_block(b):
                i = b % 2
                k.dma("sp", lambda e: e.dma_start(out=xsb[i].t[:], in_=Xs[b * BLK:(b + 1) * BLK, :].rearrange("(r p) d -> p r d", p=128)),
                      reads=[R("xs")], writes=[xsb[i]])
                for wsb, wdr, nch, g in ((w1b[i], w1r, NCH, GK), (w3b[i], w3r, NCH, GK), (w2b[i], w2r, FC, 1)):
                    for c in range(nch):
                        k.dma("pool", lambda e, wsb=wsb, wdr=wdr, c=c, g=g: e.indirect_dma_start(
                            out=wsb.t[:, c * g:(c + 1) * g, :].rearrange("p a b -> p (a b)"), out_offset=None, in_=wdr[l][c][:, :],
                            in_offset=bass.IndirectOffsetOnAxis(ap=widx.t[:, b:b + 1], axis=0)), reads=[widx], writes=[wsb],
                              accumulate_writes=(c > 0))

            load_block(0)
            for b in range(NB):
                i = b % 2
                if b + 1 < NB:
                    load_block(b + 1)
                for r in range(2):
                    for kc in range(KC):
                        k.op("pe", lambda e, kc=kc, r=r: e.transpose(out=pT.t[:, kc, :], in_=xsb[i].t[:, r, kc * 128:(kc + 1) * 128], identity=ident_bf.t[:]),
                             reads=[xsb[i], ident_bf], writes=[pT])
                    k.op("act", lambda e, r=r: e.activation(out=xTb.t[:, :, r * 128:(r + 1) * 128], in_=pT.t[:], func=AF.Copy), reads=[pT], writes=[xTb])
                for fc in range(FC):
                    p1 = nxt(pA, "pa")
                    p3 = nxt(pA, "pa")
                    for kc in range(KC):
                        k.op("pe", lambda e, kc=kc: e.matmul(p1.t[:, 0:BLK], lhsT=w1b[i].t[:, kc, fc * 128:(fc + 1) * 128], rhs=xTb.t[:, kc, :],
                                                             start=(kc == 0), stop=(kc == KC - 1)), reads=[w1b[i], xTb], writes=[p1])
                    for kc in range(KC):
                        k.op("pe", lambda e, kc=kc: e.matmul(p3.t[:, 0:BLK], lhsT=w3b[i].t[:, kc, fc * 128:(fc + 1) * 128], rhs=xTb.t[:, kc, :],
                                                             start=(kc == 0), stop=(kc == KC - 1)), reads=[w3b[i], xTb], writes=[p3])
                    sg = nxt(f512, "f")
                    k.op("act", lambda e: e.activation(out=sg.t[:, 0:BLK], in_=p1.t[:, 0:BLK], func=AF.Silu), reads=[p1], writes=[sg])
                    k.op("dve", lambda e: e.tensor_tensor(out=hid.t[:, fc, :], in0=p3.t[:, 0:BLK], in1=sg.t[:, 0:BLK], op=ALU.mult),
                         reads=[p3, sg], writes=[hid])
                for r in range(2):
                    yo = nxt(yo2, "yo")
                    for nb in range(4):
                        pp = nxt(pA, "pa")
                        for fc in range(FC):
                            k.op("pe", lambda e, fc=fc: e.matmul(pp.t[:], lhsT=hid.t[:, fc, r * 128:(r + 1) * 128], rhs=w2b[i].t[:, fc, nb * 512:(nb + 1) * 512],
                                                                 start=(fc == 0), stop=(fc == FC - 1)), reads=[hid, w2b[i]], writes=[pp])
                        if nb % 2 == 0:
                            k.op("act", lambda e, nb=nb: e.activation(out=yo.t[:, nb * 512:(nb + 1) * 512], in_=pp.t[:], func=AF.Copy), reads=[pp], writes=[yo])
                        else:
                            k.op("dve", lambda e, nb=nb: e.tensor_copy(out=yo.t[:, nb * 512:(nb + 1) * 512], in_=pp.t[:]), reads=[pp], writes=[yo])
                    rr0 = b * BLK + r * 128
                    k.dma("sp", lambda e: e.dma_start(out=Ys[rr0:rr0 + 128, :], in_=yo.t[:]), reads=[yo], writes=[R("ys")], accumulate_writes=True)
            st_.close()

            st_ = Stage()
            xt2 = [st_.sb("xt", [128, D], F32) for _ in range(2)]
            y02 = [st_.sb("y0", [128, D], F32) for _ in range(2)]
            y12 = [st_.sb("y1", [128, D], F32) for _ in range(2)]
            dst = out if last else xres
            for tt in range(NT):
                r0 = tt * 128
                xt = nxt(xt2, "x")
                k.dma("sp", lambda e: e.dma_start(out=xt.t[:], in_=xres[r0:r0 + 128, :]), reads=[R("xres", tt)], writes=[xt])
                ys = [nxt(y02, "y0"), nxt(y12, "y1")]
                for kk in range(2):
                    k.dma("pool", lambda e, kk=kk: e.indirect_dma_start(
                        out=ys[kk].t[:], out_offset=None, in_=Ys[:, :],
                        in_offset=bass.IndirectOffsetOnAxis(ap=desti.t[:, tt * 2 + kk:tt * 2 + kk + 1], axis=0)),
                          reads=[R("ys"), desti], writes=[ys[kk]])
                k.op("dve", lambda e: e.scalar_tensor_tensor(out=xt.t[:], in0=ys[0].t[:], scalar=gates.t[:, tt, 0:1], in1=xt.t[:],
                                                             op0=ALU.mult, op1=ALU.add), reads=[ys[0], gates], writes=[xt])
                k.op("dve", lambda e: e.scalar_tensor_tensor(out=xt.t[:], in0=ys[1].t[:], scalar=gates.t[:, tt, 1:2], in1=xt.t[:],
                                                             op0=ALU.mult, op1=ALU.add), reads=[ys[1], gates], writes=[xt])
                k.dma("sp", lambda e: e.dma_start(out=dst[r0:r0 + 128, :], in_=xt.t[:]), reads=[xt],
                      writes=[R("out" if last else "xres", tt)])
            st_.close()
        LS.close()

    k.barrier()
    return nc, k


def t5_bucket_np(dist):
    max_exact = 16
    d_f = np.maximum(dist, 1).astype(np.float32)
    large = max_exact + (np.log(d_f / np.float32(max_exact)) / np.float32(math.log(128 / max_exact)) * np.float32(16)).astype(np.int32)
    large = np.minimum(large, 31)
    return np.where(dist < max_exact, dist, large)


def host_prep(inp, L, S, DE):
    f = lambda a: np.ascontiguousarray(np.asarray(a, dtype=np.float32))
    rep = lambda v: np.broadcast_to(np.asarray(v, np.float32).reshape(1, -1), (128, np.asarray(v).size))
    pars = np.zeros((L, 128, NPAR), np.float32)

    def put(l, name, arr):
        o, w = _off[name]
        pars[l, :, o:o + w] = arr.reshape(128, w)

    tbl = np.asarray(inp["rel_bias_table"], np.float32)
    for l in range(L):
        put(l, "gmixT", np.asarray(inp["g_mix"][l]).reshape(KC, 128).T)
        put(l, "gcrossT", np.asarray(inp["g_cross"][l]).reshape(KC, 128).T)
        put(l, "gmemT", np.asarray(inp["g_mem"][l]).reshape(KC, 128).T)
        put(l, "gq2", np.tile(np.asarray(inp["g_q"][l]), 2))
        put(l, "gk2", np.tile(np.asarray(inp["g_k"][l]), 2))
        put(l, "gsub", np.asarray(inp["g_subln"][l]))
        put(l, "gqc", np.asarray(inp["g_qc"][l]))
        put(l, "gkc", np.asarray(inp["g_kc"][l]))
        cw = np.asarray(inp["conv_w"][l])
        put(l, "convw", cw.reshape(CW, 8, 128).transpose(2, 1, 0))
        put(l, "convb", np.asarray(inp["conv_b"][l]).reshape(8, 128).T)
        put(l, "lng", np.asarray(inp["conv_ln_g"][l]).reshape(8, 128).T)
        put(l, "lnb", np.asarray(inp["conv_ln_b"][l]).reshape(8, 128).T)
        put(l, "dl", rep(np.asarray(inp["diff_lambda"][l]).reshape(-1)))
        put(l, "brt", rep(np.concatenate([np.asarray(inp["b_group"][l]), np.asarray(inp["b_router"][l])])))
        put(l, "relc", rep(tbl[31, :]))
    gffnb = np.stack([rep(inp["g_ffn"][l]) for l in range(L)]).astype(np.float32)
    kk_, qq_ = np.meshgrid(np.arange(128), np.arange(128), indexing="ij")
    biasT = np.zeros((128, NH * 2, 128), np.float32)
    for dlt in range(2):
        dist = dlt * 128 + qq_ - kk_
        bk = t5_bucket_np(np.maximum(dist, 0))
        for h in range(NH):
            biasT[:, h * 2 + dlt, :] = np.where(dist >= 0, tbl[bk, h], np.float32(MASKV))
    consts = np.zeros((128, 5, 128), np.float32)
    consts[:, 0, :] = np.eye(128)
    consts[:, 1, :] = 1.0
    consts[0:64, 2, 0:64] = 1.0
    consts[64:128, 2, 64:128] = 1.0
    consts[:, 3, :] = (kk_ < qq_)
    consts[:, 4, :] = np.arange(128).reshape(128, 1)
    w_rt = np.concatenate([np.asarray(inp["w_group"], np.float32)[:L], np.asarray(inp["w_router"], np.float32)[:L]], axis=-1)
    FC = DE // 128
    shared = {
        "pars": pars, "gffnb": gffnb, "biasT": biasT, "consts": consts,
        "w_in": f(inp["w_in"][:L]), "w_out": f(inp["w_out"][:L]), "wq_c": f(inp["wq_c"][:L]), "wkv_c": f(inp["wkv_c"][:L]),
        "wo_c": f(inp["wo_c"][:L]), "w_rt": f(w_rt),
    }
    if "F" in STAGES:
        NCH = KC * DE // 2048
        GK = 2048 // DE
        for nm, src_ in (("w1r", "w1"), ("w3r", "w3")):
            for l in range(L):
                a6 = np.asarray(inp[src_][l], np.float32).reshape(NE, NCH, GK, 128, DE)
                for c in range(NCH):
                    shared[f"{nm}_{l}_{c}"] = f(a6[:, c].transpose(0, 2, 1, 3).reshape(NE * 128, GK * DE))
        for l in range(L):
            a5 = np.asarray(inp["w2"][l], np.float32).reshape(NE, FC, 128, D)
            for c in range(FC):
                shared[f"w2r_{l}_{c}"] = f(a5[:, c].reshape(NE * 128, D))
    return shared


_CACHE = {}


def run(inp, L, NSEQ, S, DE, debug=()):
    key = (L, NSEQ, S, DE, tuple(debug))
    if key not in _CACHE:
        _CACHE[key] = build_program(L, NSEQ, S, DE, debug)
    nc, kb = _CACHE[key]
    shared = host_prep(inp, L, S, DE)
    T = NSEQ * S
    NB = -(-(2 * T + NE * (BLK - 1)) // BLK)
    shared["thr"] = np.ascontiguousarray(np.broadcast_to((np.arange(NB, dtype=np.float32) * BLK).reshape(1, NB), (128, NB)))
    x = np.asarray(inp["x"], np.float32)
    mem = np.asarray(inp["mem"], np.float32)
    in_maps = []
    for c in range(NCORES):
        m = dict(shared)
        m["x"] = np.ascontiguousarray(x[c * NSEQ:(c + 1) * NSEQ].reshape(T, D))
        m["mem"] = np.ascontiguousarray(mem[c * NSEQ:(c + 1) * NSEQ].reshape(NSEQ * MEM, D))
        in_maps.append(m)
    res = run_bass_kernel_spmd(nc, in_maps, core_ids=list(range(NCORES)))
    return res


def kernel(**inputs):
    L, NSEQ, S, DE = 2, 2, 2048, 512
    res = run(inputs, L, NSEQ, S, DE)
    outs = [r["out"].reshape(NSEQ, S, D) for r in res.results]
    return np.concatenate(outs, axis=0).astype(np.float32)
```

```python
import math
from contextlib import ExitStack

import numpy as np
import concourse.bass as bass
import concourse.mybir as mybir
from concourse.bass_utils import run_bass_kernel_spmd

AF = mybir.ActivationFunctionType
ALU = mybir.AluOpType
AX = mybir.AxisListType
F32 = mybir.dt.float32
BF16 = mybir.dt.bfloat16
I32 = mybir.dt.int32

D = 2048
KC = D // 128
DQ = 64
NH = 8
D_IN = 5120
CW = 31
MEM = 256
NHC = 4
NG = 8
NE = 64
BLK = 256
EPS = 1e-6
NCORES = 8
MASKV = -30000.0
STAGES = "ABCDEF"

_off = {}
_n = 0
for _name, _w in [("gmixT", 16), ("gcrossT", 16), ("gmemT", 16), ("gq2", 1), ("gk2", 1), ("gsub", 1),
                  ("gqc", 1), ("gkc", 1), ("convw", 8 * CW), ("convb", 8), ("lng", 8), ("lnb", 8),
                  ("dl", 4 * DQ), ("brt", 72), ("relc", 8)]:
    _off[_name] = (_n, _w)
    _n += _w
NPAR = _n


class Buf:
    __slots__ = ("t", "w", "r")

    def __init__(self, t):
        self.t = t
        self.w = []
        self.r = []


class KB:
    NRING = 64

    def __init__(self, nc, es):
        self.nc = nc
        self.es = es
        self.eng = {"pe": nc.tensor, "dve": nc.vector, "act": nc.scalar, "pool": nc.gpsimd, "sp": nc.sync}
        self.chan = {}
        self.waited = {e: {} for e in self.eng}
        self.ring = [es.enter_context(nc.semaphore(f"dq{i}")) for i in range(self.NRING)]
        self.ring_cnt = [0] * self.NRING
        self.ring_i = 0
        self.nsem = 0
        self.ninstr = 0

    def _chan(self, e):
        c = self.chan.get(e)
        if c is None or c[2] >= 30000:
            self.nsem += 1
            c = [f"c{e}{self.nsem}", self.es.enter_context(self.nc.semaphore(f"c_{e}_{self.nsem}")), 0]
            self.chan[e] = c
        return c

    def _wait(self, e, toks):
        wd = self.waited[e]
        for (key, sem, v, prod) in toks:
            if prod == e and e == "pe":
                continue
            if wd.get(key, 0) >= v:
                continue
            wd[key] = v
            self.eng[e].wait_ge(sem, v)

    @staticmethod
    def _prune(lst):
        last = {}
        out = []
        for t in lst:
            if t[3] == "dma":
                out.append(t)
            else:
                last[t[3]] = t
        return out + list(last.values())

    def _deps(self, e, reads, writes):
        toks = []
        for b in reads:
            toks += b.w
        for b in writes:
            toks += b.w
            toks += b.r
        self._wait(e, toks)

    def _commit(self, tok, reads, writes):
        for b in reads:
            b.r.append(tok)
            if len(b.r) > 6:
                b.r = self._prune(b.r)
        for b in writes:
            b.w = [tok]
            b.r = []

    def op(self, e, fn, reads=(), writes=()):
        self._deps(e, reads, writes)
        ins = fn(self.eng[e])
        c = self._chan(e)
        c[2] += 1
        ins.then_inc(c[1], 1)
        tok = (c[0], c[1], c[2], e)
        self._commit(tok, reads, writes)
        self.ninstr += 1
        return tok

    def dma(self, q, fn, reads=(), writes=(), accumulate_writes=False):
        if accumulate_writes:
            toks = []
            for b in reads:
                toks += b.w
            for b in writes:
                if b.r:
                    toks += b.w
                    toks += b.r
            self._wait(q, toks)
        else:
            self._deps(q, reads, writes)
        i = self.ring_i
        self.ring_i = (i + 1) % self.NRING
        if self.ring_cnt[i] > 0:
            self._wait(q, [(f"dq{i}", self.ring[i], self.ring_cnt[i], "dma")])
        ins = fn(self.eng[q])
        self.ring_cnt[i] += 16
        ins.then_inc(self.ring[i], 16)
        tok = (f"dq{i}", self.ring[i], self.ring_cnt[i], "dma")
        for b in reads:
            b.r.append(tok)
        for b in writes:
            if accumulate_writes and not b.r:
                b.w.append(tok)
            else:
                b.w = [tok]
                b.r = []
        self.ninstr += 1
        return tok

    def barrier(self):
        toks = [(c[0], c[1], c[2], e) for e, c in self.chan.items() if c[2] > 0]
        toks += [(f"dq{i}", self.ring[i], self.ring_cnt[i], "dma") for i in range(self.NRING) if self.ring_cnt[i] > 0]
        for e in self.eng:
            self._wait(e, toks)

    def drain(self, e, bufs):
        toks = []
        for b in bufs:
            toks += b.w
            toks += b.r
        self._wait(e, toks)


def build_program(L, NSEQ, S, DE, debug=()):
    T = NSEQ * S
    NT = T // 128
    NB = -(-(2 * T + NE * (BLK - 1)) // BLK)
    PR = NB * BLK
    FC = DE // 128
    nc = bass.Bass("TRN2", target_bir_lowering=False)
    es = ExitStack()

    def dram(name, shape, dt, kind="Internal"):
        return nc.dram_tensor(name, list(shape), dt, kind=kind).ap()

    x_in = dram("x", [T, D], F32, "ExternalInput")
    mem_in = dram("mem", [NSEQ * MEM, D], F32, "ExternalInput")
    pars_in = dram("pars", [L, 128, NPAR], F32, "ExternalInput")
    gffn_in = dram("gffnb", [L, 128, D], F32, "ExternalInput")
    biasT_in = dram("biasT", [128, NH * 2, 128], F32, "ExternalInput")
    consts_in = dram("consts", [128, 5, 128], F32, "ExternalInput")
    thr_in = dram("thr", [128, NB], F32, "ExternalInput")
    w_in = dram("w_in", [L, D, D_IN], F32, "ExternalInput")
    w_out = dram("w_out", [L, D, D], F32, "ExternalInput")
    wq_c = dram("wq_c", [L, D, 512], F32, "ExternalInput")
    wkv_c = dram("wkv_c", [L, D, 1024], F32, "ExternalInput")
    wo_c = dram("wo_c", [L, 512, D], F32, "ExternalInput")
    w_rt = dram("w_rt", [L, D, 72], F32, "ExternalInput")
    if "F" in STAGES:
        NCH = KC * DE // 2048
        GK = 2048 // DE
        w1r = [[dram(f"w1r_{l_}_{c}", [NE * 128, 2048], F32, "ExternalInput") for c in range(NCH)] for l_ in range(L)]
        w3r = [[dram(f"w3r_{l_}_{c}", [NE * 128, 2048], F32, "ExternalInput") for c in range(NCH)] for l_ in range(L)]
        w2r = [[dram(f"w2r_{l_}_{c}", [NE * 128, 2048], F32, "ExternalInput") for c in range(FC)] for l_ in range(L)]
    out = dram("out", [T, D], F32, "ExternalOutput")

    xres = dram("xres", [T, D], F32)
    qTs = dram("qTs", [NSEQ, NH, 128, S], BF16)
    kTs = dram("kTs", [NSEQ, NH, 128, S], BF16)
    Vs = dram("Vs", [NSEQ, S, NH * 128], BF16)
    Us = dram("Us", [NSEQ, 8, 128, S], F32)
    mixT = dram("mixT", [16, 128, T], BF16)
    Hrows = dram("Hrows", [T, D], BF16)
    Xs = dram("Xs", [PR, D], BF16)
    Ys = dram("Ys", [PR, D], F32)
    dbg = {}
    for name, shape, dt in debug:
        dbg[name] = dram("dbg_" + name, shape, dt, "ExternalOutput")

    k = KB(nc, es)
    regions = {}
    uid = [0]

    def R(*key):
        b = regions.get(key)
        if b is None:
            b = Buf(None)
            regions[key] = b
        return b

    class Stage:
        def __init__(self):
            self.es = ExitStack()

        def sb(self, name, shape, dt):
            uid[0] += 1
            return Buf(self.es.enter_context(nc.sbuf_tensor(f"{name}_{uid[0]}", list(shape), dt)))

        def close(self):
            k.barrier()
            self.es.close()

    G = Stage()

    def ps(name, shape, dt=F32):
        return Buf(es.enter_context(nc.psum_tensor(name, list(shape), dt)))

    cst = G.sb("cst", [128, 5, 128], F32)
    ident_bf = G.sb("ident_bf", [128, 128], BF16)
    ones_bf = G.sb("ones_bf", [128, 128], BF16)
    ltri_bf = G.sb("ltri_bf", [128, 128], BF16)
    biasT = G.sb("biasT_sb", [128, NH * 2, 128], BF16)
    pars = G.sb("pars_sb", [128, NPAR], F32)
    small = G.sb("small", [128, 16], F32)
    col = [G.sb(f"col{i}", [128, 8], F32) for i in range(4)]
    f512 = [G.sb(f"f512_{i}", [128, 512], F32) for i in range(6)]
    b512 = [G.sb(f"b512_{i}", [128, 512], BF16) for i in range(4)]
    ones_f = cst.t[:, 1, :]
    bd64_f = cst.t[:, 2, :]
    iota_p = cst.t[:, 4, 0:1]
    pT = ps("pT", [128, KC, 128], BF16)
    pA = [ps(f"pA{i}", [128, 512]) for i in range(6)]

    k.dma("sp", lambda e: e.dma_start(out=cst.t[:], in_=consts_in[:, :, :]), writes=[cst])
    k.dma("pool", lambda e: e.dma_start(out=biasT.t[:], in_=biasT_in[:, :, :]), writes=[biasT])
    k.op("dve", lambda e: e.tensor_copy(out=ident_bf.t[:], in_=cst.t[:, 0, :]), reads=[cst], writes=[ident_bf])
    k.op("dve", lambda e: e.tensor_copy(out=ones_bf.t[:], in_=cst.t[:, 1, :]), reads=[cst], writes=[ones_bf])
    k.op("dve", lambda e: e.tensor_copy(out=ltri_bf.t[:], in_=cst.t[:, 3, :]), reads=[cst], writes=[ltri_bf])

    def P(name, j=0, w=1):
        o, _ = _off[name]
        return pars.t[:, o + j:o + j + w]

    cnt = {}

    def nxt(lst, key):
        i = cnt.get(key, 0)
        cnt[key] = i + 1
        return lst[i % len(lst)]

    def rms_tile(W, x_rows, xreg, gcolT, dst, dst_c0, a=1.0 / D):
        xt = nxt(W["xt"], "x")
        k.dma("sp", lambda e: e.dma_start(out=xt.t[:], in_=x_rows), reads=[xreg], writes=[xt])
        c0 = nxt(col, "c")
        junk = W["junk"]
        k.op("act", lambda e: e.activation(out=junk.t[:], in_=xt.t[:], func=AF.Square, accum_out=c0.t[:, 0:1]),
             reads=[xt], writes=[junk, c0])
        k.op("act", lambda e: e.activation(out=c0.t[:, 1:2], in_=c0.t[:, 0:1], func=AF.Sqrt, scale=a, bias=EPS),
             reads=[c0], writes=[c0])
        k.op("dve", lambda e: e.reciprocal(out=c0.t[:, 2:3], in_=c0.t[:, 1:2]), reads=[c0], writes=[c0])
        hb = nxt(W["hb"], "hb")
        k.op("act", lambda e: e.activation(out=hb.t[:], in_=xt.t[:], func=AF.Copy, scale=c0.t[:, 2:3]),
             reads=[xt, c0], writes=[hb])
        for kc in range(KC):
            k.op("pe", lambda e, kc=kc: e.transpose(out=pT.t[:, kc, :], in_=hb.t[:, kc * 128:(kc + 1) * 128],
                                                    identity=ident_bf.t[:]),
                 reads=[hb, ident_bf], writes=[pT])
        k.op("dve", lambda e: e.tensor_tensor(out=dst.t[:, :, dst_c0:dst_c0 + 128], in0=pT.t[:],
                                              in1=gcolT.unsqueeze(2).to_broadcast([128, KC, 128]), op=ALU.mult),
             reads=[pT, pars], writes=[dst])
        return xt

    def load_w(W, dram_view, cols, width=512):
        wb = nxt(W["wb"], "w")
        k.dma("pool", lambda e: e.dma_start(out=wb.t[:, :, 0:width], in_=dram_view[:, :, cols:cols + width]),
              writes=[wb])
        return wb

    def fm_norm(psrc, gcol, a, bconst, n, ones_mat):
        sq = nxt(f512, "f")
        k.op("act", lambda e: e.activation(out=sq.t[:, 0:n], in_=psrc.t[:, 0:n], func=AF.Square), reads=[psrc], writes=[sq])
        pss = nxt(pA, "pa")
        k.op("pe", lambda e: e.matmul(pss.t[:, 0:n], lhsT=ones_mat, rhs=sq.t[:, 0:n], start=True, stop=True),
             reads=[sq, cst], writes=[pss])
        rs = nxt(f512, "f")
        k.op("act", lambda e: e.activation(out=rs.t[:, 0:n], in_=pss.t[:, 0:n], func=AF.Sqrt, scale=a, bias=bconst),
             reads=[pss], writes=[rs])
        k.op("dve", lambda e: e.reciprocal(out=rs.t[:, 0:n], in_=rs.t[:, 0:n]), reads=[rs], writes=[rs])
        ob = nxt(b512, "b")
        k.op("dve", lambda e: e.scalar_tensor_tensor(out=ob.t[:, 0:n], in0=psrc.t[:, 0:n], scalar=gcol, in1=rs.t[:, 0:n],
                                                     op0=ALU.mult, op1=ALU.mult),
             reads=[psrc, rs, pars, small], writes=[ob])
        return ob

    def rmsW(st):
        return {"xt": [st.sb("xt", [128, D], F32) for _ in range(2)],
                "hb": [st.sb("hb", [128, D], BF16) for _ in range(2)],
                "junk": st.sb("junk", [128, D], BF16)}

    x_src = x_in
    x_key = "xin"
    for l in range(L):
        last = (l == L - 1)
        lam_init = 0.8 - 0.6 * math.exp(-0.3 * l)
        LS = Stage()
        k.dma("sp", lambda e: e.dma_start(out=pars.t[:], in_=pars_in[l, :, :]), writes=[pars])
        o_dl = _off["dl"][0]
        tmpc = f512[0]
        for i in range(2):
            k.op("dve", lambda e, i=i: e.tensor_tensor(out=tmpc.t[:, 0:DQ], in0=pars.t[:, o_dl + 2 * i * DQ:o_dl + (2 * i + 1) * DQ],
                                                       in1=pars.t[:, o_dl + (2 * i + 1) * DQ:o_dl + (2 * i + 2) * DQ], op=ALU.mult),
                 reads=[pars], writes=[tmpc])
            k.op("dve", lambda e, i=i: e.tensor_reduce(out=small.t[:, 2 + i:3 + i], in_=tmpc.t[:, 0:DQ], axis=AX.X, op=ALU.add),
                 reads=[tmpc], writes=[small])
        k.op("act", lambda e: e.activation(out=small.t[:, 4:6], in_=small.t[:, 2:4], func=AF.Exp), reads=[small], writes=[small])
        k.op("dve", lambda e: e.scalar_tensor_tensor(out=small.t[:, 0:1], in0=small.t[:, 5:6], scalar=-lam_init, in1=small.t[:, 4:5],
                                                     op0=ALU.add, op1=ALU.subtract), reads=[small], writes=[small])
        k.op("dve", lambda e: e.tensor_scalar(out=small.t[:, 1:2], in0=P("gsub"), scalar1=1.0 - lam_init, scalar2=None, op0=ALU.mult),
             reads=[pars], writes=[small])
        neglam = small.t[:, 0:1]
        gsubs = small.t[:, 1:2]

        if "A" in STAGES:
            st_ = Stage()
            W = rmsW(st_)
            W["wb"] = [st_.sb("wb", [128, KC, 512], BF16) for _ in range(3)]
            hT = st_.sb("hT", [128, KC, S], BF16)
            w_in_v = w_in[l].rearrange("(kc p) n -> p kc n", p=128)
            w_order = [(s_, nb_) for s_ in range(NSEQ) for nb_ in (0, 1, 2, 3, 4, 5, 6, 8, 7, 9)]
            w_loaded = {}
            w_pos = [0]

            def get_w(s_, nb_):
                target = min(w_order.index((s_, nb_)) + 1, len(w_order) - 1)
                while w_pos[0] <= target:
                    key_ = w_order[w_pos[0]]
                    w_loaded[key_] = load_w(W, w_in_v, key_[1] * 512)
                    w_pos[0] += 1
                return w_loaded[(s_, nb_)]

            for s in range(NSEQ):
                for tt in range(S // 128):
                    r0 = s * S + tt * 128
                    rms_tile(W, x_src[r0:r0 + 128, :], R(x_key, r0 // 128), P("gmixT", 0, 16), hT, tt * 128)
                for nb in range(4):
                    wb = get_w(s, nb)
                    isq = nb < 2
                    dstT = qTs if isq else kTs
                    gcol = P("gq2") if isq else P("gk2")
                    a, bc = (1.0, DQ * EPS) if isq else (1.0 / DQ, EPS)
                    for j in range(4):
                        head = (nb % 2) * 4 + j
                        for tb in range(S // 512):
                            pp = nxt(pA, "pa")
                            for kc in range(KC):
                                k.op("pe", lambda e, kc=kc: e.matmul(pp.t[:], lhsT=wb.t[:, kc, j * 128:(j + 1) * 128],
                                                                     rhs=hT.t[:, kc, tb * 512:(tb + 1) * 512],
                                                                     start=(kc == 0), stop=(kc == KC - 1)),
                                     reads=[wb, hT], writes=[pp])
                            ob = fm_norm(pp, gcol, a, bc, 512, bd64_f)
                            k.dma("sp", lambda e: e.dma_start(out=dstT[s, head, :, tb * 512:(tb + 1) * 512], in_=ob.t[:]),
                                  reads=[ob], writes=[R("qk", isq, s, head)], accumulate_writes=True)
                for nb in range(4, 6):
                    wb = get_w(s, nb)
                    for tt in range(S // 128):
                        pp = nxt(pA, "pa")
                        for kc in range(KC):
                            k.op("pe", lambda e, kc=kc: e.matmul(pp.t[:], lhsT=hT.t[:, kc, tt * 128:(tt + 1) * 128], rhs=wb.t[:, kc, :],
                                                                 start=(kc == 0), stop=(kc == KC - 1)),
                                 reads=[wb, hT], writes=[pp])
                        ob = nxt(b512, "b")
                        k.op("act", lambda e: e.activation(out=ob.t[:], in_=pp.t[:], func=AF.Copy), reads=[pp], writes=[ob])
                        k.dma("sp", lambda e: e.dma_start(out=Vs[s, tt * 128:(tt + 1) * 128, (nb - 4) * 512:(nb - 3) * 512], in_=ob.t[:]),
                              reads=[ob], writes=[R("v", s)], accumulate_writes=True)
                for i in range(2):
                    wa = get_w(s, 6 + i)
                    wg = get_w(s, 8 + i)
                    for j in range(4):
                        ch = i * 4 + j
                        for tb in range(S // 512):
                            pa_ = nxt(pA, "pa")
                            pg_ = nxt(pA, "pa")
                            for kc in range(KC):
                                k.op("pe", lambda e, kc=kc: e.matmul(pa_.t[:], lhsT=wa.t[:, kc, j * 128:(j + 1) * 128],
                                                                     rhs=hT.t[:, kc, tb * 512:(tb + 1) * 512],
                                                                     start=(kc == 0), stop=(kc == KC - 1)),
                                     reads=[wa, hT], writes=[pa_])
                            for kc in range(KC):
                                k.op("pe", lambda e, kc=kc: e.matmul(pg_.t[:], lhsT=wg.t[:, kc, j * 128:(j + 1) * 128],
                                                                     rhs=hT.t[:, kc, tb * 512:(tb + 1) * 512],
                                                                     start=(kc == 0), stop=(kc == KC - 1)),
                                     reads=[wg, hT], writes=[pg_])
                            sg = nxt(f512, "f")
                            k.op("act", lambda e: e.activation(out=sg.t[:], in_=pg_.t[:], func=AF.Sigmoid), reads=[pg_], writes=[sg])
                            ub = nxt(f512, "f")
                            k.op("dve", lambda e: e.tensor_tensor(out=ub.t[:], in0=pa_.t[:], in1=sg.t[:], op=ALU.mult),
                                 reads=[pa_, sg], writes=[ub])
                            k.dma("sp", lambda e: e.dma_start(out=Us[s, ch, :, tb * 512:(tb + 1) * 512], in_=ub.t[:]),
                                  reads=[ub], writes=[R("u", s, ch)], accumulate_writes=True)
            st_.close()

        if "qT" in dbg and l == 0:
            k.dma("sp", lambda e: e.dma_start(out=dbg["qT"][:, :, :, :], in_=qTs[:, :, :, :]), writes=[R("dbg", "qT")])
            k.dma("sp", lambda e: e.dma_start(out=dbg["kT"][:, :, :, :], in_=kTs[:, :, :, :]), writes=[R("dbg", "kT")])
            k.dma("sp", lambda e: e.dma_start(out=dbg["V"][:, :, :], in_=Vs[:, :, :]), writes=[R("dbg", "V")])
            k.dma("sp", lambda e: e.dma_start(out=dbg["U"][:, :, :, :], in_=Us[:, :, :, :]), writes=[R("dbg", "U")])
            k.barrier()

        if "B" in STAGES:
            st_ = Stage()
            qh2 = [st_.sb("qh", [128, S], BF16) for i in range(2)]
            kh2 = [st_.sb("kh", [128, S], BF16) for i in range(2)]
            vh2 = [st_.sb("vh", [128, S // 128, 128], BF16) for i in range(2)]
            attn_r = [st_.sb("attn_r", [128, 512], F32) for i in range(2)]
            attn_t = [st_.sb("attn_t", [128, 512], F32) for i in range(2)]
            it = 0
            for s in range(NSEQ):
                for h in range(NH):
                    qh, kh, vh = qh2[it % 2], kh2[it % 2], vh2[it % 2]
                    it += 1
                    k.dma("sp", lambda e: e.dma_start(out=qh.t[:], in_=qTs[s, h, :, :]), reads=[R("qk", True, s, h)], writes=[qh])
                    k.dma("sp", lambda e: e.dma_start(out=kh.t[:], in_=kTs[s, h, :, :]), reads=[R("qk", False, s, h)], writes=[kh])
                    k.dma("sp", lambda e: e.dma_start(out=vh.t[:], in_=Vs[s, :, h * 128:(h + 1) * 128].rearrange("(t p) d -> p t d", p=128)),
                          reads=[R("v", s)], writes=[vh])
                    for qc in range(S // 512):
                        tms = []
                        for m in range(2):
                            pO = pA[2 * m]
                            pL = pA[2 * m + 1]
                            nk = (qc + 1) * 4

                            def emit_qk(kj, m=m, qc=qc):
                                q_lo = max(qc * 512, kj * 128)
                                c0 = q_lo - qc * 512
                                sT = nxt(pA[4:6], "pa_s")
                                blist = [dlt for dlt in (0, 1) if qc * 4 <= kj + dlt < (qc + 1) * 4]
                                k.op("pe", lambda e: e.matmul(sT.t[:, c0:512], lhsT=kh.t[m * 64:(m + 1) * 64, kj * 128:(kj + 1) * 128],
                                                              rhs=qh.t[m * 64:(m + 1) * 64, q_lo:(qc + 1) * 512],
                                                              start=True, stop=(len(blist) == 0)),
                                     reads=[kh, qh], writes=[sT])
                                for bi, dlt in enumerate(blist):
                                    cc = (kj + dlt) * 128 - qc * 512
                                    k.op("pe", lambda e, cc=cc, dlt=dlt, bi=bi: e.matmul(
                                        sT.t[:, cc:cc + 128], lhsT=ident_bf.t[:], rhs=biasT.t[:, h * 2 + dlt, :],
                                        start=False, stop=(bi == len(blist) - 1)),
                                         reads=[ident_bf, biasT], writes=[sT])
                                return sT, c0

                            nxt_qk = emit_qk(0)
                            for kj in range(nk):
                                sT, c0 = nxt_qk
                                if kj + 1 < nk:
                                    nxt_qk = emit_qk(kj + 1)
                                near_end = min((kj + 2) * 128, (qc + 1) * 512) - qc * 512
                                pt = nxt(b512, "b")
                                if near_end > c0:
                                    k.op("act", lambda e: e.activation(out=pt.t[:, c0:near_end], in_=sT.t[:, c0:near_end], func=AF.Exp),
                                         reads=[sT], writes=[pt])
                                if near_end < 512:
                                    lo = max(near_end, c0)
                                    k.op("act", lambda e: e.activation(out=pt.t[:, lo:512], in_=sT.t[:, lo:512], func=AF.Exp,
                                                                       bias=P("relc", h)),
                                         reads=[sT, pars], writes=[pt])
                                k.op("pe", lambda e: e.matmul(pO.t[:, c0:512], lhsT=vh.t[:, kj, :], rhs=pt.t[:, c0:512],
                                                              start=(kj == 0), stop=(kj == nk - 1)),
                                     reads=[vh, pt], writes=[pO])
                                k.op("pe", lambda e: e.matmul(pL.t[:, c0:512], lhsT=ones_bf.t[:], rhs=pt.t[:, c0:512],
                                                              start=(kj == 0), stop=(kj == nk - 1)),
                                     reads=[ones_bf, pt], writes=[pL])
                            r_ = attn_r[m]
                            k.op("dve", lambda e: e.reciprocal(out=r_.t[:], in_=pL.t[:]), reads=[pL], writes=[r_])
                            t_ = attn_t[m]
                            k.op("dve", lambda e: e.tensor_tensor(out=t_.t[:], in0=pO.t[:], in1=r_.t[:], op=ALU.mult),
                                 reads=[pO, r_], writes=[t_])
                            tms.append(t_)
                        a_ = nxt(f512, "f")
                        k.op("dve", lambda e: e.scalar_tensor_tensor(out=a_.t[:], in0=tms[1].t[:], scalar=neglam, in1=tms[0].t[:],
                                                                     op0=ALU.mult, op1=ALU.add),
                             reads=[tms[0], tms[1], small], writes=[a_])
                        ob = fm_norm(a_, gsubs, 1.0 / 128, EPS, 512, ones_f)
                        c_lo = s * S + qc * 512
                        k.dma("sp", lambda e: e.dma_start(out=mixT[h, :, c_lo:c_lo + 512], in_=ob.t[:]),
                              reads=[ob], writes=[R("mix", c_lo // 512)], accumulate_writes=True)
            st_.close()

        if "C" in STAGES:
            st_ = Stage()
            Ubuf = st_.sb("Ubuf", [128, 32 + S], F32)
            Yc = [st_.sb("Yc", [128, S], F32) for j in range(8)]
            mean = st_.sb("mean", [128, 512], F32)
            msq = st_.sb("msq", [128, 512], F32)
            var = st_.sb("var", [128, 512], F32)
            k.op("dve", lambda e: e.memset(Ubuf.t[:, 0:32], 0.0), writes=[Ubuf])
            o_cw = _off["convw"][0]
            for s in range(NSEQ):
                for j in range(8):
                    k.dma("sp", lambda e: e.dma_start(out=Ubuf.t[:, 32:32 + S], in_=Us[s, j, :, :]), reads=[R("u", s, j)], writes=[Ubuf])
                    k.op("dve", lambda e: e.tensor_scalar(out=Yc[j].t[:], in0=Ubuf.t[:, 2:2 + S], scalar1=pars.t[:, o_cw + j * CW:o_cw + j * CW + 1],
                                                          scalar2=P("convb", j), op0=ALU.mult, op1=ALU.add),
                         reads=[Ubuf, pars], writes=[Yc[j]])
                    for tap in range(1, CW):
                        k.op("dve", lambda e, tap=tap: e.scalar_tensor_tensor(
                            out=Yc[j].t[:], in0=Ubuf.t[:, 2 + tap:2 + tap + S], scalar=pars.t[:, o_cw + j * CW + tap:o_cw + j * CW + tap + 1],
                            in1=Yc[j].t[:], op0=ALU.mult, op1=ALU.add),
                             reads=[Ubuf, pars], writes=[Yc[j]])
                for tb in range(S // 512):
                    cs = slice(tb * 512, (tb + 1) * 512)
                    pM = nxt(pA, "pa")
                    pQ = nxt(pA, "pa")
                    for j in range(8):
                        k.op("pe", lambda e, j=j: e.matmul(pM.t[:], lhsT=ones_f, rhs=Yc[j].t[:, cs], start=(j == 0), stop=(j == 7)),
                             reads=[cst, Yc[j]], writes=[pM])
                    for j in range(8):
                        sq = nxt(f512, "f")
                        k.op("act", lambda e, j=j: e.activation(out=sq.t[:], in_=Yc[j].t[:, cs], func=AF.Square), reads=[Yc[j]], writes=[sq])
                        k.op("pe", lambda e, j=j: e.matmul(pQ.t[:], lhsT=ones_f, rhs=sq.t[:], start=(j == 0), stop=(j == 7)),
                             reads=[cst, sq], writes=[pQ])
                    k.op("act", lambda e: e.activation(out=mean.t[:], in_=pM.t[:], func=AF.Copy, scale=1.0 / 1024), reads=[pM], writes=[mean])
                    k.op("act", lambda e: e.activation(out=msq.t[:], in_=mean.t[:], func=AF.Square), reads=[mean], writes=[msq])
                    k.op("dve", lambda e: e.scalar_tensor_tensor(out=var.t[:], in0=pQ.t[:], scalar=1.0 / 1024, in1=msq.t[:],
                                                                 op0=ALU.mult, op1=ALU.subtract), reads=[pQ, msq], writes=[var])
                    k.op("act", lambda e: e.activation(out=var.t[:], in_=var.t[:], func=AF.Sqrt, scale=1.0, bias=EPS), reads=[var], writes=[var])
                    k.op("dve", lambda e: e.reciprocal(out=var.t[:], in_=var.t[:]), reads=[var], writes=[var])
                    for j in range(8):
                        t_ = nxt(f512, "f")
                        k.op("dve", lambda e, j=j: e.tensor_tensor(out=t_.t[:], in0=Yc[j].t[:, cs], in1=mean.t[:], op=ALU.subtract),
                             reads=[Yc[j], mean], writes=[t_])
                        k.op("dve", lambda e: e.tensor_tensor(out=t_.t[:], in0=t_.t[:], in1=var.t[:], op=ALU.mult),
                             reads=[var], writes=[t_])
                        ob = nxt(b512, "b")
                        k.op("act", lambda e, j=j: e.activation(out=ob.t[:], in_=t_.t[:], func=AF.Silu, scale=P("lng", j), bias=P("lnb", j)),
                             reads=[t_, pars], writes=[ob])
                        c_lo = s * S + tb * 512
                        k.dma("sp", lambda e, j=j: e.dma_start(out=mixT[8 + j, :, c_lo:c_lo + 512], in_=ob.t[:]),
                              reads=[ob], writes=[R("mix", c_lo // 512)], accumulate_writes=True)
            st_.close()

        if "mixT" in dbg and l == 0:
            k.dma("sp", lambda e: e.dma_start(out=dbg["mixT"][:, :, :], in_=mixT[:, :, :]), writes=[R("dbg", "mixT")])
            k.barrier()

        if "D" in STAGES:
            st_ = Stage()
            wo_v = w_out[l].rearrange("(kc p) n -> p kc n", p=128)
            wfull = st_.sb("wfull", [128, KC, D], BF16)
            mb2 = [st_.sb("mb", [128, KC, 512], BF16) for _ in range(2)]
            xt2 = [st_.sb("xt", [128, D], F32) for _ in range(2)]
            xo2 = [st_.sb("xo", [128, D], F32) for _ in range(2)]
            for nb in range(4):
                k.dma("pool", lambda e, nb=nb: e.dma_start(out=wfull.t[:, :, nb * 512:(nb + 1) * 512], in_=wo_v[:, :, nb * 512:(nb + 1) * 512]),
                      writes=[wfull], accumulate_writes=True)
            mix_v = mixT.rearrange("c p t -> p c t")
            for tb in range(T // 512):
                mb = nxt(mb2, "mb")
                k.dma("sp", lambda e: e.dma_start(out=mb.t[:], in_=mix_v[:, :, tb * 512:(tb + 1) * 512]), reads=[R("mix", tb)], writes=[mb])
                for tt in range(4):
                    r0 = tb * 512 + tt * 128
                    xt = nxt(xt2, "x")
                    k.dma("sp", lambda e: e.dma_start(out=xt.t[:], in_=x_src[r0:r0 + 128, :]), reads=[R(x_key, r0 // 128)], writes=[xt])
                    xo = nxt(xo2, "xo")
                    for nb in range(4):
                        pp = nxt(pA, "pa")
                        for c in range(KC):
                            k.op("pe", lambda e, c=c: e.matmul(pp.t[:], lhsT=mb.t[:, c, tt * 128:(tt + 1) * 128],
                                                               rhs=wfull.t[:, c, nb * 512:(nb + 1) * 512],
                                                               start=(c == 0), stop=(c == KC - 1)),
                                 reads=[mb, wfull], writes=[pp])
                        k.op("dve", lambda e, nb=nb: e.tensor_tensor(out=xo.t[:, nb * 512:(nb + 1) * 512], in0=pp.t[:],
                                                                     in1=xt.t[:, nb * 512:(nb + 1) * 512], op=ALU.add),
                             reads=[pp, xt], writes=[xo])
                    k.dma("sp", lambda e: e.dma_start(out=xres[r0:r0 + 128, :], in_=xo.t[:]), reads=[xo], writes=[R("xres", r0 // 128)])
            st_.close()
            x_src = xres
            x_key = "xres"

        if "x1" in dbg and l == 0:
            k.dma("sp", lambda e: e.dma_start(out=dbg["x1"][:, :], in_=xres[:, :]), writes=[R("dbg", "x1")])
            k.barrier()

        if "E" in STAGES:
            st_ = Stage()
            W = rmsW(st_)
            W["wb"] = [st_.sb("wb", [128, KC, 512], BF16) for _ in range(2)]
            xo2 = [st_.sb("xo", [128, D], F32) for _ in range(2)]
            memT = st_.sb("memT", [128, KC, MEM], BF16)
            kcT = st_.sb("kcT", [128, NHC, MEM], BF16)
            vcs = st_.sb("vcs", [128, 2, 512], BF16)
            ocT = st_.sb("ocT", [128, NHC, 512], BF16)
            hTb = st_.sb("hTb", [128, KC, 512], BF16)
            wq_sb = st_.sb("wq_sb", [128, KC, 512], BF16)
            wo_sb = st_.sb("wo_sb", [128, NHC, D], BF16)
            wq_v = wq_c[l].rearrange("(kc p) n -> p kc n", p=128)
            wkv_v = wkv_c[l].rearrange("(kc p) n -> p kc n", p=128)
            woc_v = wo_c[l].rearrange("(kc p) n -> p kc n", p=128)
            k.dma("pool", lambda e: e.dma_start(out=wq_sb.t[:], in_=wq_v[:, :, :]), writes=[wq_sb])
            k.dma("pool", lambda e: e.dma_start(out=wo_sb.t[:], in_=woc_v[:, :, :]), writes=[wo_sb])
            for s in range(NSEQ):
                for mt in range(2):
                    r0 = s * MEM + mt * 128
                    rms_tile(W, mem_in[r0:r0 + 128, :], R("memin"), P("gmemT", 0, 16), memT, mt * 128)
                wk_ = load_w(W, wkv_v, 0)
                for h in range(NHC):
                    pp = nxt(pA, "pa")
                    for kc in range(KC):
                        k.op("pe", lambda e, kc=kc: e.matmul(pp.t[:, 0:MEM], lhsT=wk_.t[:, kc, h * 128:(h + 1) * 128], rhs=memT.t[:, kc, :],
                                                             start=(kc == 0), stop=(kc == KC - 1)), reads=[wk_, memT], writes=[pp])
                    ob = fm_norm(pp, P("gkc"), 1.0 / 128, EPS, MEM, ones_f)
                    k.op("dve", lambda e: e.tensor_copy(out=kcT.t[:, h, :], in_=ob.t[:, 0:MEM]), reads=[ob], writes=[kcT])
                wv_ = load_w(W, wkv_v, 512)
                for mt in range(2):
                    pp = nxt(pA, "pa")
                    for kc in range(KC):
                        k.op("pe", lambda e, kc=kc: e.matmul(pp.t[:], lhsT=memT.t[:, kc, mt * 128:(mt + 1) * 128], rhs=wv_.t[:, kc, :],
                                                             start=(kc == 0), stop=(kc == KC - 1)), reads=[wv_, memT], writes=[pp])
                    k.op("act", lambda e: e.activation(out=vcs.t[:, mt, :], in_=pp.t[:], func=AF.Copy), reads=[pp], writes=[vcs])
                for tb in range(S // 512):
                    for tt in range(4):
                        r0 = s * S + tb * 512 + tt * 128
                        rms_tile(W, xres[r0:r0 + 128, :], R("xres", r0 // 128), P("gcrossT", 0, 16), hTb, tt * 128)
                    for h in range(NHC):
                        pp = nxt(pA, "pa")
                        for kc in range(KC):
                            k.op("pe", lambda e, kc=kc: e.matmul(pp.t[:], lhsT=wq_sb.t[:, kc, h * 128:(h + 1) * 128], rhs=hTb.t[:, kc, :],
                                                                 start=(kc == 0), stop=(kc == KC - 1)), reads=[wq_sb, hTb], writes=[pp])
                        qn = fm_norm(pp, P("gqc"), 1.0, 128 * EPS, 512, ones_f)
                        pO = nxt(pA, "pa")
                        pL = nxt(pA, "pa")
                        for mt in range(2):
                            sT = nxt(pA, "pa")
                            k.op("pe", lambda e: e.matmul(sT.t[:], lhsT=kcT.t[:, h, mt * 128:(mt + 1) * 128], rhs=qn.t[:], start=True, stop=True),
                                 reads=[kcT, qn], writes=[sT])
                            pt = nxt(b512, "b")
                            k.op("act", lambda e: e.activation(out=pt.t[:], in_=sT.t[:], func=AF.Exp), reads=[sT], writes=[pt])
                            k.op("pe", lambda e: e.matmul(pO.t[:], lhsT=vcs.t[:, mt, h * 128:(h + 1) * 128], rhs=pt.t[:], start=(mt == 0), stop=(mt == 1)),
                                 reads=[vcs, pt], writes=[pO])
                            k.op("pe", lambda e: e.matmul(pL.t[:], lhsT=ones_bf.t[:], rhs=pt.t[:], start=(mt == 0), stop=(mt == 1)),
                                 reads=[ones_bf, pt], writes=[pL])
                        r_ = nxt(f512, "f")
                        k.op("dve", lambda e: e.reciprocal(out=r_.t[:], in_=pL.t[:]), reads=[pL], writes=[r_])
                        k.op("dve", lambda e: e.tensor_tensor(out=ocT.t[:, h, :], in0=pO.t[:], in1=r_.t[:], op=ALU.mult),
                             reads=[pO, r_], writes=[ocT])
                    for tt in range(4):
                        r0 = s * S + tb * 512 + tt * 128
                        xt = nxt(W["xt"], "x")
                        k.dma("sp", lambda e: e.dma_start(out=xt.t[:], in_=xres[r0:r0 + 128, :]), reads=[R("xres", r0 // 128)], writes=[xt])
                        xo = nxt(xo2, "xo")
                        for nb in range(4):
                            pp = nxt(pA, "pa")
                            for h in range(NHC):
                                k.op("pe", lambda e, h=h: e.matmul(pp.t[:], lhsT=ocT.t[:, h, tt * 128:(tt + 1) * 128],
                                                                   rhs=wo_sb.t[:, h, nb * 512:(nb + 1) * 512],
                                                                   start=(h == 0), stop=(h == NHC - 1)), reads=[ocT, wo_sb], writes=[pp])
                            k.op("dve", lambda e, nb=nb: e.tensor_tensor(out=xo.t[:, nb * 512:(nb + 1) * 512], in0=pp.t[:],
                                                                         in1=xt.t[:, nb * 512:(nb + 1) * 512], op=ALU.add),
                                 reads=[pp, xt], writes=[xo])
                        k.dma("sp", lambda e: e.dma_start(out=xres[r0:r0 + 128, :], in_=xo.t[:]), reads=[xo], writes=[R("xres", r0 // 128)])
            st_.close()

        if "x2" in dbg and l == 0:
            k.dma("sp", lambda e: e.dma_start(out=dbg["x2"][:, :], in_=xres[:, :]), writes=[R("dbg", "x2")])
            k.barrier()

        if "F" in STAGES:
            gates = LS.sb("gates", [128, NT, 2], F32)
            desti = LS.sb("desti", [128, NT * 2], I32)
            widx = LS.sb("widx", [128, NB], I32)
            st_ = Stage()
            xt2 = [st_.sb("xt", [128, D], F32) for _ in range(2)]
            hb2 = [st_.sb("hb", [128, D], BF16) for _ in range(2)]
            junk = st_.sb("junk", [128, D], BF16)
            gffn = st_.sb("gffn", [128, D], F32)
            wr = st_.sb("wr", [128, KC, 72], BF16)
            hTt = st_.sb("hTt", [128, KC, 128], BF16)
            OH = st_.sb("OH", [128, NT, 2, NE], F32)
            RANK = st_.sb("RANK", [128, NT, NE], F32)
            cum = st_.sb("cum", [128, NE], F32)
            rt = [st_.sb("rt", [128, 128], F32) for i in range(6)]
            Mb = st_.sb("Mb", [128, NE], BF16)
            destf = st_.sb("destf", [128, NT * 2], F32)
            pcs = [st_.sb("pcs", [128, NE], F32) for i in range(3)]
            pci = st_.sb("pci", [128, NE], I32)
            thr = st_.sb("thr_sb", [128, NB], F32)
            cmpb = st_.sb("cmpb", [128, NB, NE], F32)
            bef = st_.sb("bef", [128, NB], F32)
            k.dma("sp", lambda e: e.dma_start(out=thr.t[:], in_=thr_in[:, :]), writes=[thr])
            k.dma("sp", lambda e: e.dma_start(out=gffn.t[:], in_=gffn_in[l, :, :]), writes=[gffn])
            k.dma("pool", lambda e: e.dma_start(out=wr.t[:], in_=w_rt[l].rearrange("(kc p) n -> p kc n", p=128)), writes=[wr])
            k.op("dve", lambda e: e.memset(cum.t[:], 0.0), writes=[cum])
            o_brt = _off["brt"][0]
            for tt in range(NT):
                r0 = tt * 128
                xt = nxt(xt2, "x")
                k.dma("sp", lambda e: e.dma_start(out=xt.t[:], in_=xres[r0:r0 + 128, :]), reads=[R("xres", tt)], writes=[xt])
                c0 = nxt(col, "c")
                k.op("act", lambda e: e.activation(out=junk.t[:], in_=xt.t[:], func=AF.Square, accum_out=c0.t[:, 0:1]),
                     reads=[xt], writes=[junk, c0])
                k.op("act", lambda e: e.activation(out=c0.t[:, 1:2], in_=c0.t[:, 0:1], func=AF.Sqrt, scale=1.0 / D, bias=EPS), reads=[c0], writes=[c0])
                k.op("dve", lambda e: e.reciprocal(out=c0.t[:, 2:3], in_=c0.t[:, 1:2]), reads=[c0], writes=[c0])
                hb = nxt(hb2, "hb")
                k.op("dve", lambda e: e.scalar_tensor_tensor(out=hb.t[:], in0=xt.t[:], scalar=c0.t[:, 2:3], in1=gffn.t[:],
                                                             op0=ALU.mult, op1=ALU.mult), reads=[xt, c0, gffn], writes=[hb])
                k.dma("sp", lambda e: e.dma_start(out=Hrows[r0:r0 + 128, :], in_=hb.t[:]), reads=[hb], writes=[R("hrows", tt)])
                for kc in range(KC):
                    k.op("pe", lambda e, kc=kc: e.transpose(out=pT.t[:, kc, :], in_=hb.t[:, kc * 128:(kc + 1) * 128], identity=ident_bf.t[:]),
                         reads=[hb, ident_bf], writes=[pT])
                k.op("act", lambda e: e.activation(out=hTt.t[:], in_=pT.t[:], func=AF.Copy), reads=[pT], writes=[hTt])
                pp = nxt(pA, "pa")
                for kc in range(KC):
                    k.op("pe", lambda e, kc=kc: e.matmul(pp.t[:, 0:72], lhsT=hTt.t[:, kc, :], rhs=wr.t[:, kc, :], start=(kc == 0), stop=(kc == KC - 1)),
                         reads=[hTt, wr], writes=[pp])
                lg, w1_, w2_, w3_, w4_, w5_ = rt
                k.op("dve", lambda e: e.tensor_tensor(out=lg.t[:, 0:72], in0=pp.t[:, 0:72], in1=pars.t[:, o_brt:o_brt + 72], op=ALU.add),
                     reads=[pp, pars], writes=[lg])
                k.op("dve", lambda e: e.tensor_reduce(out=w1_.t[:, 0:1], in_=lg.t[:, 0:8], axis=AX.X, op=ALU.max), reads=[lg], writes=[w1_])
                k.op("dve", lambda e: e.tensor_scalar(out=w2_.t[:, 0:8], in0=lg.t[:, 0:8], scalar1=w1_.t[:, 0:1], scalar2=None, op0=ALU.is_equal),
                     reads=[lg, w1_], writes=[w2_])
                k.op("dve", lambda e: e.tensor_scalar(out=w1_.t[:, 1:2], in0=w1_.t[:, 0:1], scalar1=-1.0, scalar2=None, op0=ALU.mult),
                     reads=[w1_], writes=[w1_])
                k.op("act", lambda e: e.activation(out=w2_.t[:, 8:16], in_=lg.t[:, 0:8], func=AF.Exp, bias=w1_.t[:, 1:2], accum_out=w1_.t[:, 2:3]),
                     reads=[lg, w1_], writes=[w2_, w1_])
                k.op("dve", lambda e: e.reciprocal(out=w1_.t[:, 3:4], in_=w1_.t[:, 2:3]), reads=[w1_], writes=[w1_])
                k.op("dve", lambda e: e.tensor_tensor(out=w3_.t[:, 0:64].rearrange("p (g j) -> p g j", j=8),
                                                      in0=lg.t[:, 8:72].rearrange("p (g j) -> p g j", j=8),
                                                      in1=w2_.t[:, 0:8].unsqueeze(2).to_broadcast([128, 8, 8]), op=ALU.mult),
                     reads=[lg, w2_], writes=[w3_])
                k.op("dve", lambda e: e.tensor_reduce(out=w4_.t[:, 0:8], in_=w3_.t[:, 0:64].rearrange("p (g j) -> p j g", j=8), axis=AX.X, op=ALU.add),
                     reads=[w3_], writes=[w4_])
                k.op("dve", lambda e: e.tensor_reduce(out=w1_.t[:, 4:5], in_=w4_.t[:, 0:8], axis=AX.X, op=ALU.max), reads=[w4_], writes=[w1_])
                k.op("dve", lambda e: e.tensor_scalar(out=w4_.t[:, 8:16], in0=w4_.t[:, 0:8], scalar1=w1_.t[:, 4:5], scalar2=None, op0=ALU.is_equal),
                     reads=[w4_, w1_], writes=[w4_])
                k.op("dve", lambda e: e.scalar_tensor_tensor(out=w4_.t[:, 16:24], in0=w4_.t[:, 8:16], scalar=-1e30, in1=w4_.t[:, 0:8],
                                                             op0=ALU.mult, op1=ALU.add), reads=[w4_], writes=[w4_])
                k.op("dve", lambda e: e.tensor_reduce(out=w1_.t[:, 5:6], in_=w4_.t[:, 16:24], axis=AX.X, op=ALU.max), reads=[w4_], writes=[w1_])
                k.op("dve", lambda e: e.tensor_scalar(out=w4_.t[:, 24:32], in0=w4_.t[:, 16:24], scalar1=w1_.t[:, 5:6], scalar2=None, op0=ALU.is_equal),
                     reads=[w4_, w1_], writes=[w4_])
                k.op("dve", lambda e: e.tensor_tensor(out=w1_.t[:, 6:7], in0=w1_.t[:, 5:6], in1=w1_.t[:, 4:5], op=ALU.subtract), reads=[w1_], writes=[w1_])
                k.op("act", lambda e: e.activation(out=w1_.t[:, 7:8], in_=w1_.t[:, 6:7], func=AF.Exp), reads=[w1_], writes=[w1_])
                k.op("dve", lambda e: e.tensor_scalar(out=w1_.t[:, 8:9], in0=w1_.t[:, 7:8], scalar1=1.0, scalar2=None, op0=ALU.add), reads=[w1_], writes=[w1_])
                k.op("dve", lambda e: e.reciprocal(out=w1_.t[:, 9:10], in_=w1_.t[:, 8:9]), reads=[w1_], writes=[w1_])
                k.op("dve", lambda e: e.tensor_tensor(out=gates.t[:, tt, 0:1], in0=w1_.t[:, 3:4], in1=w1_.t[:, 9:10], op=ALU.mult), reads=[w1_], writes=[gates])
                k.op("dve", lambda e: e.tensor_tensor(out=gates.t[:, tt, 1:2], in0=gates.t[:, tt, 0:1], in1=w1_.t[:, 7:8], op=ALU.mult), reads=[w1_], writes=[gates])
                for kk in range(2):
                    k.op("dve", lambda e, kk=kk: e.tensor_tensor(
                        out=OH.t[:, tt, kk, :].rearrange("p (g j) -> p g j", j=8),
                        in0=w2_.t[:, 0:8].unsqueeze(2).to_broadcast([128, 8, 8]),
                        in1=w4_.t[:, 8 + 16 * kk:16 + 16 * kk].unsqueeze(1).to_broadcast([128, 8, 8]), op=ALU.mult),
                         reads=[w2_, w4_], writes=[OH])
                k.op("dve", lambda e: e.tensor_tensor(out=Mb.t[:], in0=OH.t[:, tt, 0, :], in1=OH.t[:, tt, 1, :], op=ALU.add), reads=[OH], writes=[Mb])
                pr = nxt(pA, "pa")
                k.op("pe", lambda e: e.matmul(pr.t[:, 0:NE], lhsT=ltri_bf.t[:], rhs=Mb.t[:], start=True, stop=True), reads=[ltri_bf, Mb], writes=[pr])
                k.op("dve", lambda e: e.tensor_tensor(out=RANK.t[:, tt, :], in0=pr.t[:, 0:NE], in1=cum.t[:], op=ALU.add), reads=[pr, cum], writes=[RANK])
                pc_ = nxt(pA, "pa")
                k.op("pe", lambda e: e.matmul(pc_.t[:, 0:NE], lhsT=ones_bf.t[:], rhs=Mb.t[:], start=True, stop=True), reads=[ones_bf, Mb], writes=[pc_])
                k.op("dve", lambda e: e.tensor_tensor(out=cum.t[:], in0=cum.t[:], in1=pc_.t[:, 0:NE], op=ALU.add), reads=[pc_], writes=[cum])
            k.op("dve", lambda e: e.tensor_scalar(out=pcs[0].t[:], in0=cum.t[:], scalar1=float(BLK - 1), scalar2=None, op0=ALU.add), reads=[cum], writes=[pcs[0]])
            k.op("dve", lambda e: e.tensor_copy(out=pci.t[:], in_=pcs[0].t[:]), reads=[pcs[0]], writes=[pci])
            sh = int(math.log2(BLK))
            k.op("dve", lambda e: e.tensor_single_scalar(out=pci.t[:], in_=pci.t[:], scalar=sh, op=ALU.arith_shift_right), writes=[pci])
            k.op("dve", lambda e: e.tensor_single_scalar(out=pci.t[:], in_=pci.t[:], scalar=sh, op=ALU.logical_shift_left), writes=[pci])
            k.op("dve", lambda e: e.tensor_copy(out=pcs[0].t[:], in_=pci.t[:]), reads=[pci], writes=[pcs[0]])
            k.op("dve", lambda e: e.tensor_copy(out=pcs[1].t[:], in_=pcs[0].t[:]), reads=[pcs[0]], writes=[pcs[1]])
            cur, oth = pcs[1], pcs[2]
            stp = 1
            while stp < NE:
                k.op("dve", lambda e, stp=stp, cur=cur, oth=oth: e.tensor_copy(out=oth.t[:, 0:stp], in_=cur.t[:, 0:stp]), reads=[cur], writes=[oth])
                k.op("dve", lambda e, stp=stp, cur=cur, oth=oth: e.tensor_tensor(out=oth.t[:, stp:NE], in0=cur.t[:, stp:NE], in1=cur.t[:, 0:NE - stp], op=ALU.add),
                     reads=[cur], writes=[oth])
                cur, oth = oth, cur
                stp *= 2
            pend = cur
            pstart = oth
            k.op("dve", lambda e: e.tensor_tensor(out=pstart.t[:], in0=pend.t[:], in1=pcs[0].t[:], op=ALU.subtract), reads=[pend, pcs[0]], writes=[pstart])
            for tt in range(NT):
                k.op("dve", lambda e, tt=tt: e.tensor_tensor(out=RANK.t[:, tt, :], in0=RANK.t[:, tt, :], in1=pstart.t[:], op=ALU.add), reads=[pstart], writes=[RANK])
                for kk in range(2):
                    k.op("dve", lambda e, tt=tt, kk=kk: e.tensor_tensor(out=rt[2].t[:, 0:NE], in0=RANK.t[:, tt, :], in1=OH.t[:, tt, kk, :], op=ALU.mult),
                         reads=[RANK, OH], writes=[rt[2]])
                    k.op("dve", lambda e, tt=tt, kk=kk: e.tensor_reduce(out=destf.t[:, tt * 2 + kk:tt * 2 + kk + 1], in_=rt[2].t[:, 0:NE], axis=AX.X, op=ALU.add),
                         reads=[rt[2]], writes=[destf])
            k.op("dve", lambda e: e.tensor_copy(out=desti.t[:], in_=destf.t[:]), reads=[destf], writes=[desti])
            k.op("dve", lambda e: e.tensor_tensor(out=cmpb.t[:], in0=pend.t[:].unsqueeze(1).to_broadcast([128, NB, NE]),
                                                  in1=thr.t[:].unsqueeze(2).to_broadcast([128, NB, NE]), op=ALU.is_le), reads=[pend, thr], writes=[cmpb])
            k.op("dve", lambda e: e.tensor_reduce(out=bef.t[:], in_=cmpb.t[:], axis=AX.X, op=ALU.add), reads=[cmpb], writes=[bef])
            k.op("dve", lambda e: e.tensor_scalar(out=bef.t[:], in0=bef.t[:], scalar1=float(NE - 1), scalar2=128.0, op0=ALU.min, op1=ALU.mult), writes=[bef])
            k.op("dve", lambda e: e.tensor_scalar(out=bef.t[:], in0=bef.t[:], scalar1=iota_p, scalar2=None, op0=ALU.add), reads=[cst], writes=[bef])
            k.op("dve", lambda e: e.tensor_copy(out=widx.t[:], in_=bef.t[:]), reads=[bef], writes=[widx])

            k.op("dve", lambda e: e.memset(junk.t[:], 0.0), writes=[junk])
            RPP = PR // 128
            zc = 24
            xs_v = Xs.rearrange("(p r) d -> p r d", p=128)
            for z0 in range(0, RPP, zc):
                zn = min(zc, RPP - z0)
                k.dma("sp", lambda e, z0=z0, zn=zn: e.dma_start(out=xs_v[:, z0:z0 + zn, :],
                                                                 in_=junk.t[:].unsqueeze(1).to_broadcast([128, zn, D])),
                      reads=[junk], writes=[R("xs")], accumulate_writes=True)
            k.barrier()
            for tt in range(NT):
                hb = nxt(hb2, "hb")
                k.dma("sp", lambda e: e.dma_start(out=hb.t[:], in_=Hrows[tt * 128:(tt + 1) * 128, :]), reads=[R("hrows", tt)], writes=[hb])
                for kk in range(2):
                    k.dma("pool", lambda e, kk=kk: e.indirect_dma_start(
                        out=Xs[:, :], out_offset=bass.IndirectOffsetOnAxis(ap=desti.t[:, tt * 2 + kk:tt * 2 + kk + 1], axis=0),
                        in_=hb.t[:], in_offset=None), reads=[hb, desti], writes=[R("xs")], accumulate_writes=True)

            if "moe" in dbg and l == 0:
                k.dma("sp", lambda e: e.dma_start(out=dbg["moe"][:, 0:NT * 2], in_=destf.t[:]), reads=[destf], writes=[R("dbg", "moe")])
                k.dma("sp", lambda e: e.dma_start(out=dbg["moe"][:, NT * 2:NT * 2 + NB], in_=bef.t[:]), reads=[bef], writes=[R("dbg", "moe2")])
                k.dma("sp", lambda e: e.dma_start(out=dbg["moe"][:, NT * 2 + NB:NT * 4 + NB], in_=gates.t[:].rearrange("p t k -> p (t k)")),
                      reads=[gates], writes=[R("dbg", "moe3")])
            st_.close()

            st_ = Stage()
            yo2 = [st_.sb("yo", [128, D], F32) for _ in range(2)]
            xsb = [st_.sb("xsb", [128, 2, D], BF16) for i in range(2)]
            xTb = st_.sb("xTb", [128, KC, BLK], BF16)
            hid = st_.sb("hid", [128, FC, BLK], BF16)
            w1b = [st_.sb("w1b", [128, KC, DE], BF16) for i in range(2)]
            w3b = [st_.sb("w3b", [128, KC, DE], BF16) for i in range(2)]
            w2b = [st_.sb("w2b", [128, FC, D], BF16) for i in range(2)]

            def load_block(b):
                i = b % 2
                k.dma("sp", lambda e: e.dma_start(out=xsb[i].t[:], in_=Xs[b * BLK:(b + 1) * BLK, :].rearrange("(r p) d -> p r d", p=128)),
                      reads=[R("xs")], writes=[xsb[i]])
                for wsb, wdr, nch, g in ((w1b[i], w1r, NCH, GK), (w3b[i], w3r, NCH, GK), (w2b[i], w2r, FC, 1)):
                    for c in range(nch):
                        k.dma("pool", lambda e, wsb=wsb, wdr=wdr, c=c, g=g: e.indirect_dma_start(
                            out=wsb.t[:, c * g:(c + 1) * g, :].rearrange("p a b -> p (a b)"), out_offset=None, in_=wdr[l][c][:, :],
                            in_offset=bass.IndirectOffsetOnAxis(ap=widx.t[:, b:b + 1], axis=0)), reads=[widx], writes=[wsb],
                              accumulate_writes=(c > 0))

            load_block(0)
            for b in range(NB):
                i = b % 2
                if b + 1 < NB:
                    load_block(b + 1)
                for r in range(2):
                    for kc in range(KC):
                        k.op("pe", lambda e, kc=kc, r=r: e.transpose(out=pT.t[:, kc, :], in_=xsb[i].t[:, r, kc * 128:(kc + 1) * 128], identity=ident_bf.t[:]),
                             reads=[xsb[i], ident_bf], writes=[pT])
                    k.op("act", lambda e, r=r: e.activation(out=xTb.t[:, :, r * 128:(r + 1) * 128], in_=pT.t[:], func=AF.Copy), reads=[pT], writes=[xTb])
                for fc in range(FC):
                    p1 = nxt(pA, "pa")
                    p3 = nxt(pA, "pa")
                    for kc in range(KC):
                        k.op("pe", lambda e, kc=kc: e.matmul(p1.t[:, 0:BLK], lhsT=w1b[i].t[:, kc, fc * 128:(fc + 1) * 128], rhs=xTb.t[:, kc, :],
                                                             start=(kc == 0), stop=(kc == KC - 1)), reads=[w1b[i], xTb], writes=[p1])
                    for kc in range(KC):
                        k.op("pe", lambda e, kc=kc: e.matmul(p3.t[:, 0:BLK], lhsT=w3b[i].t[:, kc, fc * 128:(fc + 1) * 128], rhs=xTb.t[:, kc, :],
                                                             start=(kc == 0), stop=(kc == KC - 1)), reads=[w3b[i], xTb], writes=[p3])
                    sg = nxt(f512, "f")
                    k.op("act", lambda e: e.activation(out=sg.t[:, 0:BLK], in_=p1.t[:, 0:BLK], func=AF.Silu), reads=[p1], writes=[sg])
                    k.op("dve", lambda e: e.tensor_tensor(out=hid.t[:, fc, :], in0=p3.t[:, 0:BLK], in1=sg.t[:, 0:BLK], op=ALU.mult),
                         reads=[p3, sg], writes=[hid])
                for r in range(2):
                    yo = nxt(yo2, "yo")
                    for nb in range(4):
                        pp = nxt(pA, "pa")
                        for fc in range(FC):
                            k.op("pe", lambda e, fc=fc: e.matmul(pp.t[:], lhsT=hid.t[:, fc, r * 128:(r + 1) * 128], rhs=w2b[i].t[:, fc, nb * 512:(nb + 1) * 512],
                                                                 start=(fc == 0), stop=(fc == FC - 1)), reads=[hid, w2b[i]], writes=[pp])
                        if nb % 2 == 0:
                            k.op("act", lambda e, nb=nb: e.activation(out=yo.t[:, nb * 512:(nb + 1) * 512], in_=pp.t[:], func=AF.Copy), reads=[pp], writes=[yo])
                        else:
                            k.op("dve", lambda e, nb=nb: e.tensor_copy(out=yo.t[:, nb * 512:(nb + 1) * 512], in_=pp.t[:]), reads=[pp], writes=[yo])
                    rr0 = b * BLK + r * 128
                    k.dma("sp", lambda e: e.dma_start(out=Ys[rr0:rr0 + 128, :], in_=yo.t[:]), reads=[yo], writes=[R("ys")], accumulate_writes=True)
            st_.close()

            st_ = Stage()
            xt2 = [st_.sb("xt", [128, D], F32) for _ in range(2)]
            y02 = [st_.sb("y0", [128, D], F32) for _ in range(2)]
            y12 = [st_.sb("y1", [128, D], F32) for _ in range(2)]
            dst = out if last else xres
            for tt in range(NT):
                r0 = tt * 128
                xt = nxt(xt2, "x")
                k.dma("sp", lambda e: e.dma_start(out=xt.t[:], in_=xres[r0:r0 + 128, :]), reads=[R("xres", tt)], writes=[xt])
                ys = [nxt(y02, "y0"), nxt(y12, "y1")]
                for kk in range(2):
                    k.dma("pool", lambda e, kk=kk: e.indirect_dma_start(
                        out=ys[kk].t[:], out_offset=None, in_=Ys[:, :],
                        in_offset=bass.IndirectOffsetOnAxis(ap=desti.t[:, tt * 2 + kk:tt * 2 + kk + 1], axis=0)),
                          reads=[R("ys"), desti], writes=[ys[kk]])
                k.op("dve", lambda e: e.scalar_tensor_tensor(out=xt.t[:], in0=ys[0].t[:], scalar=gates.t[:, tt, 0:1], in1=xt.t[:],
                                                             op0=ALU.mult, op1=ALU.add), reads=[ys[0], gates], writes=[xt])
                k.op("dve", lambda e: e.scalar_tensor_tensor(out=xt.t[:], in0=ys[1].t[:], scalar=gates.t[:, tt, 1:2], in1=xt.t[:],
                                                             op0=ALU.mult, op1=ALU.add), reads=[ys[1], gates], writes=[xt])
                k.dma("sp", lambda e: e.dma_start(out=dst[r0:r0 + 128, :], in_=xt.t[:]), reads=[xt],
                      writes=[R("out" if last else "xres", tt)])
            st_.close()
        LS.close()

    k.barrier()
    return nc, k


def t5_bucket_np(dist):
    max_exact = 16
    d_f = np.maximum(dist, 1).astype(np.float32)
    large = max_exact + (np.log(d_f / np.float32(max_exact)) / np.float32(math.log(128 / max_exact)) * np.float32(16)).astype(np.int32)
    large = np.minimum(large, 31)
    return np.where(dist < max_exact, dist, large)


def host_prep(inp, L, S, DE):
    f = lambda a: np.ascontiguousarray(np.asarray(a, dtype=np.float32))
    rep = lambda v: np.broadcast_to(np.asarray(v, np.float32).reshape(1, -1), (128, np.asarray(v).size))
    pars = np.zeros((L, 128, NPAR), np.float32)

    def put(l, name, arr):
        o, w = _off[name]
        pars[l, :, o:o + w] = arr.reshape(128, w)

    tbl = np.asarray(inp["rel_bias_table"], np.float32)
    for l in range(L):
        put(l, "gmixT", np.asarray(inp["g_mix"][l]).reshape(KC, 128).T)
        put(l, "gcrossT", np.asarray(inp["g_cross"][l]).reshape(KC, 128).T)
        put(l, "gmemT", np.asarray(inp["g_mem"][l]).reshape(KC, 128).T)
        put(l, "gq2", np.tile(np.asarray(inp["g_q"][l]), 2))
        put(l, "gk2", np.tile(np.asarray(inp["g_k"][l]), 2))
        put(l, "gsub", np.asarray(inp["g_subln"][l]))
        put(l, "gqc", np.asarray(inp["g_qc"][l]))
        put(l, "gkc", np.asarray(inp["g_kc"][l]))
        cw = np.asarray(inp["conv_w"][l])
        put(l, "convw", cw.reshape(CW, 8, 128).transpose(2, 1, 0))
        put(l, "convb", np.asarray(inp["conv_b"][l]).reshape(8, 128).T)
        put(l, "lng", np.asarray(inp["conv_ln_g"][l]).reshape(8, 128).T)
        put(l, "lnb", np.asarray(inp["conv_ln_b"][l]).reshape(8, 128).T)
        put(l, "dl", rep(np.asarray(inp["diff_lambda"][l]).reshape(-1)))
        put(l, "brt", rep(np.concatenate([np.asarray(inp["b_group"][l]), np.asarray(inp["b_router"][l])])))
        put(l, "relc", rep(tbl[31, :]))
    gffnb = np.stack([rep(inp["g_ffn"][l]) for l in range(L)]).astype(np.float32)
    kk_, qq_ = np.meshgrid(np.arange(128), np.arange(128), indexing="ij")
    biasT = np.zeros((128, NH * 2, 128), np.float32)
    for dlt in range(2):
        dist = dlt * 128 + qq_ - kk_
        bk = t5_bucket_np(np.maximum(dist, 0))
        for h in range(NH):
            biasT[:, h * 2 + dlt, :] = np.where(dist >= 0, tbl[bk, h], np.float32(MASKV))
    consts = np.zeros((128, 5, 128), np.float32)
    consts[:, 0, :] = np.eye(128)
    consts[:, 1, :] = 1.0
    consts[0:64, 2, 0:64] = 1.0
    consts[64:128, 2, 64:128] = 1.0
    consts[:, 3, :] = (kk_ < qq_)
    consts[:, 4, :] = np.arange(128).reshape(128, 1)
    w_rt = np.concatenate([np.asarray(inp["w_group"], np.float32)[:L], np.asarray(inp["w_router"], np.float32)[:L]], axis=-1)
    FC = DE // 128
    shared = {
        "pars": pars, "gffnb": gffnb, "biasT": biasT, "consts": consts,
        "w_in": f(inp["w_in"][:L]), "w_out": f(inp["w_out"][:L]), "wq_c": f(inp["wq_c"][:L]), "wkv_c": f(inp["wkv_c"][:L]),
        "wo_c": f(inp["wo_c"][:L]), "w_rt": f(w_rt),
    }
    if "F" in STAGES:
        NCH = KC * DE // 2048
        GK = 2048 // DE
        for nm, src_ in (("w1r", "w1"), ("w3r", "w3")):
            for l in range(L):
                a6 = np.asarray(inp[src_][l], np.float32).reshape(NE, NCH, GK, 128, DE)
                for c in range(NCH):
                    shared[f"{nm}_{l}_{c}"] = f(a6[:, c].transpose(0, 2, 1, 3).reshape(NE * 128, GK * DE))
        for l in range(L):
            a5 = np.asarray(inp["w2"][l], np.float32).reshape(NE, FC, 128, D)
            for c in range(FC):
                shared[f"w2r_{l}_{c}"] = f(a5[:, c].reshape(NE * 128, D))
    return shared


_CACHE = {}


def run(inp, L, NSEQ, S, DE, debug=()):
    key = (L, NSEQ, S, DE, tuple(debug))
    if key not in _CACHE:
        _CACHE[key] = build_program(L, NSEQ, S, DE, debug)
    nc, kb = _CACHE[key]
    shared = host_prep(inp, L, S, DE)
    T = NSEQ * S
    NB = -(-(2 * T + NE * (BLK - 1)) // BLK)
    shared["thr"] = np.ascontiguousarray(np.broadcast_to((np.arange(NB, dtype=np.float32) * BLK).reshape(1, NB), (128, NB)))
    x = np.asarray(inp["x"], np.float32)
    mem = np.asarray(inp["mem"], np.float32)
    in_maps = []
    for c in range(NCORES):
        m = dict(shared)
        m["x"] = np.ascontiguousarray(x[c * NSEQ:(c + 1) * NSEQ].reshape(T, D))
        m["mem"] = np.ascontiguousarray(mem[c * NSEQ:(c + 1) * NSEQ].reshape(NSEQ * MEM, D))
        in_maps.append(m)
    res = run_bass_kernel_spmd(nc, in_maps, core_ids=list(range(NCORES)))
    return res


def kernel(**inputs):
    L, NSEQ, S, DE = 2, 2, 2048, 512
    res = run(inputs, L, NSEQ, S, DE)
    outs = [r["out"].reshape(NSEQ, S, D) for r in res.results]
    return np.concatenate(outs, axis=0).astype(np.float32)
```

```python
import math
from contextlib import ExitStack

import numpy as np
import concourse.bass as bass
import concourse.mybir as mybir
from concourse.bass_utils import run_bass_kernel_spmd

AF = mybir.ActivationFunctionType
ALU = mybir.AluOpType
AX = mybir.AxisListType
F32 = mybir.dt.float32
BF16 = mybir.dt.bfloat16
I32 = mybir.dt.int32

D = 2048
KC = D // 128
DQ = 64
NH = 8
D_IN = 5120
CW = 31
MEM = 256
NHC = 4
NG = 8
NE = 64
BLK = 256
EPS = 1e-6
NCORES = 8
MASKV = -30000.0
STAGES = "ABCDEF"

_off = {}
_n = 0
for _name, _w in [("gmixT", 16), ("gcrossT", 16), ("gmemT", 16), ("gq2", 1), ("gk2", 1), ("gsub", 1),
                  ("gqc", 1), ("gkc", 1), ("convw", 8 * CW), ("convb", 8), ("lng", 8), ("lnb", 8),
                  ("dl", 4 * DQ), ("brt", 72), ("relc", 8)]:
    _off[_name] = (_n, _w)
    _n += _w
NPAR = _n


class Buf:
    __slots__ = ("t", "w", "r")

    def __init__(self, t):
        self.t = t
        self.w = []
        self.r = []


class KB:
    NRING = 64

    def __init__(self, nc, es):
        self.nc = nc
        self.es = es
        self.eng = {"pe": nc.tensor, "dve": nc.vector, "act": nc.scalar, "pool": nc.gpsimd, "sp": nc.sync}
        self.chan = {}
        self.waited = {e: {} for e in self.eng}
        self.ring = [es.enter_context(nc.semaphore(f"dq{i}")) for i in range(self.NRING)]
        self.ring_cnt = [0] * self.NRING
        self.ring_i = 0
        self.nsem = 0
        self.ninstr = 0

    def _chan(self, e):
        c = self.chan.get(e)
        if c is None or c[2] >= 30000:
            self.nsem += 1
            c = [f"c{e}{self.nsem}", self.es.enter_context(self.nc.semaphore(f"c_{e}_{self.nsem}")), 0]
            self.chan[e] = c
        return c

    def _wait(self, e, toks):
        wd = self.waited[e]
        for (key, sem, v, prod) in toks:
            if prod == e and e == "pe":
                continue
            if wd.get(key, 0) >= v:
                continue
            wd[key] = v
            self.eng[e].wait_ge(sem, v)

    @staticmethod
    def _prune(lst):
        last = {}
        out = []
        for t in lst:
            if t[3] == "dma":
                out.append(t)
            else:
                last[t[3]] = t
        return out + list(last.values())

    def _deps(self, e, reads, writes):
        toks = []
        for b in reads:
            toks += b.w
        for b in writes:
            toks += b.w
            toks += b.r
        self._wait(e, toks)

    def _commit(self, tok, reads, writes):
        for b in reads:
            b.r.append(tok)
            if len(b.r) > 6:
                b.r = self._prune(b.r)
        for b in writes:
            b.w = [tok]
            b.r = []

    def op(self, e, fn, reads=(), writes=()):
        self._deps(e, reads, writes)
        ins = fn(self.eng[e])
        c = self._chan(e)
        c[2] += 1
        ins.then_inc(c[1], 1)
        tok = (c[0], c[1], c[2], e)
        self._commit(tok, reads, writes)
        self.ninstr += 1
        return tok

    def dma(self, q, fn, reads=(), writes=(), accumulate_writes=False):
        if accumulate_writes:
            toks = []
            for b in reads:
                toks += b.w
            for b in writes:
                if b.r:
                    toks += b.w
                    toks += b.r
            self._wait(q, toks)
        else:
            self._deps(q, reads, writes)
        i = self.ring_i
        self.ring_i = (i + 1) % self.NRING
        if self.ring_cnt[i] > 0:
            self._wait(q, [(f"dq{i}", self.ring[i], self.ring_cnt[i], "dma")])
        ins = fn(self.eng[q])
        self.ring_cnt[i] += 16
        ins.then_inc(self.ring[i], 16)
        tok = (f"dq{i}", self.ring[i], self.ring_cnt[i], "dma")
        for b in reads:
            b.r.append(tok)
        for b in writes:
            if accumulate_writes and not b.r:
                b.w.append(tok)
            else:
                b.w = [tok]
                b.r = []
        self.ninstr += 1
        return tok

    def barrier(self):
        toks = [(c[0], c[1], c[2], e) for e, c in self.chan.items() if c[2] > 0]
        toks += [(f"dq{i}", self.ring[i], self.ring_cnt[i], "dma") for i in range(self.NRING) if self.ring_cnt[i] > 0]
        for e in self.eng:
            self._wait(e, toks)

    def drain(self, e, bufs):
        toks = []
        for b in bufs:
            toks += b.w
            toks += b.r
        self._wait(e, toks)


def build_program(L, NSEQ, S, DE, debug=()):
    T = NSEQ * S
    NT = T // 128
    NB = -(-(2 * T + NE * (BLK - 1)) // BLK)
    PR = NB * BLK
    FC = DE // 128
    nc = bass.Bass("TRN2", target_bir_lowering=False)
    es = ExitStack()

    def dram(name, shape, dt, kind="Internal"):
        return nc.dram_tensor(name, list(shape), dt, kind=kind).ap()

    x_in = dram("x", [T, D], F32, "ExternalInput")
    mem_in = dram("mem", [NSEQ * MEM, D], F32, "ExternalInput")
    pars_in = dram("pars", [L, 128, NPAR], F32, "ExternalInput")
    gffn_in = dram("gffnb", [L, 128, D], F32, "ExternalInput")
    biasT_in = dram("biasT", [128, NH * 2, 128], F32, "ExternalInput")
    consts_in = dram("consts", [128, 5, 128], F32, "ExternalInput")
    thr_in = dram("thr", [128, NB], F32, "ExternalInput")
    w_in = dram("w_in", [L, D, D_IN], F32, "ExternalInput")
    w_out = dram("w_out", [L, D, D], F32, "ExternalInput")
    wq_c = dram("wq_c", [L, D, 512], F32, "ExternalInput")
    wkv_c = dram("wkv_c", [L, D, 1024], F32, "ExternalInput")
    wo_c = dram("wo_c", [L, 512, D], F32, "ExternalInput")
    w_rt = dram("w_rt", [L, D, 72], F32, "ExternalInput")
    if "F" in STAGES:
        NCH = KC * DE // 2048
        GK = 2048 // DE
        w1r = [[dram(f"w1r_{l_}_{c}", [NE * 128, 2048], F32, "ExternalInput") for c in range(NCH)] for l_ in range(L)]
        w3r = [[dram(f"w3r_{l_}_{c}", [NE * 128, 2048], F32, "ExternalInput") for c in range(NCH)] for l_ in range(L)]
        w2r = [[dram(f"w2r_{l_}_{c}", [NE * 128, 2048], F32, "ExternalInput") for c in range(FC)] for l_ in range(L)]
    out = dram("out", [T, D], F32, "ExternalOutput")

    xres = dram("xres", [T, D], F32)
    qTs = dram("qTs", [NSEQ, NH, 128, S], BF16)
    kTs = dram("kTs", [NSEQ, NH, 128, S], BF16)
    Vs = dram("Vs", [NSEQ, S, NH * 128], BF16)
    Us = dram("Us", [NSEQ, 8, 128, S], F32)
    mixT = dram("mixT", [16, 128, T], BF16)
    Hrows = dram("Hrows", [T, D], BF16)
    Xs = dram("Xs", [PR, D], BF16)
    Ys = dram("Ys", [PR, D], F32)
    dbg = {}
    for name, shape, dt in debug:
        dbg[name] = dram("dbg_" + name, shape, dt, "ExternalOutput")

    k = KB(nc, es)
    regions = {}
    uid = [0]

    def R(*key):
        b = regions.get(key)
        if b is None:
            b = Buf(None)
            regions[key] = b
        return b

    class Stage:
        def __init__(self):
            self.es = ExitStack()

        def sb(self, name, shape, dt):
            uid[0] += 1
            return Buf(self.es.enter_context(nc.sbuf_tensor(f"{name}_{uid[0]}", list(shape), dt)))

        def close(self):
            k.barrier()
            self.es.close()

    G = Stage()

    def ps(name, shape, dt=F32):
        return Buf(es.enter_context(nc.psum_tensor(name, list(shape), dt)))

    cst = G.sb("cst", [128, 5, 128], F32)
    ident_bf = G.sb("ident_bf", [128, 128], BF16)
    ones_bf = G.sb("ones_bf", [128, 128], BF16)
    ltri_bf = G.sb("ltri_bf", [128, 128], BF16)
    biasT = G.sb("biasT_sb", [128, NH * 2, 128], BF16)
    pars = G.sb("pars_sb", [128, NPAR], F32)
    small = G.sb("small", [128, 16], F32)
    col = [G.sb(f"col{i}", [128, 8], F32) for i in range(4)]
    f512 = [G.sb(f"f512_{i}", [128, 512], F32) for i in range(6)]
    b512 = [G.sb(f"b512_{i}", [128, 512], BF16) for i in range(4)]
    ones_f = cst.t[:, 1, :]
    bd64_f = cst.t[:, 2, :]
    iota_p = cst.t[:, 4, 0:1]
    pT = ps("pT", [128, KC, 128], BF16)
    pA = [ps(f"pA{i}", [128, 512]) for i in range(6)]

    k.dma("sp", lambda e: e.dma_start(out=cst.t[:], in_=consts_in[:, :, :]), writes=[cst])
    k.dma("pool", lambda e: e.dma_start(out=biasT.t[:], in_=biasT_in[:, :, :]), writes=[biasT])
    k.op("dve", lambda e: e.tensor_copy(out=ident_bf.t[:], in_=cst.t[:, 0, :]), reads=[cst], writes=[ident_bf])
    k.op("dve", lambda e: e.tensor_copy(out=ones_bf.t[:], in_=cst.t[:, 1, :]), reads=[cst], writes=[ones_bf])
    k.op("dve", lambda e: e.tensor_copy(out=ltri_bf.t[:], in_=cst.t[:, 3, :]), reads=[cst], writes=[ltri_bf])

    def P(name, j=0, w=1):
        o, _ = _off[name]
        return pars.t[:, o + j:o + j + w]

    cnt = {}

    def nxt(lst, key):
        i = cnt.get(key, 0)
        cnt[key] = i + 1
        return lst[i % len(lst)]

    def rms_tile(W, x_rows, xreg, gcolT, dst, dst_c0, a=1.0 / D):
        xt = nxt(W["xt"], "x")
        k.dma("sp", lambda e: e.dma_start(out=xt.t[:], in_=x_rows), reads=[xreg], writes=[xt])
        c0 = nxt(col, "c")
        junk = W["junk"]
        k.op("act", lambda e: e.activation(out=junk.t[:], in_=xt.t[:], func=AF.Square, accum_out=c0.t[:, 0:1]),
             reads=[xt], writes=[junk, c0])
        k.op("act", lambda e: e.activation(out=c0.t[:, 1:2], in_=c0.t[:, 0:1], func=AF.Sqrt, scale=a, bias=EPS),
             reads=[c0], writes=[c0])
        k.op("dve", lambda e: e.reciprocal(out=c0.t[:, 2:3], in_=c0.t[:, 1:2]), reads=[c0], writes=[c0])
        hb = nxt(W["hb"], "hb")
        k.op("act", lambda e: e.activation(out=hb.t[:], in_=xt.t[:], func=AF.Copy, scale=c0.t[:, 2:3]),
             reads=[xt, c0], writes=[hb])
        for kc in range(KC):
            k.op("pe", lambda e, kc=kc: e.transpose(out=pT.t[:, kc, :], in_=hb.t[:, kc * 128:(kc + 1) * 128],
                                                    identity=ident_bf.t[:]),
                 reads=[hb, ident_bf], writes=[pT])
        k.op("dve", lambda e: e.tensor_tensor(out=dst.t[:, :, dst_c0:dst_c0 + 128], in0=pT.t[:],
                                              in1=gcolT.unsqueeze(2).to_broadcast([128, KC, 128]), op=ALU.mult),
             reads=[pT, pars], writes=[dst])
        return xt

    def load_w(W, dram_view, cols, width=512):
        wb = nxt(W["wb"], "w")
        k.dma("pool", lambda e: e.dma_start(out=wb.t[:, :, 0:width], in_=dram_view[:, :, cols:cols + width]),
              writes=[wb])
        return wb

    def fm_norm(psrc, gcol, a, bconst, n, ones_mat):
        sq = nxt(f512, "f")
        k.op("act", lambda e: e.activation(out=sq.t[:, 0:n], in_=psrc.t[:, 0:n], func=AF.Square), reads=[psrc], writes=[sq])
        pss = nxt(pA, "pa")
        k.op("pe", lambda e: e.matmul(pss.t[:, 0:n], lhsT=ones_mat, rhs=sq.t[:, 0:n], start=True, stop=True),
             reads=[sq, cst], writes=[pss])
        rs = nxt(f512, "f")
        k.op("act", lambda e: e.activation(out=rs.t[:, 0:n], in_=pss.t[:, 0:n], func=AF.Sqrt, scale=a, bias=bconst),
             reads=[pss], writes=[rs])
        k.op("dve", lambda e: e.reciprocal(out=rs.t[:, 0:n], in_=rs.t[:, 0:n]), reads=[rs], writes=[rs])
        ob = nxt(b512, "b")
        k.op("dve", lambda e: e.scalar_tensor_tensor(out=ob.t[:, 0:n], in0=psrc.t[:, 0:n], scalar=gcol, in1=rs.t[:, 0:n],
                                                     op0=ALU.mult, op1=ALU.mult),
             reads=[psrc, rs, pars, small], writes=[ob])
        return ob

    def rmsW(st):
        return {"xt": [st.sb("xt", [128, D], F32) for _ in range(2)],
                "hb": [st.sb("hb", [128, D], BF16) for _ in range(2)],
                "junk": st.sb("junk", [128, D], BF16)}

    x_src = x_in
    x_key = "xin"
    for l in range(L):
        last = (l == L - 1)
        lam_init = 0.8 - 0.6 * math.exp(-0.3 * l)
        LS = Stage()
        k.dma("sp", lambda e: e.dma_start(out=pars.t[:], in_=pars_in[l, :, :]), writes=[pars])
        o_dl = _off["dl"][0]
        tmpc = f512[0]
        for i in range(2):
            k.op("dve", lambda e, i=i: e.tensor_tensor(out=tmpc.t[:, 0:DQ], in0=pars.t[:, o_dl + 2 * i * DQ:o_dl + (2 * i + 1) * DQ],
                                                       in1=pars.t[:, o_dl + (2 * i + 1) * DQ:o_dl + (2 * i + 2) * DQ], op=ALU.mult),
                 reads=[pars], writes=[tmpc])
            k.op("dve", lambda e, i=i: e.tensor_reduce(out=small.t[:, 2 + i:3 + i], in_=tmpc.t[:, 0:DQ], axis=AX.X, op=ALU.add),
                 reads=[tmpc], writes=[small])
        k.op("act", lambda e: e.activation(out=small.t[:, 4:6], in_=small.t[:, 2:4], func=AF.Exp), reads=[small], writes=[small])
        k.op("dve", lambda e: e.scalar_tensor_tensor(out=small.t[:, 0:1], in0=small.t[:, 5:6], scalar=-lam_init, in1=small.t[:, 4:5],
                                                     op0=ALU.add, op1=ALU.subtract), reads=[small], writes=[small])
        k.op("dve", lambda e: e.tensor_scalar(out=small.t[:, 1:2], in0=P("gsub"), scalar1=1.0 - lam_init, scalar2=None, op0=ALU.mult),
             reads=[pars], writes=[small])
        neglam = small.t[:, 0:1]
        gsubs = small.t[:, 1:2]

        if "A" in STAGES:
            st_ = Stage()
            W = rmsW(st_)
            W["wb"] = [st_.sb("wb", [128, KC, 512], BF16) for _ in range(3)]
            hT = st_.sb("hT", [128, KC, S], BF16)
            w_in_v = w_in[l].rearrange("(kc p) n -> p kc n", p=128)
            w_order = [(s_, nb_) for s_ in range(NSEQ) for nb_ in (0, 1, 2, 3, 4, 5, 6, 8, 7, 9)]
            w_loaded = {}
            w_pos = [0]

            def get_w(s_, nb_):
                target = min(w_order.index((s_, nb_)) + 1, len(w_order) - 1)
                while w_pos[0] <= target:
                    key_ = w_order[w_pos[0]]
                    w_loaded[key_] = load_w(W, w_in_v, key_[1] * 512)
                    w_pos[0] += 1
                return w_loaded[(s_, nb_)]

            for s in range(NSEQ):
                for tt in range(S // 128):
                    r0 = s * S + tt * 128
                    rms_tile(W, x_src[r0:r0 + 128, :], R(x_key, r0 // 128), P("gmixT", 0, 16), hT, tt * 128)
                for nb in range(4):
                    wb = get_w(s, nb)
                    isq = nb < 2
                    dstT = qTs if isq else kTs
                    gcol = P("gq2") if isq else P("gk2")
                    a, bc = (1.0, DQ * EPS) if isq else (1.0 / DQ, EPS)
                    for j in range(4):
                        head = (nb % 2) * 4 + j
                        for tb in range(S // 512):
                            pp = nxt(pA, "pa")
                            for kc in range(KC):
                                k.op("pe", lambda e, kc=kc: e.matmul(pp.t[:], lhsT=wb.t[:, kc, j * 128:(j + 1) * 128],
                                                                     rhs=hT.t[:, kc, tb * 512:(tb + 1) * 512],
                                                                     start=(kc == 0), stop=(kc == KC - 1)),
                                     reads=[wb, hT], writes=[pp])
                            ob = fm_norm(pp, gcol, a, bc, 512, bd64_f)
                            k.dma("sp", lambda e: e.dma_start(out=dstT[s, head, :, tb * 512:(tb + 1) * 512], in_=ob.t[:]),
                                  reads=[ob], writes=[R("qk", isq, s, head)], accumulate_writes=True)
                for nb in range(4, 6):
                    wb = get_w(s, nb)
                    for tt in range(S // 128):
                        pp = nxt(pA, "pa")
                        for kc in range(KC):
                            k.op("pe", lambda e, kc=kc: e.matmul(pp.t[:], lhsT=hT.t[:, kc, tt * 128:(tt + 1) * 128], rhs=wb.t[:, kc, :],
                                                                 start=(kc == 0), stop=(kc == KC - 1)),
                                 reads=[wb, hT], writes=[pp])
                        ob = nxt(b512, "b")
                        k.op("act", lambda e: e.activation(out=ob.t[:], in_=pp.t[:], func=AF.Copy), reads=[pp], writes=[ob])
                        k.dma("sp", lambda e: e.dma_start(out=Vs[s, tt * 128:(tt + 1) * 128, (nb - 4) * 512:(nb - 3) * 512], in_=ob.t[:]),
                              reads=[ob], writes=[R("v", s)], accumulate_writes=True)
                for i in range(2):
                    wa = get_w(s, 6 + i)
                    wg = get_w(s, 8 + i)
                    for j in range(4):
                        ch = i * 4 + j
                        for tb in range(S // 512):
                            pa_ = nxt(pA, "pa")
                            pg_ = nxt(pA, "pa")
                            for kc in range(KC):
                                k.op("pe", lambda e, kc=kc: e.matmul(pa_.t[:], lhsT=wa.t[:, kc, j * 128:(j + 1) * 128],
                                                                     rhs=hT.t[:, kc, tb * 512:(tb + 1) * 512],
                                                                     start=(kc == 0), stop=(kc == KC - 1)),
                                     reads=[wa, hT], writes=[pa_])
                            for kc in range(KC):
                                k.op("pe", lambda e, kc=kc: e.matmul(pg_.t[:], lhsT=wg.t[:, kc, j * 128:(j + 1) * 128],
                                                                     rhs=hT.t[:, kc, tb * 512:(tb + 1) * 512],
                                                                     start=(kc == 0), stop=(kc == KC - 1)),
                                     reads=[wg, hT], writes=[pg_])
                            sg = nxt(f512, "f")
                            k.op("act", lambda e: e.activation(out=sg.t[:], in_=pg_.t[:], func=AF.Sigmoid), reads=[pg_], writes=[sg])
                            ub = nxt(f512, "f")
                            k.op("dve", lambda e: e.tensor_tensor(out=ub.t[:], in0=pa_.t[:], in1=sg.t[:], op=ALU.mult),
                                 reads=[pa_, sg], writes=[ub])
                            k.dma("sp", lambda e: e.dma_start(out=Us[s, ch, :, tb * 512:(tb + 1) * 512], in_=ub.t[:]),
                                  reads=[ub], writes=[R("u", s, ch)], accumulate_writes=True)
            st_.close()

        if "qT" in dbg and l == 0:
            k.dma("sp", lambda e: e.dma_start(out=dbg["qT"][:, :, :, :], in_=qTs[:, :, :, :]), writes=[R("dbg", "qT")])
            k.dma("sp", lambda e: e.dma_start(out=dbg["kT"][:, :, :, :], in_=kTs[:, :, :, :]), writes=[R("dbg", "kT")])
            k.dma("sp", lambda e: e.dma_start(out=dbg["V"][:, :, :], in_=Vs[:, :, :]), writes=[R("dbg", "V")])
            k.dma("sp", lambda e: e.dma_start(out=dbg["U"][:, :, :, :], in_=Us[:, :, :, :]), writes=[R("dbg", "U")])
            k.barrier()

        if "B" in STAGES:
            st_ = Stage()
            qh2 = [st_.sb("qh", [128, S], BF16) for i in range(2)]
            kh2 = [st_.sb("kh", [128, S], BF16) for i in range(2)]
            vh2 = [st_.sb("vh", [128, S // 128, 128], BF16) for i in range(2)]
            attn_r = [st_.sb("attn_r", [128, 512], F32) for i in range(2)]
            attn_t = [st_.sb("attn_t", [128, 512], F32) for i in range(2)]
            it = 0
            for s in range(NSEQ):
                for h in range(NH):
                    qh, kh, vh = qh2[it % 2], kh2[it % 2], vh2[it % 2]
                    it += 1
                    k.dma("sp", lambda e: e.dma_start(out=qh.t[:], in_=qTs[s, h, :, :]), reads=[R("qk", True, s, h)], writes=[qh])
                    k.dma("sp", lambda e: e.dma_start(out=kh.t[:], in_=kTs[s, h, :, :]), reads=[R("qk", False, s, h)], writes=[kh])
                    k.dma("sp", lambda e: e.dma_start(out=vh.t[:], in_=Vs[s, :, h * 128:(h + 1) * 128].rearrange("(t p) d -> p t d", p=128)),
                          reads=[R("v", s)], writes=[vh])
                    for qc in range(S // 512):
                        tms = []
                        for m in range(2):
                            pO = pA[2 * m]
                            pL = pA[2 * m + 1]
                            nk = (qc + 1) * 4

                            def emit_qk(kj, m=m, qc=qc):
                                q_lo = max(qc * 512, kj * 128)
                                c0 = q_lo - qc * 512
                                sT = nxt(pA[4:6], "pa_s")
                                blist = [dlt for dlt in (0, 1) if qc * 4 <= kj + dlt < (qc + 1) * 4]
                                k.op("pe", lambda e: e.matmul(sT.t[:, c0:512], lhsT=kh.t[m * 64:(m + 1) * 64, kj * 128:(kj + 1) * 128],
                                                              rhs=qh.t[m * 64:(m + 1) * 64, q_lo:(qc + 1) * 512],
                                                              start=True, stop=(len(blist) == 0)),
                                     reads=[kh, qh], writes=[sT])
                                for bi, dlt in enumerate(blist):
                                    cc = (kj + dlt) * 128 - qc * 512
                                    k.op("pe", lambda e, cc=cc, dlt=dlt, bi=bi: e.matmul(
                                        sT.t[:, cc:cc + 128], lhsT=ident_bf.t[:], rhs=biasT.t[:, h * 2 + dlt, :],
                                        start=False, stop=(bi == len(blist) - 1)),
                                         reads=[ident_bf, biasT], writes=[sT])
                                return sT, c0

                            nxt_qk = emit_qk(0)
                            for kj in range(nk):
                                sT, c0 = nxt_qk
                                if kj + 1 < nk:
                                    nxt_qk = emit_qk(kj + 1)
                                near_end = min((kj + 2) * 128, (qc + 1) * 512) - qc * 512
                                pt = nxt(b512, "b")
                                if near_end > c0:
                                    k.op("act", lambda e: e.activation(out=pt.t[:, c0:near_end], in_=sT.t[:, c0:near_end], func=AF.Exp),
                                         reads=[sT], writes=[pt])
                                if near_end < 512:
                                    lo = max(near_end, c0)
                                    k.op("act", lambda e: e.activation(out=pt.t[:, lo:512], in_=sT.t[:, lo:512], func=AF.Exp,
                                                                       bias=P("relc", h)),
                                         reads=[sT, pars], writes=[pt])
                                k.op("pe", lambda e: e.matmul(pO.t[:, c0:512], lhsT=vh.t[:, kj, :], rhs=pt.t[:, c0:512],
                                                              start=(kj == 0), stop=(kj == nk - 1)),
                                     reads=[vh, pt], writes=[pO])
                                k.op("pe", lambda e: e.matmul(pL.t[:, c0:512], lhsT=ones_bf.t[:], rhs=pt.t[:, c0:512],
                                                              start=(kj == 0), stop=(kj == nk - 1)),
                                     reads=[ones_bf, pt], writes=[pL])
                            r_ = attn_r[m]
                            k.op("dve", lambda e: e.reciprocal(out=r_.t[:], in_=pL.t[:]), reads=[pL], writes=[r_])
                            t_ = attn_t[m]
                            k.op("dve", lambda e: e.tensor_tensor(out=t_.t[:], in0=pO.t[:], in1=r_.t[:], op=ALU.mult),
                                 reads=[pO, r_], writes=[t_])
                            tms.append(t_)
                        a_ = nxt(f512, "f")
                        k.op("dve", lambda e: e.scalar_tensor_tensor(out=a_.t[:], in0=tms[1].t[:], scalar=neglam, in1=tms[0].t[:],
                                                                     op0=ALU.mult, op1=ALU.add),
                             reads=[tms[0], tms[1], small], writes=[a_])
                        ob = fm_norm(a_, gsubs, 1.0 / 128, EPS, 512, ones_f)
                        c_lo = s * S + qc * 512
                        k.dma("sp", lambda e: e.dma_start(out=mixT[h, :, c_lo:c_lo + 512], in_=ob.t[:]),
                              reads=[ob], writes=[R("mix", c_lo // 512)], accumulate_writes=True)
            st_.close()

        if "C" in STAGES:
            st_ = Stage()
            Ubuf = st_.sb("Ubuf", [128, 32 + S], F32)
            Yc = [st_.sb("Yc", [128, S], F32) for j in range(8)]
            mean = st_.sb("mean", [128, 512], F32)
            msq = st_.sb("msq", [128, 512], F32)
            var = st_.sb("var", [128, 512], F32)
            k.op("dve", lambda e: e.memset(Ubuf.t[:, 0:32], 0.0), writes=[Ubuf])
            o_cw = _off["convw"][0]
            for s in range(NSEQ):
                for j in range(8):
                    k.dma("sp", lambda e: e.dma_start(out=Ubuf.t[:, 32:32 + S], in_=Us[s, j, :, :]), reads=[R("u", s, j)], writes=[Ubuf])
                    k.op("dve", lambda e: e.tensor_scalar(out=Yc[j].t[:], in0=Ubuf.t[:, 2:2 + S], scalar1=pars.t[:, o_cw + j * CW:o_cw + j * CW + 1],
                                                          scalar2=P("convb", j), op0=ALU.mult, op1=ALU.add),
                         reads=[Ubuf, pars], writes=[Yc[j]])
                    for tap in range(1, CW):
                        k.op("dve", lambda e, tap=tap: e.scalar_tensor_tensor(
                            out=Yc[j].t[:], in0=Ubuf.t[:, 2 + tap:2 + tap + S], scalar=pars.t[:, o_cw + j * CW + tap:o_cw + j * CW + tap + 1],
                            in1=Yc[j].t[:], op0=ALU.mult, op1=ALU.add),
                             reads=[Ubuf, pars], writes=[Yc[j]])
                for tb in range(S // 512):
                    cs = slice(tb * 512, (tb + 1) * 512)
                    pM = nxt(pA, "pa")
                    pQ = nxt(pA, "pa")
                    for j in range(8):
                        k.op("pe", lambda e, j=j: e.matmul(pM.t[:], lhsT=ones_f, rhs=Yc[j].t[:, cs], start=(j == 0), stop=(j == 7)),
                             reads=[cst, Yc[j]], writes=[pM])
                    for j in range(8):
                        sq = nxt(f512, "f")
                        k.op("act", lambda e, j=j: e.activation(out=sq.t[:], in_=Yc[j].t[:, cs], func=AF.Square), reads=[Yc[j]], writes=[sq])
                        k.op("pe", lambda e, j=j: e.matmul(pQ.t[:], lhsT=ones_f, rhs=sq.t[:], start=(j == 0), stop=(j == 7)),
                             reads=[cst, sq], writes=[pQ])
                    k.op("act", lambda e: e.activation(out=mean.t[:], in_=pM.t[:], func=AF.Copy, scale=1.0 / 1024), reads=[pM], writes=[mean])
                    k.op("act", lambda e: e.activation(out=msq.t[:], in_=mean.t[:], func=AF.Square), reads=[mean], writes=[msq])
                    k.op("dve", lambda e: e.scalar_tensor_tensor(out=var.t[:], in0=pQ.t[:], scalar=1.0 / 1024, in1=msq.t[:],
                                                                 op0=ALU.mult, op1=ALU.subtract), reads=[pQ, msq], writes=[var])
                    k.op("act", lambda e: e.activation(out=var.t[:], in_=var.t[:], func=AF.Sqrt, scale=1.0, bias=EPS), reads=[var], writes=[var])
                    k.op("dve", lambda e: e.reciprocal(out=var.t[:], in_=var.t[:]), reads=[var], writes=[var])
                    for j in range(8):
                        t_ = nxt(f512, "f")
                        k.op("dve", lambda e, j=j: e.tensor_tensor(out=t_.t[:], in0=Yc[j].t[:, cs], in1=mean.t[:], op=ALU.subtract),
                             reads=[Yc[j], mean], writes=[t_])
                        k.op("dve", lambda e: e.tensor_tensor(out=t_.t[:], in0=t_.t[:], in1=var.t[:], op=ALU.mult),
                             reads=[var], writes=[t_])
                        ob = nxt(b512, "b")
                        k.op("act", lambda e, j=j: e.activation(out=ob.t[:], in_=t_.t[:], func=AF.Silu, scale=P("lng", j), bias=P("lnb", j)),
                             reads=[t_, pars], writes=[ob])
                        c_lo = s * S + tb * 512
                        k.dma("sp", lambda e, j=j: e.dma_start(out=mixT[8 + j, :, c_lo:c_lo + 512], in_=ob.t[:]),
                              reads=[ob], writes=[R("mix", c_lo // 512)], accumulate_writes=True)
            st_.close()

        if "mixT" in dbg and l == 0:
            k.dma("sp", lambda e: e.dma_start(out=dbg["mixT"][:, :, :], in_=mixT[:, :, :]), writes=[R("dbg", "mixT")])
            k.barrier()

        if "D" in STAGES:
            st_ = Stage()
            wo_v = w_out[l].rearrange("(kc p) n -> p kc n", p=128)
            wfull = st_.sb("wfull", [128, KC, D], BF16)
            mb2 = [st_.sb("mb", [128, KC, 512], BF16) for _ in range(2)]
            xt2 = [st_.sb("xt", [128, D], F32) for _ in range(2)]
            xo2 = [st_.sb("xo", [128, D], F32) for _ in range(2)]
            for nb in range(4):
                k.dma("pool", lambda e, nb=nb: e.dma_start(out=wfull.t[:, :, nb * 512:(nb + 1) * 512], in_=wo_v[:, :, nb * 512:(nb + 1) * 512]),
                      writes=[wfull], accumulate_writes=True)
            mix_v = mixT.rearrange("c p t -> p c t")
            for tb in range(T // 512):
                mb = nxt(mb2, "mb")
                k.dma("sp", lambda e: e.dma_start(out=mb.t[:], in_=mix_v[:, :, tb * 512:(tb + 1) * 512]), reads=[R("mix", tb)], writes=[mb])
                for tt in range(4):
                    r0 = tb * 512 + tt * 128
                    xt = nxt(xt2, "x")
                    k.dma("sp", lambda e: e.dma_start(out=xt.t[:], in_=x_src[r0:r0 + 128, :]), reads=[R(x_key, r0 // 128)], writes=[xt])
                    xo = nxt(xo2, "xo")
                    for nb in range(4):
                        pp = nxt(pA, "pa")
                        for c in range(KC):
                            k.op("pe", lambda e, c=c: e.matmul(pp.t[:], lhsT=mb.t[:, c, tt * 128:(tt + 1) * 128],
                                                               rhs=wfull.t[:, c, nb * 512:(nb + 1) * 512],
                                                               start=(c == 0), stop=(c == KC - 1)),
                                 reads=[mb, wfull], writes=[pp])
                        k.op("dve", lambda e, nb=nb: e.tensor_tensor(out=xo.t[:, nb * 512:(nb + 1) * 512], in0=pp.t[:],
                                                                     in1=xt.t[:, nb * 512:(nb + 1) * 512], op=ALU.add),
                             reads=[pp, xt], writes=[xo])
                    k.dma("sp", lambda e: e.dma_start(out=xres[r0:r0 + 128, :], in_=xo.t[:]), reads=[xo], writes=[R("xres", r0 // 128)])
            st_.close()
            x_src = xres
            x_key = "xres"

        if "x1" in dbg and l == 0:
            k.dma("sp", lambda e: e.dma_start(out=dbg["x1"][:, :], in_=xres[:, :]), writes=[R("dbg", "x1")])
            k.barrier()

        if "E" in STAGES:
            st_ = Stage()
            W = rmsW(st_)
            W["wb"] = [st_.sb("wb", [128, KC, 512], BF16) for _ in range(2)]
            xo2 = [st_.sb("xo", [128, D], F32) for _ in range(2)]
            memT = st_.sb("memT", [128, KC, MEM], BF16)
            kcT = st_.sb("kcT", [128, NHC, MEM], BF16)
            vcs = st_.sb("vcs", [128, 2, 512], BF16)
            ocT = st_.sb("ocT", [128, NHC, 512], BF16)
            hTb = st_.sb("hTb", [128, KC, 512], BF16)
            wq_sb = st_.sb("wq_sb", [128, KC, 512], BF16)
            qn4 = [st_.sb("qn4", [128, 512], BF16) for _ in range(NHC)]
            wo_sb = st_.sb("wo_sb", [128, NHC, D], BF16)
            wq_v = wq_c[l].rearrange("(kc p) n -> p kc n", p=128)
            wkv_v = wkv_c[l].rearrange("(kc p) n -> p kc n", p=128)
            woc_v = wo_c[l].rearrange("(kc p) n -> p kc n", p=128)
            k.dma("pool", lambda e: e.dma_start(out=wq_sb.t[:], in_=wq_v[:, :, :]), writes=[wq_sb])
            k.dma("pool", lambda e: e.dma_start(out=wo_sb.t[:], in_=woc_v[:, :, :]), writes=[wo_sb])
            for s in range(NSEQ):
                for mt in range(2):
                    r0 = s * MEM + mt * 128
                    rms_tile(W, mem_in[r0:r0 + 128, :], R("memin"), P("gmemT", 0, 16), memT, mt * 128)
                wk_ = load_w(W, wkv_v, 0)
                for h in range(NHC):
                    pp = nxt(pA, "pa")
                    for kc in range(KC):
                        k.op("pe", lambda e, kc=kc: e.matmul(pp.t[:, 0:MEM], lhsT=wk_.t[:, kc, h * 128:(h + 1) * 128], rhs=memT.t[:, kc, :],
                                                             start=(kc == 0), stop=(kc == KC - 1)), reads=[wk_, memT], writes=[pp])
                    ob = fm_norm(pp, P("gkc"), 1.0 / 128, EPS, MEM, ones_f)
                    k.op("dve", lambda e: e.tensor_copy(out=kcT.t[:, h, :], in_=ob.t[:, 0:MEM]), reads=[ob], writes=[kcT])
                wv_ = load_w(W, wkv_v, 512)
                for mt in range(2):
                    pp = nxt(pA, "pa")
                    for kc in range(KC):
                        k.op("pe", lambda e, kc=kc: e.matmul(pp.t[:], lhsT=memT.t[:, kc, mt * 128:(mt + 1) * 128], rhs=wv_.t[:, kc, :],
                                                             start=(kc == 0), stop=(kc == KC - 1)), reads=[wv_, memT], writes=[pp])
                    k.op("act", lambda e: e.activation(out=vcs.t[:, mt, :], in_=pp.t[:], func=AF.Copy), reads=[pp], writes=[vcs])
                for tb in range(S // 512):
                    for tt in range(4):
                        r0 = s * S + tb * 512 + tt * 128
                        rms_tile(W, xres[r0:r0 + 128, :], R("xres", r0 // 128), P("gcrossT", 0, 16), hTb, tt * 128)
                    for h in range(NHC):
                        pp = pA[h]
                        for kc in range(KC):
                            k.op("pe", lambda e, kc=kc, pp=pp: e.matmul(pp.t[:], lhsT=wq_sb.t[:, kc, h * 128:(h + 1) * 128], rhs=hTb.t[:, kc, :],
                                                                        start=(kc == 0), stop=(kc == KC - 1)), reads=[wq_sb, hTb], writes=[pp])
                    for h0 in (0, 2):
                        hs = (h0, h0 + 1)
                        for h in hs:
                            k.op("act", lambda e, h=h: e.activation(out=f512[h].t[:], in_=pA[h].t[:], func=AF.Square), reads=[pA[h]], writes=[f512[h]])
                        for h in hs:
                            k.op("pe", lambda e, h=h: e.matmul(pA[4 + h % 2].t[:], lhsT=ones_f, rhs=f512[h].t[:], start=True, stop=True),
                                 reads=[f512[h], cst], writes=[pA[4 + h % 2]])
                        for h in hs:
                            k.op("act", lambda e, h=h: e.activation(out=f512[h].t[:], in_=pA[4 + h % 2].t[:], func=AF.Sqrt, scale=1.0, bias=128 * EPS),
                                 reads=[pA[4 + h % 2]], writes=[f512[h]])
                        for h in hs:
                            k.op("dve", lambda e, h=h: e.reciprocal(out=f512[h].t[:], in_=f512[h].t[:]), reads=[f512[h]], writes=[f512[h]])
                        for h in hs:
                            k.op("dve", lambda e, h=h: e.scalar_tensor_tensor(out=qn4[h].t[:], in0=pA[h].t[:], scalar=P("gqc"), in1=f512[h].t[:],
                                                                              op0=ALU.mult, op1=ALU.mult),
                                 reads=[pA[h], f512[h], pars], writes=[qn4[h]])
                    for h in range(NHC):
                        pO = pA[(h % 2) * 2]
                        pL = pA[(h % 2) * 2 + 1]
                        sTs = [pA[4], pA[5]]
                        pts = [nxt(b512, "b"), nxt(b512, "b")]
                        for mt in range(2):
                            k.op("pe", lambda e, mt=mt: e.matmul(sTs[mt].t[:], lhsT=kcT.t[:, h, mt * 128:(mt + 1) * 128], rhs=qn4[h].t[:], start=True, stop=True),
                                 reads=[kcT, qn4[h]], writes=[sTs[mt]])
                        for mt in range(2):
                            k.op("act", lambda e, mt=mt: e.activation(out=pts[mt].t[:], in_=sTs[mt].t[:], func=AF.Exp), reads=[sTs[mt]], writes=[pts[mt]])
                        for mt in range(2):
                            k.op("pe", lambda e, mt=mt: e.matmul(pO.t[:], lhsT=vcs.t[:, mt, h * 128:(h + 1) * 128], rhs=pts[mt].t[:], start=(mt == 0), stop=(mt == 1)),
                                 reads=[vcs, pts[mt]], writes=[pO])
                            k.op("pe", lambda e, mt=mt: e.matmul(pL.t[:], lhsT=ones_bf.t[:], rhs=pts[mt].t[:], start=(mt == 0), stop=(mt == 1)),
                                 reads=[ones_bf, pts[mt]], writes=[pL])
                        r_ = f512[4 + h % 2]
                        k.op("dve", lambda e: e.reciprocal(out=r_.t[:], in_=pL.t[:]), reads=[pL], writes=[r_])
                        k.op("dve", lambda e: e.tensor_tensor(out=ocT.t[:, h, :], in0=pO.t[:], in1=r_.t[:], op=ALU.mult),
                             reads=[pO, r_], writes=[ocT])
                    for tt in range(4):
                        r0 = s * S + tb * 512 + tt * 128
                        xt = nxt(W["xt"], "x")
                        k.dma("sp", lambda e: e.dma_start(out=xt.t[:], in_=xres[r0:r0 + 128, :]), reads=[R("xres", r0 // 128)], writes=[xt])
                        xo = nxt(xo2, "xo")
                        for nb in range(4):
                            pp = nxt(pA, "pa")
                            for h in range(NHC):
                                k.op("pe", lambda e, h=h: e.matmul(pp.t[:], lhsT=ocT.t[:, h, tt * 128:(tt + 1) * 128],
                                                                   rhs=wo_sb.t[:, h, nb * 512:(nb + 1) * 512],
                                                                   start=(h == 0), stop=(h == NHC - 1)), reads=[ocT, wo_sb], writes=[pp])
                            k.op("dve", lambda e, nb=nb: e.tensor_tensor(out=xo.t[:, nb * 512:(nb + 1) * 512], in0=pp.t[:],
                                                                         in1=xt.t[:, nb * 512:(nb + 1) * 512], op=ALU.add),
                                 reads=[pp, xt], writes=[xo])
                        k.dma("sp", lambda e: e.dma_start(out=xres[r0:r0 + 128, :], in_=xo.t[:]), reads=[xo], writes=[R("xres", r0 // 128)])
            st_.close()

        if "x2" in dbg and l == 0:
            k.dma("sp", lambda e: e.dma_start(out=dbg["x2"][:, :], in_=xres[:, :]), writes=[R("dbg", "x2")])
            k.barrier()

        if "F" in STAGES:
            gates = LS.sb("gates", [128, NT, 2], F32)
            desti = LS.sb("desti", [128, NT * 2], I32)
            widx = LS.sb("widx", [128, NB], I32)
            st_ = Stage()
            xt2 = [st_.sb("xt", [128, D], F32) for _ in range(2)]
            hb2 = [st_.sb("hb", [128, D], BF16) for _ in range(2)]
            junk = st_.sb("junk", [128, D], BF16)
            gffn = st_.sb("gffn", [128, D], F32)
            wr = st_.sb("wr", [128, KC, 72], BF16)
            hTt = st_.sb("hTt", [128, KC, 128], BF16)
            OH = st_.sb("OH", [128, NT, 2, NE], F32)
            RANK = st_.sb("RANK", [128, NT, NE], F32)
            cum = st_.sb("cum", [128, NE], F32)
            rt = [st_.sb("rt", [128, 128], F32) for i in range(6)]
            Mb = st_.sb("Mb", [128, NE], BF16)
            destf = st_.sb("destf", [128, NT * 2], F32)
            pcs = [st_.sb("pcs", [128, NE], F32) for i in range(3)]
            pci = st_.sb("pci", [128, NE], I32)
            thr = st_.sb("thr_sb", [128, NB], F32)
            cmpb = st_.sb("cmpb", [128, NB, NE], F32)
            bef = st_.sb("bef", [128, NB], F32)
            k.dma("sp", lambda e: e.dma_start(out=thr.t[:], in_=thr_in[:, :]), writes=[thr])
            k.dma("sp", lambda e: e.dma_start(out=gffn.t[:], in_=gffn_in[l, :, :]), writes=[gffn])
            k.dma("pool", lambda e: e.dma_start(out=wr.t[:], in_=w_rt[l].rearrange("(kc p) n -> p kc n", p=128)), writes=[wr])
            k.op("dve", lambda e: e.memset(cum.t[:], 0.0), writes=[cum])
            o_brt = _off["brt"][0]
            for tt in range(NT):
                r0 = tt * 128
                xt = nxt(xt2, "x")
                k.dma("sp", lambda e: e.dma_start(out=xt.t[:], in_=xres[r0:r0 + 128, :]), reads=[R("xres", tt)], writes=[xt])
                c0 = nxt(col, "c")
                k.op("act", lambda e: e.activation(out=junk.t[:], in_=xt.t[:], func=AF.Square, accum_out=c0.t[:, 0:1]),
                     reads=[xt], writes=[junk, c0])
                k.op("act", lambda e: e.activation(out=c0.t[:, 1:2], in_=c0.t[:, 0:1], func=AF.Sqrt, scale=1.0 / D, bias=EPS), reads=[c0], writes=[c0])
                k.op("dve", lambda e: e.reciprocal(out=c0.t[:, 2:3], in_=c0.t[:, 1:2]), reads=[c0], writes=[c0])
                hb = nxt(hb2, "hb")
                k.op("dve", lambda e: e.scalar_tensor_tensor(out=hb.t[:], in0=xt.t[:], scalar=c0.t[:, 2:3], in1=gffn.t[:],
                                                             op0=ALU.mult, op1=ALU.mult), reads=[xt, c0, gffn], writes=[hb])
                k.dma("sp", lambda e: e.dma_start(out=Hrows[r0:r0 + 128, :], in_=hb.t[:]), reads=[hb], writes=[R("hrows", tt)])
                for kc in range(KC):
                    k.op("pe", lambda e, kc=kc: e.transpose(out=pT.t[:, kc, :], in_=hb.t[:, kc * 128:(kc + 1) * 128], identity=ident_bf.t[:]),
                         reads=[hb, ident_bf], writes=[pT])
                k.op("act", lambda e: e.activation(out=hTt.t[:], in_=pT.t[:], func=AF.Copy), reads=[pT], writes=[hTt])
                pp = nxt(pA, "pa")
                for kc in range(KC):
                    k.op("pe", lambda e, kc=kc: e.matmul(pp.t[:, 0:72], lhsT=hTt.t[:, kc, :], rhs=wr.t[:, kc, :], start=(kc == 0), stop=(kc == KC - 1)),
                         reads=[hTt, wr], writes=[pp])
                lg, w1_, w2_, w3_, w4_, w5_ = rt
                k.op("dve", lambda e: e.tensor_tensor(out=lg.t[:, 0:72], in0=pp.t[:, 0:72], in1=pars.t[:, o_brt:o_brt + 72], op=ALU.add),
                     reads=[pp, pars], writes=[lg])
                k.op("dve", lambda e: e.tensor_reduce(out=w1_.t[:, 0:1], in_=lg.t[:, 0:8], axis=AX.X, op=ALU.max), reads=[lg], writes=[w1_])
                k.op("dve", lambda e: e.tensor_scalar(out=w2_.t[:, 0:8], in0=lg.t[:, 0:8], scalar1=w1_.t[:, 0:1], scalar2=None, op0=ALU.is_equal),
                     reads=[lg, w1_], writes=[w2_])
                k.op("dve", lambda e: e.tensor_scalar(out=w1_.t[:, 1:2], in0=w1_.t[:, 0:1], scalar1=-1.0, scalar2=None, op0=ALU.mult),
                     reads=[w1_], writes=[w1_])
                k.op("act", lambda e: e.activation(out=w2_.t[:, 8:16], in_=lg.t[:, 0:8], func=AF.Exp, bias=w1_.t[:, 1:2], accum_out=w1_.t[:, 2:3]),
                     reads=[lg, w1_], writes=[w2_, w1_])
                k.op("dve", lambda e: e.reciprocal(out=w1_.t[:, 3:4], in_=w1_.t[:, 2:3]), reads=[w1_], writes=[w1_])
                k.op("dve", lambda e: e.tensor_tensor(out=w3_.t[:, 0:64].rearrange("p (g j) -> p g j", j=8),
                                                      in0=lg.t[:, 8:72].rearrange("p (g j) -> p g j", j=8),
                                                      in1=w2_.t[:, 0:8].unsqueeze(2).to_broadcast([128, 8, 8]), op=ALU.mult),
                     reads=[lg, w2_], writes=[w3_])
                k.op("dve", lambda e: e.tensor_reduce(out=w4_.t[:, 0:8], in_=w3_.t[:, 0:64].rearrange("p (g j) -> p j g", j=8), axis=AX.X, op=ALU.add),
                     reads=[w3_], writes=[w4_])
                k.op("dve", lambda e: e.tensor_reduce(out=w1_.t[:, 4:5], in_=w4_.t[:, 0:8], axis=AX.X, op=ALU.max), reads=[w4_], writes=[w1_])
                k.op("dve", lambda e: e.tensor_scalar(out=w4_.t[:, 8:16], in0=w4_.t[:, 0:8], scalar1=w1_.t[:, 4:5], scalar2=None, op0=ALU.is_equal),
                     reads=[w4_, w1_], writes=[w4_])
                k.op("dve", lambda e: e.scalar_tensor_tensor(out=w4_.t[:, 16:24], in0=w4_.t[:, 8:16], scalar=-1e30, in1=w4_.t[:, 0:8],
                                                             op0=ALU.mult, op1=ALU.add), reads=[w4_], writes=[w4_])
                k.op("dve", lambda e: e.tensor_reduce(out=w1_.t[:, 5:6], in_=w4_.t[:, 16:24], axis=AX.X, op=ALU.max), reads=[w4_], writes=[w1_])
                k.op("dve", lambda e: e.tensor_scalar(out=w4_.t[:, 24:32], in0=w4_.t[:, 16:24], scalar1=w1_.t[:, 5:6], scalar2=None, op0=ALU.is_equal),
                     reads=[w4_, w1_], writes=[w4_])
                k.op("dve", lambda e: e.tensor_tensor(out=w1_.t[:, 6:7], in0=w1_.t[:, 5:6], in1=w1_.t[:, 4:5], op=ALU.subtract), reads=[w1_], writes=[w1_])
                k.op("act", lambda e: e.activation(out=w1_.t[:, 7:8], in_=w1_.t[:, 6:7], func=AF.Exp), reads=[w1_], writes=[w1_])
                k.op("dve", lambda e: e.tensor_scalar(out=w1_.t[:, 8:9], in0=w1_.t[:, 7:8], scalar1=1.0, scalar2=None, op0=ALU.add), reads=[w1_], writes=[w1_])
                k.op("dve", lambda e: e.reciprocal(out=w1_.t[:, 9:10], in_=w1_.t[:, 8:9]), reads=[w1_], writes=[w1_])
                k.op("dve", lambda e: e.tensor_tensor(out=gates.t[:, tt, 0:1], in0=w1_.t[:, 3:4], in1=w1_.t[:, 9:10], op=ALU.mult), reads=[w1_], writes=[gates])
                k.op("dve", lambda e: e.tensor_tensor(out=gates.t[:, tt, 1:2], in0=gates.t[:, tt, 0:1], in1=w1_.t[:, 7:8], op=ALU.mult), reads=[w1_], writes=[gates])
                for kk in range(2):
                    k.op("dve", lambda e, kk=kk: e.tensor_tensor(
                        out=OH.t[:, tt, kk, :].rearrange("p (g j) -> p g j", j=8),
                        in0=w2_.t[:, 0:8].unsqueeze(2).to_broadcast([128, 8, 8]),
                        in1=w4_.t[:, 8 + 16 * kk:16 + 16 * kk].unsqueeze(1).to_broadcast([128, 8, 8]), op=ALU.mult),
                         reads=[w2_, w4_], writes=[OH])
                k.op("dve", lambda e: e.tensor_tensor(out=Mb.t[:], in0=OH.t[:, tt, 0, :], in1=OH.t[:, tt, 1, :], op=ALU.add), reads=[OH], writes=[Mb])
                pr = nxt(pA, "pa")
                k.op("pe", lambda e: e.matmul(pr.t[:, 0:NE], lhsT=ltri_bf.t[:], rhs=Mb.t[:], start=True, stop=True), reads=[ltri_bf, Mb], writes=[pr])
                k.op("dve", lambda e: e.tensor_tensor(out=RANK.t[:, tt, :], in0=pr.t[:, 0:NE], in1=cum.t[:], op=ALU.add), reads=[pr, cum], writes=[RANK])
                pc_ = nxt(pA, "pa")
                k.op("pe", lambda e: e.matmul(pc_.t[:, 0:NE], lhsT=ones_bf.t[:], rhs=Mb.t[:], start=True, stop=True), reads=[ones_bf, Mb], writes=[pc_])
                k.op("dve", lambda e: e.tensor_tensor(out=cum.t[:], in0=cum.t[:], in1=pc_.t[:, 0:NE], op=ALU.add), reads=[pc_], writes=[cum])
            k.op("dve", lambda e: e.tensor_scalar(out=pcs[0].t[:], in0=cum.t[:], scalar1=float(BLK - 1), scalar2=None, op0=ALU.add), reads=[cum], writes=[pcs[0]])
            k.op("dve", lambda e: e.tensor_copy(out=pci.t[:], in_=pcs[0].t[:]), reads=[pcs[0]], writes=[pci])
            sh = int(math.log2(BLK))
            k.op("dve", lambda e: e.tensor_single_scalar(out=pci.t[:], in_=pci.t[:], scalar=sh, op=ALU.arith_shift_right), writes=[pci])
            k.op("dve", lambda e: e.tensor_single_scalar(out=pci.t[:], in_=pci.t[:], scalar=sh, op=ALU.logical_shift_left), writes=[pci])
            k.op("dve", lambda e: e.tensor_copy(out=pcs[0].t[:], in_=pci.t[:]), reads=[pci], writes=[pcs[0]])
            k.op("dve", lambda e: e.tensor_copy(out=pcs[1].t[:], in_=pcs[0].t[:]), reads=[pcs[0]], writes=[pcs[1]])
            cur, oth = pcs[1], pcs[2]
            stp = 1
            while stp < NE:
                k.op("dve", lambda e, stp=stp, cur=cur, oth=oth: e.tensor_copy(out=oth.t[:, 0:stp], in_=cur.t[:, 0:stp]), reads=[cur], writes=[oth])
                k.op("dve", lambda e, stp=stp, cur=cur, oth=oth: e.tensor_tensor(out=oth.t[:, stp:NE], in0=cur.t[:, stp:NE], in1=cur.t[:, 0:NE - stp], op=ALU.add),
                     reads=[cur], writes=[oth])
                cur, oth = oth, cur
                stp *= 2
            pend = cur
            pstart = oth
            k.op("dve", lambda e: e.tensor_tensor(out=pstart.t[:], in0=pend.t[:], in1=pcs[0].t[:], op=ALU.subtract), reads=[pend, pcs[0]], writes=[pstart])
            for tt in range(NT):
                k.op("dve", lambda e, tt=tt: e.tensor_tensor(out=RANK.t[:, tt, :], in0=RANK.t[:, tt, :], in1=pstart.t[:], op=ALU.add), reads=[pstart], writes=[RANK])
                for kk in range(2):
                    k.op("dve", lambda e, tt=tt, kk=kk: e.tensor_tensor(out=rt[2].t[:, 0:NE], in0=RANK.t[:, tt, :], in1=OH.t[:, tt, kk, :], op=ALU.mult),
                         reads=[RANK, OH], writes=[rt[2]])
                    k.op("dve", lambda e, tt=tt, kk=kk: e.tensor_reduce(out=destf.t[:, tt * 2 + kk:tt * 2 + kk + 1], in_=rt[2].t[:, 0:NE], axis=AX.X, op=ALU.add),
                         reads=[rt[2]], writes=[destf])
            k.op("dve", lambda e: e.tensor_copy(out=desti.t[:], in_=destf.t[:]), reads=[destf], writes=[desti])
            k.op("dve", lambda e: e.tensor_tensor(out=cmpb.t[:], in0=pend.t[:].unsqueeze(1).to_broadcast([128, NB, NE]),
                                                  in1=thr.t[:].unsqueeze(2).to_broadcast([128, NB, NE]), op=ALU.is_le), reads=[pend, thr], writes=[cmpb])
            k.op("dve", lambda e: e.tensor_reduce(out=bef.t[:], in_=cmpb.t[:], axis=AX.X, op=ALU.add), reads=[cmpb], writes=[bef])
            k.op("dve", lambda e: e.tensor_scalar(out=bef.t[:], in0=bef.t[:], scalar1=128.0, scalar2=None, op0=ALU.mult), writes=[bef])
            k.op("dve", lambda e: e.tensor_scalar(out=bef.t[:], in0=bef.t[:], scalar1=iota_p, scalar2=None, op0=ALU.add), reads=[cst], writes=[bef])
            k.op("dve", lambda e: e.tensor_copy(out=widx.t[:], in_=bef.t[:]), reads=[bef], writes=[widx])

            k.op("dve", lambda e: e.memset(junk.t[:], 0.0), writes=[junk])
            RPP = PR // 128
            zc = 24
            xs_v = Xs.rearrange("(p r) d -> p r d", p=128)
            for z0 in range(0, RPP, zc):
                zn = min(zc, RPP - z0)
                k.dma("sp", lambda e, z0=z0, zn=zn: e.dma_start(out=xs_v[:, z0:z0 + zn, :],
                                                                 in_=junk.t[:].unsqueeze(1).to_broadcast([128, zn, D])),
                      reads=[junk], writes=[R("xs")], accumulate_writes=True)
            k.barrier()
            for tt in range(NT):
                hb = nxt(hb2, "hb")
                k.dma("sp", lambda e: e.dma_start(out=hb.t[:], in_=Hrows[tt * 128:(tt + 1) * 128, :]), reads=[R("hrows", tt)], writes=[hb])
                for kk in range(2):
                    k.dma("pool", lambda e, kk=kk: e.indirect_dma_start(
                        out=Xs[:, :], out_offset=bass.IndirectOffsetOnAxis(ap=desti.t[:, tt * 2 + kk:tt * 2 + kk + 1], axis=0),
                        in_=hb.t[:], in_offset=None), reads=[hb, desti], writes=[R("xs")], accumulate_writes=True)

            if "moe" in dbg and l == 0:
                k.dma("sp", lambda e: e.dma_start(out=dbg["moe"][:, 0:NT * 2], in_=destf.t[:]), reads=[destf], writes=[R("dbg", "moe")])
                k.dma("sp", lambda e: e.dma_start(out=dbg["moe"][:, NT * 2:NT * 2 + NB], in_=bef.t[:]), reads=[bef], writes=[R("dbg", "moe2")])
                k.dma("sp", lambda e: e.dma_start(out=dbg["moe"][:, NT * 2 + NB:NT * 4 + NB], in_=gates.t[:].rearrange("p t k -> p (t k)")),
                      reads=[gates], writes=[R("dbg", "moe3")])
            st_.close()

            st_ = Stage()
            yo2 = [st_.sb("yo", [128, D], F32) for _ in range(2)]
            xsb = [st_.sb("xsb", [128, 2, D], BF16) for i in range(2)]
            xTb = st_.sb("xTb", [128, KC, BLK], BF16)
            hid = st_.sb("hid", [128, FC, BLK], BF16)
            w1b = [st_.sb("w1b", [128, KC, DE], BF16) for i in range(2)]
            w3b = [st_.sb("w3b", [128, KC, DE], BF16) for i in range(2)]
            w2b = [st_.sb("w2b", [128, FC, D], BF16) for i in range(2)]

            bc_reg = nc.gpsimd.to_reg(NE * 128 - 1)

            def load_block(b):
                i = b % 2
                k.dma("sp", lambda e: e.dma_start(out=xsb[i].t[:], in_=Xs[b * BLK:(b + 1) * BLK, :].rearrange("(r p) d -> p r d", p=128)),
                      reads=[R("xs")], writes=[xsb[i]])
                for wsb, wdr, nch, g in ((w1b[i], w1r, NCH, GK), (w3b[i], w3r, NCH, GK), (w2b[i], w2r, FC, 1)):
                    for c in range(nch):
                        k.dma("pool", lambda e, wsb=wsb, wdr=wdr, c=c, g=g: e.indirect_dma_start(
                            out=wsb.t[:, c * g:(c + 1) * g, :].rearrange("p a b -> p (a b)"), out_offset=None, in_=wdr[l][c][:, :],
                            in_offset=bass.IndirectOffsetOnAxis(ap=widx.t[:, b:b + 1], axis=0),
                            bounds_check=bc_reg, oob_is_err=False), reads=[widx], writes=[wsb],
                              accumulate_writes=(c > 0))

            load_block(0)
            for b in range(NB):
                i = b % 2
                if b + 1 < NB:
                    load_block(b + 1)
                for r in range(2):
                    for kc in range(KC):
                        k.op("pe", lambda e, kc=kc, r=r: e.transpose(out=pT.t[:, kc, :], in_=xsb[i].t[:, r, kc * 128:(kc + 1) * 128], identity=ident_bf.t[:]),
                             reads=[xsb[i], ident_bf], writes=[pT])
                    k.op("act", lambda e, r=r: e.activation(out=xTb.t[:, :, r * 128:(r + 1) * 128], in_=pT.t[:], func=AF.Copy), reads=[pT], writes=[xTb])
                for fc in range(FC):
                    p1 = nxt(pA, "pa")
                    p3 = nxt(pA, "pa")
                    for kc in range(KC):
                        k.op("pe", lambda e, kc=kc: e.matmul(p1.t[:, 0:BLK], lhsT=w1b[i].t[:, kc, fc * 128:(fc + 1) * 128], rhs=xTb.t[:, kc, :],
                                                             start=(kc == 0), stop=(kc == KC - 1)), reads=[w1b[i], xTb], writes=[p1])
                    for kc in range(KC):
                        k.op("pe", lambda e, kc=kc: e.matmul(p3.t[:, 0:BLK], lhsT=w3b[i].t[:, kc, fc * 128:(fc + 1) * 128], rhs=xTb.t[:, kc, :],
                                                             start=(kc == 0), stop=(kc == KC - 1)), reads=[w3b[i], xTb], writes=[p3])
                    sg = nxt(f512, "f")
                    k.op("act", lambda e: e.activation(out=sg.t[:, 0:BLK], in_=p1.t[:, 0:BLK], func=AF.Silu), reads=[p1], writes=[sg])
                    k.op("dve", lambda e: e.tensor_tensor(out=hid.t[:, fc, :], in0=p3.t[:, 0:BLK], in1=sg.t[:, 0:BLK], op=ALU.mult),
                         reads=[p3, sg], writes=[hid])
                for r in range(2):
                    yo = nxt(yo2, "yo")
                    for nb in range(4):
                        pp = nxt(pA, "pa")
                        for fc in range(FC):
                            k.op("pe", lambda e, fc=fc: e.matmul(pp.t[:], lhsT=hid.t[:, fc, r * 128:(r + 1) * 128], rhs=w2b[i].t[:, fc, nb * 512:(nb + 1) * 512],
                                                                 start=(fc == 0), stop=(fc == FC - 1)), reads=[hid, w2b[i]], writes=[pp])
                        if nb % 2 == 0:
                            k.op("act", lambda e, nb=nb: e.activation(out=yo.t[:, nb * 512:(nb + 1) * 512], in_=pp.t[:], func=AF.Copy), reads=[pp], writes=[yo])
                        else:
                            k.op("dve", lambda e, nb=nb: e.tensor_copy(out=yo.t[:, nb * 512:(nb + 1) * 512], in_=pp.t[:]), reads=[pp], writes=[yo])
                    rr0 = b * BLK + r * 128
                    k.dma("sp", lambda e: e.dma_start(out=Ys[rr0:rr0 + 128, :], in_=yo.t[:]), reads=[yo], writes=[R("ys")], accumulate_writes=True)
            st_.close()

            st_ = Stage()
            xt2 = [st_.sb("xt", [128, D], F32) for _ in range(2)]
            y02 = [st_.sb("y0", [128, D], F32) for _ in range(2)]
            y12 = [st_.sb("y1", [128, D], F32) for _ in range(2)]
            dst = out if last else xres
            for tt in range(NT):
                r0 = tt * 128
                xt = nxt(xt2, "x")
                k.dma("sp", lambda e: e.dma_start(out=xt.t[:], in_=xres[r0:r0 + 128, :]), reads=[R("xres", tt)], writes=[xt])
                ys = [nxt(y02, "y0"), nxt(y12, "y1")]
                for kk in range(2):
                    k.dma("pool", lambda e, kk=kk: e.indirect_dma_start(
                        out=ys[kk].t[:], out_offset=None, in_=Ys[:, :],
                        in_offset=bass.IndirectOffsetOnAxis(ap=desti.t[:, tt * 2 + kk:tt * 2 + kk + 1], axis=0)),
                          reads=[R("ys"), desti], writes=[ys[kk]])
                k.op("dve", lambda e: e.scalar_tensor_tensor(out=xt.t[:], in0=ys[0].t[:], scalar=gates.t[:, tt, 0:1], in1=xt.t[:],
                                                             op0=ALU.mult, op1=ALU.add), reads=[ys[0], gates], writes=[xt])
                k.op("dve", lambda e: e.scalar_tensor_tensor(out=xt.t[:], in0=ys[1].t[:], scalar=gates.t[:, tt, 1:2], in1=xt.t[:],
                                                             op0=ALU.mult, op1=ALU.add), reads=[ys[1], gates], writes=[xt])
                k.dma("sp", lambda e: e.dma_start(out=dst[r0:r0 + 128, :], in_=xt.t[:]), reads=[xt],
                      writes=[R("out" if last else "xres", tt)])
            st_.close()
        LS.close()

    k.barrier()
    return nc, k


def t5_bucket_np(dist):
    max_exact = 16
    d_f = np.maximum(dist, 1).astype(np.float32)
    large = max_exact + (np.log(d_f / np.float32(max_exact)) / np.float32(math.log(128 / max_exact)) * np.float32(16)).astype(np.int32)
    large = np.minimum(large, 31)
    return np.where(dist < max_exact, dist, large)


def host_prep(inp, L, S, DE):
    f = lambda a: np.ascontiguousarray(np.asarray(a, dtype=np.float32))
    rep = lambda v: np.broadcast_to(np.asarray(v, np.float32).reshape(1, -1), (128, np.asarray(v).size))
    pars = np.zeros((L, 128, NPAR), np.float32)

    def put(l, name, arr):
        o, w = _off[name]
        pars[l, :, o:o + w] = arr.reshape(128, w)

    tbl = np.asarray(inp["rel_bias_table"], np.float32)
    for l in range(L):
        put(l, "gmixT", np.asarray(inp["g_mix"][l]).reshape(KC, 128).T)
        put(l, "gcrossT", np.asarray(inp["g_cross"][l]).reshape(KC, 128).T)
        put(l, "gmemT", np.asarray(inp["g_mem"][l]).reshape(KC, 128).T)
        put(l, "gq2", np.tile(np.asarray(inp["g_q"][l]), 2))
        put(l, "gk2", np.tile(np.asarray(inp["g_k"][l]), 2))
        put(l, "gsub", np.asarray(inp["g_subln"][l]))
        put(l, "gqc", np.asarray(inp["g_qc"][l]))
        put(l, "gkc", np.asarray(inp["g_kc"][l]))
        cw = np.asarray(inp["conv_w"][l])
        put(l, "convw", cw.reshape(CW, 8, 128).transpose(2, 1, 0))
        put(l, "convb", np.asarray(inp["conv_b"][l]).reshape(8, 128).T)
        put(l, "lng", np.asarray(inp["conv_ln_g"][l]).reshape(8, 128).T)
        put(l, "lnb", np.asarray(inp["conv_ln_b"][l]).reshape(8, 128).T)
        put(l, "dl", rep(np.asarray(inp["diff_lambda"][l]).reshape(-1)))
        put(l, "brt", rep(np.concatenate([np.asarray(inp["b_group"][l]), np.asarray(inp["b_router"][l])])))
        put(l, "relc", rep(tbl[31, :]))
    gffnb = np.stack([rep(inp["g_ffn"][l]) for l in range(L)]).astype(np.float32)
    kk_, qq_ = np.meshgrid(np.arange(128), np.arange(128), indexing="ij")
    biasT = np.zeros((128, NH * 2, 128), np.float32)
    for dlt in range(2):
        dist = dlt * 128 + qq_ - kk_
        bk = t5_bucket_np(np.maximum(dist, 0))
        for h in range(NH):
            biasT[:, h * 2 + dlt, :] = np.where(dist >= 0, tbl[bk, h], np.float32(MASKV))
    consts = np.zeros((128, 5, 128), np.float32)
    consts[:, 0, :] = np.eye(128)
    consts[:, 1, :] = 1.0
    consts[0:64, 2, 0:64] = 1.0
    consts[64:128, 2, 64:128] = 1.0
    consts[:, 3, :] = (kk_ < qq_)
    consts[:, 4, :] = np.arange(128).reshape(128, 1)
    w_rt = np.concatenate([np.asarray(inp["w_group"], np.float32)[:L], np.asarray(inp["w_router"], np.float32)[:L]], axis=-1)
    FC = DE // 128
    shared = {
        "pars": pars, "gffnb": gffnb, "biasT": biasT, "consts": consts,
        "w_in": f(inp["w_in"][:L]), "w_out": f(inp["w_out"][:L]), "wq_c": f(inp["wq_c"][:L]), "wkv_c": f(inp["wkv_c"][:L]),
        "wo_c": f(inp["wo_c"][:L]), "w_rt": f(w_rt),
    }
    if "F" in STAGES:
        NCH = KC * DE // 2048
        GK = 2048 // DE
        for nm, src_ in (("w1r", "w1"), ("w3r", "w3")):
            for l in range(L):
                a6 = np.asarray(inp[src_][l], np.float32).reshape(NE, NCH, GK, 128, DE)
                for c in range(NCH):
                    shared[f"{nm}_{l}_{c}"] = f(a6[:, c].transpose(0, 2, 1, 3).reshape(NE * 128, GK * DE))
        for l in range(L):
            a5 = np.asarray(inp["w2"][l], np.float32).reshape(NE, FC, 128, D)
            for c in range(FC):
                shared[f"w2r_{l}_{c}"] = f(a5[:, c].reshape(NE * 128, D))
    return shared


_CACHE = {}


def run(inp, L, NSEQ, S, DE, debug=()):
    key = (L, NSEQ, S, DE, tuple(debug))
    if key not in _CACHE:
        _CACHE[key] = build_program(L, NSEQ, S, DE, debug)
    nc, kb = _CACHE[key]
    shared = host_prep(inp, L, S, DE)
    T = NSEQ * S
    NB = -(-(2 * T + NE * (BLK - 1)) // BLK)
    shared["thr"] = np.ascontiguousarray(np.broadcast_to((np.arange(NB, dtype=np.float32) * BLK).reshape(1, NB), (128, NB)))
    x = np.asarray(inp["x"], np.float32)
    mem = np.asarray(inp["mem"], np.float32)
    in_maps = []
    for c in range(NCORES):
        m = dict(shared)
        m["x"] = np.ascontiguousarray(x[c * NSEQ:(c + 1) * NSEQ].reshape(T, D))
        m["mem"] = np.ascontiguousarray(mem[c * NSEQ:(c + 1) * NSEQ].reshape(NSEQ * MEM, D))
        in_maps.append(m)
    res = run_bass_kernel_spmd(nc, in_maps, core_ids=list(range(NCORES)))
    return res


def kernel(**inputs):
    L, NSEQ, S, DE = 2, 2, 2048, 512
    res = run(inputs, L, NSEQ, S, DE)
    outs = [r["out"].reshape(NSEQ, S, D) for r in res.results]
    return np.concatenate(outs, axis=0).astype(np.float32)
```

```python
import math
from contextlib import ExitStack

import numpy as np
import concourse.bass as bass
import concourse.mybir as mybir
from concourse.bass_utils import run_bass_kernel_spmd

AF = mybir.ActivationFunctionType
ALU = mybir.AluOpType
AX = mybir.AxisListType
F32 = mybir.dt.float32
BF16 = mybir.dt.bfloat16
I32 = mybir.dt.int32

D = 2048
KC = D // 128
DQ = 64
NH = 8
D_IN = 5120
CW = 31
MEM = 256
NHC = 4
NG = 8
NE = 64
BLK = 256
EPS = 1e-6
NCORES = 8
MASKV = -30000.0
STAGES = "ABCDEF"

_off = {}
_n = 0
for _name, _w in [("gmixT", 16), ("gcrossT", 16), ("gmemT", 16), ("gq2", 1), ("gk2", 1), ("gsub", 1),
                  ("gqc", 1), ("gkc", 1), ("convw", 8 * CW), ("convb", 8), ("lng", 8), ("lnb", 8),
                  ("dl", 4 * DQ), ("brt", 72), ("relc", 8)]:
    _off[_name] = (_n, _w)
    _n += _w
NPAR = _n


class Buf:
    __slots__ = ("t", "w", "r")

    def __init__(self, t):
        self.t = t
        self.w = []
        self.r = []


class KB:
    NRING = 64

    def __init__(self, nc, es):
        self.nc = nc
        self.es = es
        self.eng = {"pe": nc.tensor, "dve": nc.vector, "act": nc.scalar, "pool": nc.gpsimd, "sp": nc.sync}
        self.chan = {}
        self.waited = {e: {} for e in self.eng}
        self.ring = [es.enter_context(nc.semaphore(f"dq{i}")) for i in range(self.NRING)]
        self.ring_cnt = [0] * self.NRING
        self.ring_i = 0
        self.nsem = 0
        self.ninstr = 0

    def _chan(self, e):
        c = self.chan.get(e)
        if c is None or c[2] >= 30000:
            self.nsem += 1
            c = [f"c{e}{self.nsem}", self.es.enter_context(self.nc.semaphore(f"c_{e}_{self.nsem}")), 0]
            self.chan[e] = c
        return c

    def _wait(self, e, toks):
        wd = self.waited[e]
        for (key, sem, v, prod) in toks:
            if prod == e and e == "pe":
                continue
            if wd.get(key, 0) >= v:
                continue
            wd[key] = v
            self.eng[e].wait_ge(sem, v)

    @staticmethod
    def _prune(lst):
        last = {}
        out = []
        for t in lst:
            if t[3] == "dma":
                out.append(t)
            else:
                last[t[3]] = t
        return out + list(last.values())

    def _deps(self, e, reads, writes):
        toks = []
        for b in reads:
            toks += b.w
        for b in writes:
            toks += b.w
            toks += b.r
        self._wait(e, toks)

    def _commit(self, tok, reads, writes):
        for b in reads:
            b.r.append(tok)
            if len(b.r) > 6:
                b.r = self._prune(b.r)
        for b in writes:
            b.w = [tok]
            b.r = []

    def op(self, e, fn, reads=(), writes=()):
        self._deps(e, reads, writes)
        ins = fn(self.eng[e])
        c = self._chan(e)
        c[2] += 1
        ins.then_inc(c[1], 1)
        tok = (c[0], c[1], c[2], e)
        self._commit(tok, reads, writes)
        self.ninstr += 1
        return tok

    def dma(self, q, fn, reads=(), writes=(), accumulate_writes=False):
        if accumulate_writes:
            toks = []
            for b in reads:
                toks += b.w
            for b in writes:
                if b.r:
                    toks += b.w
                    toks += b.r
            self._wait(q, toks)
        else:
            self._deps(q, reads, writes)
        i = self.ring_i
        self.ring_i = (i + 1) % self.NRING
        if self.ring_cnt[i] > 0:
            self._wait(q, [(f"dq{i}", self.ring[i], self.ring_cnt[i], "dma")])
        ins = fn(self.eng[q])
        self.ring_cnt[i] += 16
        ins.then_inc(self.ring[i], 16)
        tok = (f"dq{i}", self.ring[i], self.ring_cnt[i], "dma")
        for b in reads:
            b.r.append(tok)
        for b in writes:
            if accumulate_writes and not b.r:
                b.w.append(tok)
            else:
                b.w = [tok]
                b.r = []
        self.ninstr += 1
        return tok

    def barrier(self):
        toks = [(c[0], c[1], c[2], e) for e, c in self.chan.items() if c[2] > 0]
        toks += [(f"dq{i}", self.ring[i], self.ring_cnt[i], "dma") for i in range(self.NRING) if self.ring_cnt[i] > 0]
        for e in self.eng:
            self._wait(e, toks)

    def drain(self, e, bufs):
        toks = []
        for b in bufs:
            toks += b.w
            toks += b.r
        self._wait(e, toks)


def build_program(L, NSEQ, S, DE, debug=()):
    T = NSEQ * S
    NT = T // 128
    NB = -(-(2 * T + NE * (BLK - 1)) // BLK)
    PR = NB * BLK
    FC = DE // 128
    nc = bass.Bass("TRN2", target_bir_lowering=False)
    es = ExitStack()

    def dram(name, shape, dt, kind="Internal"):
        return nc.dram_tensor(name, list(shape), dt, kind=kind).ap()

    x_in = dram("x", [T, D], F32, "ExternalInput")
    mem_in = dram("mem", [NSEQ * MEM, D], F32, "ExternalInput")
    pars_in = dram("pars", [L, 128, NPAR], F32, "ExternalInput")
    gffn_in = dram("gffnb", [L, 128, D], F32, "ExternalInput")
    biasT_in = dram("biasT", [128, NH * 2, 128], F32, "ExternalInput")
    consts_in = dram("consts", [128, 5, 128], F32, "ExternalInput")
    thr_in = dram("thr", [128, NB], F32, "ExternalInput")
    w_in = dram("w_in", [L, D, D_IN], F32, "ExternalInput")
    w_out = dram("w_out", [L, D, D], F32, "ExternalInput")
    wq_c = dram("wq_c", [L, D, 512], F32, "ExternalInput")
    wkv_c = dram("wkv_c", [L, D, 1024], F32, "ExternalInput")
    wo_c = dram("wo_c", [L, 512, D], F32, "ExternalInput")
    w_rt = dram("w_rt", [L, D, 72], F32, "ExternalInput")
    if "F" in STAGES:
        NCH = KC * DE // 2048
        GK = 2048 // DE
        w1r = [[dram(f"w1r_{l_}_{c}", [NE * 128, 2048], F32, "ExternalInput") for c in range(NCH)] for l_ in range(L)]
        w3r = [[dram(f"w3r_{l_}_{c}", [NE * 128, 2048], F32, "ExternalInput") for c in range(NCH)] for l_ in range(L)]
        w2r = [[dram(f"w2r_{l_}_{c}", [NE * 128, 2048], F32, "ExternalInput") for c in range(FC)] for l_ in range(L)]
    out = dram("out", [T, D], F32, "ExternalOutput")

    xres = dram("xres", [T, D], F32)
    qTs = dram("qTs", [NSEQ, NH, 128, S], BF16)
    kTs = dram("kTs", [NSEQ, NH, 128, S], BF16)
    Vs = dram("Vs", [NSEQ, S, NH * 128], BF16)
    Us = dram("Us", [NSEQ, 8, 128, S], F32)
    mixT = dram("mixT", [16, 128, T], BF16)
    Hrows = dram("Hrows", [T, D], BF16)
    Xs = dram("Xs", [PR, D], BF16)
    Ys = dram("Ys", [PR, D], F32)
    dbg = {}
    for name, shape, dt in debug:
        dbg[name] = dram("dbg_" + name, shape, dt, "ExternalOutput")

    k = KB(nc, es)
    regions = {}
    uid = [0]

    def R(*key):
        b = regions.get(key)
        if b is None:
            b = Buf(None)
            regions[key] = b
        return b

    class Stage:
        def __init__(self):
            self.es = ExitStack()

        def sb(self, name, shape, dt):
            uid[0] += 1
            return Buf(self.es.enter_context(nc.sbuf_tensor(f"{name}_{uid[0]}", list(shape), dt)))

        def close(self):
            k.barrier()
            self.es.close()

    G = Stage()

    def ps(name, shape, dt=F32):
        return Buf(es.enter_context(nc.psum_tensor(name, list(shape), dt)))

    cst = G.sb("cst", [128, 5, 128], F32)
    ident_bf = G.sb("ident_bf", [128, 128], BF16)
    ones_bf = G.sb("ones_bf", [128, 128], BF16)
    ltri_bf = G.sb("ltri_bf", [128, 128], BF16)
    biasT = G.sb("biasT_sb", [128, NH * 2, 128], BF16)
    pars = G.sb("pars_sb", [128, NPAR], F32)
    small = G.sb("small", [128, 16], F32)
    col = [G.sb(f"col{i}", [128, 8], F32) for i in range(4)]
    f512 = [G.sb(f"f512_{i}", [128, 512], F32) for i in range(6)]
    b512 = [G.sb(f"b512_{i}", [128, 512], BF16) for i in range(4)]
    ones_f = cst.t[:, 1, :]
    bd64_f = cst.t[:, 2, :]
    iota_p = cst.t[:, 4, 0:1]
    pT = ps("pT", [128, KC, 128], BF16)
    pA = [ps(f"pA{i}", [128, 512]) for i in range(6)]

    k.dma("sp", lambda e: e.dma_start(out=cst.t[:], in_=consts_in[:, :, :]), writes=[cst])
    k.dma("pool", lambda e: e.dma_start(out=biasT.t[:], in_=biasT_in[:, :, :]), writes=[biasT])
    k.op("dve", lambda e: e.tensor_copy(out=ident_bf.t[:], in_=cst.t[:, 0, :]), reads=[cst], writes=[ident_bf])
    k.op("dve", lambda e: e.tensor_copy(out=ones_bf.t[:], in_=cst.t[:, 1, :]), reads=[cst], writes=[ones_bf])
    k.op("dve", lambda e: e.tensor_copy(out=ltri_bf.t[:], in_=cst.t[:, 3, :]), reads=[cst], writes=[ltri_bf])

    def P(name, j=0, w=1):
        o, _ = _off[name]
        return pars.t[:, o + j:o + j + w]

    cnt = {}

    def nxt(lst, key):
        i = cnt.get(key, 0)
        cnt[key] = i + 1
        return lst[i % len(lst)]

    def rms_tile(W, x_rows, xreg, gcolT, dst, dst_c0, a=1.0 / D):
        xt = nxt(W["xt"], "x")
        k.dma("sp", lambda e: e.dma_start(out=xt.t[:], in_=x_rows), reads=[xreg], writes=[xt])
        c0 = nxt(col, "c")
        junk = W["junk"]
        k.op("act", lambda e: e.activation(out=junk.t[:], in_=xt.t[:], func=AF.Square, accum_out=c0.t[:, 0:1]),
             reads=[xt], writes=[junk, c0])
        k.op("act", lambda e: e.activation(out=c0.t[:, 1:2], in_=c0.t[:, 0:1], func=AF.Sqrt, scale=a, bias=EPS),
             reads=[c0], writes=[c0])
        k.op("dve", lambda e: e.reciprocal(out=c0.t[:, 2:3], in_=c0.t[:, 1:2]), reads=[c0], writes=[c0])
        hb = nxt(W["hb"], "hb")
        k.op("act", lambda e: e.activation(out=hb.t[:], in_=xt.t[:], func=AF.Copy, scale=c0.t[:, 2:3]),
             reads=[xt, c0], writes=[hb])
        for kc in range(KC):
            k.op("pe", lambda e, kc=kc: e.transpose(out=pT.t[:, kc, :], in_=hb.t[:, kc * 128:(kc + 1) * 128],
                                                    identity=ident_bf.t[:]),
                 reads=[hb, ident_bf], writes=[pT])
        k.op("dve", lambda e: e.tensor_tensor(out=dst.t[:, :, dst_c0:dst_c0 + 128], in0=pT.t[:],
                                              in1=gcolT.unsqueeze(2).to_broadcast([128, KC, 128]), op=ALU.mult),
             reads=[pT, pars], writes=[dst])
        return xt

    def load_w(W, dram_view, cols, width=512):
        wb = nxt(W["wb"], "w")
        k.dma("pool", lambda e: e.dma_start(out=wb.t[:, :, 0:width], in_=dram_view[:, :, cols:cols + width]),
              writes=[wb])
        return wb

    def fm_norm(psrc, gcol, a, bconst, n, ones_mat):
        sq = nxt(f512, "f")
        k.op("act", lambda e: e.activation(out=sq.t[:, 0:n], in_=psrc.t[:, 0:n], func=AF.Square), reads=[psrc], writes=[sq])
        pss = nxt(pA, "pa")
        k.op("pe", lambda e: e.matmul(pss.t[:, 0:n], lhsT=ones_mat, rhs=sq.t[:, 0:n], start=True, stop=True),
             reads=[sq, cst], writes=[pss])
        rs = nxt(f512, "f")
        k.op("act", lambda e: e.activation(out=rs.t[:, 0:n], in_=pss.t[:, 0:n], func=AF.Sqrt, scale=a, bias=bconst),
             reads=[pss], writes=[rs])
        k.op("dve", lambda e: e.reciprocal(out=rs.t[:, 0:n], in_=rs.t[:, 0:n]), reads=[rs], writes=[rs])
        ob = nxt(b512, "b")
        k.op("dve", lambda e: e.scalar_tensor_tensor(out=ob.t[:, 0:n], in0=psrc.t[:, 0:n], scalar=gcol, in1=rs.t[:, 0:n],
                                                     op0=ALU.mult, op1=ALU.mult),
             reads=[psrc, rs, pars, small], writes=[ob])
        return ob

    def rmsW(st):
        return {"xt": [st.sb("xt", [128, D], F32) for _ in range(2)],
                "hb": [st.sb("hb", [128, D], BF16) for _ in range(2)],
                "junk": st.sb("junk", [128, D], BF16)}

    x_src = x_in
    x_key = "xin"
    for l in range(L):
        last = (l == L - 1)
        lam_init = 0.8 - 0.6 * math.exp(-0.3 * l)
        LS = Stage()
        k.dma("sp", lambda e: e.dma_start(out=pars.t[:], in_=pars_in[l, :, :]), writes=[pars])
        o_dl = _off["dl"][0]
        tmpc = f512[0]
        for i in range(2):
            k.op("dve", lambda e, i=i: e.tensor_tensor(out=tmpc.t[:, 0:DQ], in0=pars.t[:, o_dl + 2 * i * DQ:o_dl + (2 * i + 1) * DQ],
                                                       in1=pars.t[:, o_dl + (2 * i + 1) * DQ:o_dl + (2 * i + 2) * DQ], op=ALU.mult),
                 reads=[pars], writes=[tmpc])
            k.op("dve", lambda e, i=i: e.tensor_reduce(out=small.t[:, 2 + i:3 + i], in_=tmpc.t[:, 0:DQ], axis=AX.X, op=ALU.add),
                 reads=[tmpc], writes=[small])
        k.op("act", lambda e: e.activation(out=small.t[:, 4:6], in_=small.t[:, 2:4], func=AF.Exp), reads=[small], writes=[small])
        k.op("dve", lambda e: e.scalar_tensor_tensor(out=small.t[:, 0:1], in0=small.t[:, 5:6], scalar=-lam_init, in1=small.t[:, 4:5],
                                                     op0=ALU.add, op1=ALU.subtract), reads=[small], writes=[small])
        k.op("dve", lambda e: e.tensor_scalar(out=small.t[:, 1:2], in0=P("gsub"), scalar1=1.0 - lam_init, scalar2=None, op0=ALU.mult),
             reads=[pars], writes=[small])
        neglam = small.t[:, 0:1]
        gsubs = small.t[:, 1:2]

        if "A" in STAGES:
            st_ = Stage()
            W = rmsW(st_)
            W["wb"] = [st_.sb("wb", [128, KC, 512], BF16) for _ in range(3)]
            hT = st_.sb("hT", [128, KC, S], BF16)
            w_in_v = w_in[l].rearrange("(kc p) n -> p kc n", p=128)
            w_order = [(s_, nb_) for s_ in range(NSEQ) for nb_ in (0, 1, 2, 3, 4, 5, 6, 8, 7, 9)]
            w_loaded = {}
            w_pos = [0]

            def get_w(s_, nb_):
                target = min(w_order.index((s_, nb_)) + 1, len(w_order) - 1)
                while w_pos[0] <= target:
                    key_ = w_order[w_pos[0]]
                    w_loaded[key_] = load_w(W, w_in_v, key_[1] * 512)
                    w_pos[0] += 1
                return w_loaded[(s_, nb_)]

            for s in range(NSEQ):
                for tt in range(S // 128):
                    r0 = s * S + tt * 128
                    rms_tile(W, x_src[r0:r0 + 128, :], R(x_key, r0 // 128), P("gmixT", 0, 16), hT, tt * 128)
                for nb in range(4):
                    wb = get_w(s, nb)
                    isq = nb < 2
                    dstT = qTs if isq else kTs
                    gcol = P("gq2") if isq else P("gk2")
                    a, bc = (1.0, DQ * EPS) if isq else (1.0 / DQ, EPS)
                    for j in range(4):
                        head = (nb % 2) * 4 + j
                        for tb in range(S // 512):
                            pp = nxt(pA, "pa")
                            for kc in range(KC):
                                k.op("pe", lambda e, kc=kc: e.matmul(pp.t[:], lhsT=wb.t[:, kc, j * 128:(j + 1) * 128],
                                                                     rhs=hT.t[:, kc, tb * 512:(tb + 1) * 512],
                                                                     start=(kc == 0), stop=(kc == KC - 1)),
                                     reads=[wb, hT], writes=[pp])
                            ob = fm_norm(pp, gcol, a, bc, 512, bd64_f)
                            k.dma("sp", lambda e: e.dma_start(out=dstT[s, head, :, tb * 512:(tb + 1) * 512], in_=ob.t[:]),
                                  reads=[ob], writes=[R("qk", isq, s, head)], accumulate_writes=True)
                for nb in range(4, 6):
                    wb = get_w(s, nb)
                    for tt in range(S // 128):
                        pp = nxt(pA, "pa")
                        for kc in range(KC):
                            k.op("pe", lambda e, kc=kc: e.matmul(pp.t[:], lhsT=hT.t[:, kc, tt * 128:(tt + 1) * 128], rhs=wb.t[:, kc, :],
                                                                 start=(kc == 0), stop=(kc == KC - 1)),
                                 reads=[wb, hT], writes=[pp])
                        ob = nxt(b512, "b")
                        k.op("act", lambda e: e.activation(out=ob.t[:], in_=pp.t[:], func=AF.Copy), reads=[pp], writes=[ob])
                        k.dma("sp", lambda e: e.dma_start(out=Vs[s, tt * 128:(tt + 1) * 128, (nb - 4) * 512:(nb - 3) * 512], in_=ob.t[:]),
                              reads=[ob], writes=[R("v", s)], accumulate_writes=True)
                for i in range(2):
                    wa = get_w(s, 6 + i)
                    wg = get_w(s, 8 + i)
                    for j in range(4):
                        ch = i * 4 + j
                        for tb in range(S // 512):
                            pa_ = nxt(pA, "pa")
                            pg_ = nxt(pA, "pa")
                            for kc in range(KC):
                                k.op("pe", lambda e, kc=kc: e.matmul(pa_.t[:], lhsT=wa.t[:, kc, j * 128:(j + 1) * 128],
                                                                     rhs=hT.t[:, kc, tb * 512:(tb + 1) * 512],
                                                                     start=(kc == 0), stop=(kc == KC - 1)),
                                     reads=[wa, hT], writes=[pa_])
                            for kc in range(KC):
                                k.op("pe", lambda e, kc=kc: e.matmul(pg_.t[:], lhsT=wg.t[:, kc, j * 128:(j + 1) * 128],
                                                                     rhs=hT.t[:, kc, tb * 512:(tb + 1) * 512],
                                                                     start=(kc == 0), stop=(kc == KC - 1)),
                                     reads=[wg, hT], writes=[pg_])
                            sg = nxt(f512, "f")
                            k.op("act", lambda e: e.activation(out=sg.t[:], in_=pg_.t[:], func=AF.Sigmoid), reads=[pg_], writes=[sg])
                            ub = nxt(f512, "f")
                            k.op("dve", lambda e: e.tensor_tensor(out=ub.t[:], in0=pa_.t[:], in1=sg.t[:], op=ALU.mult),
                                 reads=[pa_, sg], writes=[ub])
                            k.dma("sp", lambda e: e.dma_start(out=Us[s, ch, :, tb * 512:(tb + 1) * 512], in_=ub.t[:]),
                                  reads=[ub], writes=[R("u", s, ch)], accumulate_writes=True)
            st_.close()

        if "qT" in dbg and l == 0:
            k.dma("sp", lambda e: e.dma_start(out=dbg["qT"][:, :, :, :], in_=qTs[:, :, :, :]), writes=[R("dbg", "qT")])
            k.dma("sp", lambda e: e.dma_start(out=dbg["kT"][:, :, :, :], in_=kTs[:, :, :, :]), writes=[R("dbg", "kT")])
            k.dma("sp", lambda e: e.dma_start(out=dbg["V"][:, :, :], in_=Vs[:, :, :]), writes=[R("dbg", "V")])
            k.dma("sp", lambda e: e.dma_start(out=dbg["U"][:, :, :, :], in_=Us[:, :, :, :]), writes=[R("dbg", "U")])
            k.barrier()

        if "B" in STAGES:
            st_ = Stage()
            qh2 = [st_.sb("qh", [128, S], BF16) for i in range(2)]
            kh2 = [st_.sb("kh", [128, S], BF16) for i in range(2)]
            vh2 = [st_.sb("vh", [128, S // 128, 128], BF16) for i in range(2)]
            attn_r = [st_.sb("attn_r", [128, 512], F32) for i in range(2)]
            attn_t = [st_.sb("attn_t", [128, 512], F32) for i in range(2)]
            Ubuf2 = [st_.sb("Ubuf", [128, 32 + S], F32) for _ in range(2)]
            ctmp = [st_.sb("ctmp", [128, S], F32) for _ in range(2)]
            Yc = [st_.sb("Yc", [128, S], F32) for j in range(8)]
            mean = st_.sb("mean", [128, 512], F32)
            msq = st_.sb("msq", [128, 512], F32)
            var = st_.sb("var", [128, 512], F32)
            for ub_ in Ubuf2:
                k.op("pool", lambda e, ub_=ub_: e.memset(ub_.t[:, 0:32], 0.0), writes=[ub_])
            o_cw = _off["convw"][0]
            itc = [0]

            def conv_seq(s):
                for j in range(8):
                    ub = nxt(Ubuf2, "ub")
                    k.dma("pool", lambda e: e.dma_start(out=ub.t[:, 32:32 + S], in_=Us[s, j, :, :]), reads=[R("u", s, j)], writes=[ub])
                    k.op("pool", lambda e: e.tensor_scalar(out=Yc[j].t[:], in0=ub.t[:, 2:2 + S], scalar1=pars.t[:, o_cw + j * CW:o_cw + j * CW + 1],
                                                           scalar2=P("convb", j), op0=ALU.mult, op1=ALU.add),
                         reads=[ub, pars], writes=[Yc[j]])
                    for tap in range(1, CW):
                        tmp = nxt(ctmp, "ctmp")
                        k.op("pool", lambda e, tap=tap: e.tensor_scalar(out=tmp.t[:], in0=ub.t[:, 2 + tap:2 + tap + S],
                                                                        scalar1=pars.t[:, o_cw + j * CW + tap:o_cw + j * CW + tap + 1],
                                                                        scalar2=0.0, op0=ALU.mult, op1=ALU.add),
                             reads=[ub, pars], writes=[tmp])
                        k.op("pool", lambda e: e.tensor_tensor(out=Yc[j].t[:], in0=Yc[j].t[:], in1=tmp.t[:], op=ALU.add),
                             reads=[tmp], writes=[Yc[j]])

            def attn_seq(s):
                for h in range(NH):
                    qh, kh, vh = qh2[itc[0] % 2], kh2[itc[0] % 2], vh2[itc[0] % 2]
                    itc[0] += 1
                    k.dma("sp", lambda e: e.dma_start(out=qh.t[:], in_=qTs[s, h, :, :]), reads=[R("qk", True, s, h)], writes=[qh])
                    k.dma("sp", lambda e: e.dma_start(out=kh.t[:], in_=kTs[s, h, :, :]), reads=[R("qk", False, s, h)], writes=[kh])
                    k.dma("sp", lambda e: e.dma_start(out=vh.t[:], in_=Vs[s, :, h * 128:(h + 1) * 128].rearrange("(t p) d -> p t d", p=128)),
                          reads=[R("v", s)], writes=[vh])
                    for qc in range(S // 512):
                        tms = []
                        for m in range(2):
                            pO = pA[2 * m]
                            pL = pA[2 * m + 1]
                            nk = (qc + 1) * 4

                            def emit_qk(kj, m=m, qc=qc):
                                q_lo = max(qc * 512, kj * 128)
                                c0 = q_lo - qc * 512
                                sT = nxt(pA[4:6], "pa_s")
                                blist = [dlt for dlt in (0, 1) if qc * 4 <= kj + dlt < (qc + 1) * 4]
                                k.op("pe", lambda e: e.matmul(sT.t[:, c0:512], lhsT=kh.t[m * 64:(m + 1) * 64, kj * 128:(kj + 1) * 128],
                                                              rhs=qh.t[m * 64:(m + 1) * 64, q_lo:(qc + 1) * 512],
                                                              start=True, stop=(len(blist) == 0)),
                                     reads=[kh, qh], writes=[sT])
                                for bi, dlt in enumerate(blist):
                                    cc = (kj + dlt) * 128 - qc * 512
                                    k.op("pe", lambda e, cc=cc, dlt=dlt, bi=bi: e.matmul(
                                        sT.t[:, cc:cc + 128], lhsT=ident_bf.t[:], rhs=biasT.t[:, h * 2 + dlt, :],
                                        start=False, stop=(bi == len(blist) - 1)),
                                         reads=[ident_bf, biasT], writes=[sT])
                                return sT, c0

                            nxt_qk = emit_qk(0)
                            for kj in range(nk):
                                sT, c0 = nxt_qk
                                if kj + 1 < nk:
                                    nxt_qk = emit_qk(kj + 1)
                                near_end = min((kj + 2) * 128, (qc + 1) * 512) - qc * 512
                                pt = nxt(b512, "b")
                                if near_end > c0:
                                    k.op("act", lambda e: e.activation(out=pt.t[:, c0:near_end], in_=sT.t[:, c0:near_end], func=AF.Exp),
                                         reads=[sT], writes=[pt])
                                if near_end < 512:
                                    lo = max(near_end, c0)
                                    k.op("act", lambda e: e.activation(out=pt.t[:, lo:512], in_=sT.t[:, lo:512], func=AF.Exp,
                                                                       bias=P("relc", h)),
                                         reads=[sT, pars], writes=[pt])
                                k.op("pe", lambda e: e.matmul(pO.t[:, c0:512], lhsT=vh.t[:, kj, :], rhs=pt.t[:, c0:512],
                                                              start=(kj == 0), stop=(kj == nk - 1)),
                                     reads=[vh, pt], writes=[pO])
                                k.op("pe", lambda e: e.matmul(pL.t[:, c0:512], lhsT=ones_bf.t[:], rhs=pt.t[:, c0:512],
                                                              start=(kj == 0), stop=(kj == nk - 1)),
                                     reads=[ones_bf, pt], writes=[pL])
                            r_ = attn_r[m]
                            k.op("dve", lambda e: e.reciprocal(out=r_.t[:], in_=pL.t[:]), reads=[pL], writes=[r_])
                            t_ = attn_t[m]
                            k.op("dve", lambda e: e.tensor_tensor(out=t_.t[:], in0=pO.t[:], in1=r_.t[:], op=ALU.mult),
                                 reads=[pO, r_], writes=[t_])
                            tms.append(t_)
                        a_ = nxt(f512, "f")
                        k.op("dve", lambda e: e.scalar_tensor_tensor(out=a_.t[:], in0=tms[1].t[:], scalar=neglam, in1=tms[0].t[:],
                                                                     op0=ALU.mult, op1=ALU.add),
                             reads=[tms[0], tms[1], small], writes=[a_])
                        ob = fm_norm(a_, gsubs, 1.0 / 128, EPS, 512, ones_f)
                        c_lo = s * S + qc * 512
                        k.dma("sp", lambda e: e.dma_start(out=mixT[h, :, c_lo:c_lo + 512], in_=ob.t[:]),
                              reads=[ob], writes=[R("mix", c_lo // 512)], accumulate_writes=True)

            def ln_seq(s):
                for tb in range(S // 512):
                    cs = slice(tb * 512, (tb + 1) * 512)
                    pM = nxt(pA, "pa")
                    pQ = nxt(pA, "pa")
                    for j in range(8):
                        k.op("pe", lambda e, j=j: e.matmul(pM.t[:], lhsT=ones_f, rhs=Yc[j].t[:, cs], start=(j == 0), stop=(j == 7)),
                             reads=[cst, Yc[j]], writes=[pM])
                    for j in range(8):
                        sq = nxt(f512, "f")
                        k.op("act", lambda e, j=j: e.activation(out=sq.t[:], in_=Yc[j].t[:, cs], func=AF.Square), reads=[Yc[j]], writes=[sq])
                        k.op("pe", lambda e, j=j: e.matmul(pQ.t[:], lhsT=ones_f, rhs=sq.t[:], start=(j == 0), stop=(j == 7)),
                             reads=[cst, sq], writes=[pQ])
                    k.op("act", lambda e: e.activation(out=mean.t[:], in_=pM.t[:], func=AF.Copy, scale=1.0 / 1024), reads=[pM], writes=[mean])
                    k.op("act", lambda e: e.activation(out=msq.t[:], in_=mean.t[:], func=AF.Square), reads=[mean], writes=[msq])
                    k.op("dve", lambda e: e.scalar_tensor_tensor(out=var.t[:], in0=pQ.t[:], scalar=1.0 / 1024, in1=msq.t[:],
                                                                 op0=ALU.mult, op1=ALU.subtract), reads=[pQ, msq], writes=[var])
                    k.op("act", lambda e: e.activation(out=var.t[:], in_=var.t[:], func=AF.Sqrt, scale=1.0, bias=EPS), reads=[var], writes=[var])
                    k.op("dve", lambda e: e.reciprocal(out=var.t[:], in_=var.t[:]), reads=[var], writes=[var])
                    for j in range(8):
                        t_ = nxt(f512, "f")
                        k.op("dve", lambda e, j=j: e.tensor_tensor(out=t_.t[:], in0=Yc[j].t[:, cs], in1=mean.t[:], op=ALU.subtract),
                             reads=[Yc[j], mean], writes=[t_])
                        k.op("dve", lambda e: e.tensor_tensor(out=t_.t[:], in0=t_.t[:], in1=var.t[:], op=ALU.mult),
                             reads=[var], writes=[t_])
                        ob = nxt(b512, "b")
                        k.op("act", lambda e, j=j: e.activation(out=ob.t[:], in_=t_.t[:], func=AF.Silu, scale=P("lng", j), bias=P("lnb", j)),
                             reads=[t_, pars], writes=[ob])
                        c_lo = s * S + tb * 512
                        k.dma("sp", lambda e, j=j: e.dma_start(out=mixT[8 + j, :, c_lo:c_lo + 512], in_=ob.t[:]),
                              reads=[ob], writes=[R("mix", c_lo // 512)], accumulate_writes=True)

            for s in range(NSEQ):
                conv_seq(s)
                attn_seq(s)
                ln_seq(s)
            st_.close()

        if "mixT" in dbg and l == 0:
            k.dma("sp", lambda e: e.dma_start(out=dbg["mixT"][:, :, :], in_=mixT[:, :, :]), writes=[R("dbg", "mixT")])
            k.barrier()

        if "D" in STAGES:
            st_ = Stage()
            wo_v = w_out[l].rearrange("(kc p) n -> p kc n", p=128)
            wfull = st_.sb("wfull", [128, KC, D], BF16)
            mb2 = [st_.sb("mb", [128, KC, 512], BF16) for _ in range(2)]
            xt2 = [st_.sb("xt", [128, D], F32) for _ in range(2)]
            xo2 = [st_.sb("xo", [128, D], F32) for _ in range(2)]
            for nb in range(4):
                k.dma("pool", lambda e, nb=nb: e.dma_start(out=wfull.t[:, :, nb * 512:(nb + 1) * 512], in_=wo_v[:, :, nb * 512:(nb + 1) * 512]),
                      writes=[wfull], accumulate_writes=True)
            mix_v = mixT.rearrange("c p t -> p c t")
            for tb in range(T // 512):
                mb = nxt(mb2, "mb")
                k.dma("sp", lambda e: e.dma_start(out=mb.t[:], in_=mix_v[:, :, tb * 512:(tb + 1) * 512]), reads=[R("mix", tb)], writes=[mb])
                for tt in range(4):
                    r0 = tb * 512 + tt * 128
                    xt = nxt(xt2, "x")
                    k.dma("sp", lambda e: e.dma_start(out=xt.t[:], in_=x_src[r0:r0 + 128, :]), reads=[R(x_key, r0 // 128)], writes=[xt])
                    xo = nxt(xo2, "xo")
                    for nb in range(4):
                        pp = nxt(pA, "pa")
                        for c in range(KC):
                            k.op("pe", lambda e, c=c: e.matmul(pp.t[:], lhsT=mb.t[:, c, tt * 128:(tt + 1) * 128],
                                                               rhs=wfull.t[:, c, nb * 512:(nb + 1) * 512],
                                                               start=(c == 0), stop=(c == KC - 1)),
                                 reads=[mb, wfull], writes=[pp])
                        k.op("dve", lambda e, nb=nb: e.tensor_tensor(out=xo.t[:, nb * 512:(nb + 1) * 512], in0=pp.t[:],
                                                                     in1=xt.t[:, nb * 512:(nb + 1) * 512], op=ALU.add),
                             reads=[pp, xt], writes=[xo])
                    k.dma("sp", lambda e: e.dma_start(out=xres[r0:r0 + 128, :], in_=xo.t[:]), reads=[xo], writes=[R("xres", r0 // 128)])
            st_.close()
            x_src = xres
            x_key = "xres"

        if "x1" in dbg and l == 0:
            k.dma("sp", lambda e: e.dma_start(out=dbg["x1"][:, :], in_=xres[:, :]), writes=[R("dbg", "x1")])
            k.barrier()

        if "E" in STAGES:
            st_ = Stage()
            W = rmsW(st_)
            W["wb"] = [st_.sb("wb", [128, KC, 512], BF16) for _ in range(2)]
            xo2 = [st_.sb("xo", [128, D], F32) for _ in range(2)]
            memT = st_.sb("memT", [128, KC, MEM], BF16)
            kcT = st_.sb("kcT", [128, NHC, MEM], BF16)
            vcs = st_.sb("vcs", [128, 2, 512], BF16)
            ocT = st_.sb("ocT", [128, NHC, 512], BF16)
            hTb = st_.sb("hTb", [128, KC, 512], BF16)
            wq_sb = st_.sb("wq_sb", [128, KC, 512], BF16)
            qn4 = [st_.sb("qn4", [128, 512], BF16) for _ in range(NHC)]
            wo_sb = st_.sb("wo_sb", [128, NHC, D], BF16)
            wq_v = wq_c[l].rearrange("(kc p) n -> p kc n", p=128)
            wkv_v = wkv_c[l].rearrange("(kc p) n -> p kc n", p=128)
            woc_v = wo_c[l].rearrange("(kc p) n -> p kc n", p=128)
            k.dma("pool", lambda e: e.dma_start(out=wq_sb.t[:], in_=wq_v[:, :, :]), writes=[wq_sb])
            k.dma("pool", lambda e: e.dma_start(out=wo_sb.t[:], in_=woc_v[:, :, :]), writes=[wo_sb])
            for s in range(NSEQ):
                for mt in range(2):
                    r0 = s * MEM + mt * 128
                    rms_tile(W, mem_in[r0:r0 + 128, :], R("memin"), P("gmemT", 0, 16), memT, mt * 128)
                wk_ = load_w(W, wkv_v, 0)
                for h in range(NHC):
                    pp = nxt(pA, "pa")
                    for kc in range(KC):
                        k.op("pe", lambda e, kc=kc: e.matmul(pp.t[:, 0:MEM], lhsT=wk_.t[:, kc, h * 128:(h + 1) * 128], rhs=memT.t[:, kc, :],
                                                             start=(kc == 0), stop=(kc == KC - 1)), reads=[wk_, memT], writes=[pp])
                    ob = fm_norm(pp, P("gkc"), 1.0 / 128, EPS, MEM, ones_f)
                    k.op("dve", lambda e: e.tensor_copy(out=kcT.t[:, h, :], in_=ob.t[:, 0:MEM]), reads=[ob], writes=[kcT])
                wv_ = load_w(W, wkv_v, 512)
                for mt in range(2):
                    pp = nxt(pA, "pa")
                    for kc in range(KC):
                        k.op("pe", lambda e, kc=kc: e.matmul(pp.t[:], lhsT=memT.t[:, kc, mt * 128:(mt + 1) * 128], rhs=wv_.t[:, kc, :],
                                                             start=(kc == 0), stop=(kc == KC - 1)), reads=[wv_, memT], writes=[pp])
                    k.op("act", lambda e: e.activation(out=vcs.t[:, mt, :], in_=pp.t[:], func=AF.Copy), reads=[pp], writes=[vcs])
                for tb in range(S // 512):
                    for tt in range(4):
                        r0 = s * S + tb * 512 + tt * 128
                        rms_tile(W, xres[r0:r0 + 128, :], R("xres", r0 // 128), P("gcrossT", 0, 16), hTb, tt * 128)
                    for h in range(NHC):
                        pp = pA[h]
                        for kc in range(KC):
                            k.op("pe", lambda e, kc=kc, pp=pp: e.matmul(pp.t[:], lhsT=wq_sb.t[:, kc, h * 128:(h + 1) * 128], rhs=hTb.t[:, kc, :],
                                                                        start=(kc == 0), stop=(kc == KC - 1)), reads=[wq_sb, hTb], writes=[pp])
                    for h0 in (0, 2):
                        hs = (h0, h0 + 1)
                        for h in hs:
                            k.op("act", lambda e, h=h: e.activation(out=f512[h].t[:], in_=pA[h].t[:], func=AF.Square), reads=[pA[h]], writes=[f512[h]])
                        for h in hs:
                            k.op("pe", lambda e, h=h: e.matmul(pA[4 + h % 2].t[:], lhsT=ones_f, rhs=f512[h].t[:], start=True, stop=True),
                                 reads=[f512[h], cst], writes=[pA[4 + h % 2]])
                        for h in hs:
                            k.op("act", lambda e, h=h: e.activation(out=f512[h].t[:], in_=pA[4 + h % 2].t[:], func=AF.Sqrt, scale=1.0, bias=128 * EPS),
                                 reads=[pA[4 + h % 2]], writes=[f512[h]])
                        for h in hs:
                            k.op("dve", lambda e, h=h: e.reciprocal(out=f512[h].t[:], in_=f512[h].t[:]), reads=[f512[h]], writes=[f512[h]])
                        for h in hs:
                            k.op("dve", lambda e, h=h: e.scalar_tensor_tensor(out=qn4[h].t[:], in0=pA[h].t[:], scalar=P("gqc"), in1=f512[h].t[:],
                                                                              op0=ALU.mult, op1=ALU.mult),
                                 reads=[pA[h], f512[h], pars], writes=[qn4[h]])
                    for h in range(NHC):
                        pO = pA[(h % 2) * 2]
                        pL = pA[(h % 2) * 2 + 1]
                        sTs = [pA[4], pA[5]]
                        pts = [nxt(b512, "b"), nxt(b512, "b")]
                        for mt in range(2):
                            k.op("pe", lambda e, mt=mt: e.matmul(sTs[mt].t[:], lhsT=kcT.t[:, h, mt * 128:(mt + 1) * 128], rhs=qn4[h].t[:], start=True, stop=True),
                                 reads=[kcT, qn4[h]], writes=[sTs[mt]])
                        for mt in range(2):
                            k.op("act", lambda e, mt=mt: e.activation(out=pts[mt].t[:], in_=sTs[mt].t[:], func=AF.Exp), reads=[sTs[mt]], writes=[pts[mt]])
                        for mt in range(2):
                            k.op("pe", lambda e, mt=mt: e.matmul(pO.t[:], lhsT=vcs.t[:, mt, h * 128:(h + 1) * 128], rhs=pts[mt].t[:], start=(mt == 0), stop=(mt == 1)),
                                 reads=[vcs, pts[mt]], writes=[pO])
                            k.op("pe", lambda e, mt=mt: e.matmul(pL.t[:], lhsT=ones_bf.t[:], rhs=pts[mt].t[:], start=(mt == 0), stop=(mt == 1)),
                                 reads=[ones_bf, pts[mt]], writes=[pL])
                        r_ = f512[4 + h % 2]
                        k.op("dve", lambda e: e.reciprocal(out=r_.t[:], in_=pL.t[:]), reads=[pL], writes=[r_])
                        k.op("dve", lambda e: e.tensor_tensor(out=ocT.t[:, h, :], in0=pO.t[:], in1=r_.t[:], op=ALU.mult),
                             reads=[pO, r_], writes=[ocT])
                    for tt in range(4):
                        r0 = s * S + tb * 512 + tt * 128
                        xt = nxt(W["xt"], "x")
                        k.dma("sp", lambda e: e.dma_start(out=xt.t[:], in_=xres[r0:r0 + 128, :]), reads=[R("xres", r0 // 128)], writes=[xt])
                        xo = nxt(xo2, "xo")
                        for nb in range(4):
                            pp = nxt(pA, "pa")
                            for h in range(NHC):
                                k.op("pe", lambda e, h=h: e.matmul(pp.t[:], lhsT=ocT.t[:, h, tt * 128:(tt + 1) * 128],
                                                                   rhs=wo_sb.t[:, h, nb * 512:(nb + 1) * 512],
                                                                   start=(h == 0), stop=(h == NHC - 1)), reads=[ocT, wo_sb], writes=[pp])
                            k.op("dve", lambda e, nb=nb: e.tensor_tensor(out=xo.t[:, nb * 512:(nb + 1) * 512], in0=pp.t[:],
                                                                         in1=xt.t[:, nb * 512:(nb + 1) * 512], op=ALU.add),
                                 reads=[pp, xt], writes=[xo])
                        k.dma("sp", lambda e: e.dma_start(out=xres[r0:r0 + 128, :], in_=xo.t[:]), reads=[xo], writes=[R("xres", r0 // 128)])
            st_.close()

        if "x2" in dbg and l == 0:
            k.dma("sp", lambda e: e.dma_start(out=dbg["x2"][:, :], in_=xres[:, :]), writes=[R("dbg", "x2")])
            k.barrier()

        if "F" in STAGES:
            gates = LS.sb("gates", [128, NT, 2], F32)
            desti = LS.sb("desti", [128, NT * 2], I32)
            widx = LS.sb("widx", [128, NB], I32)
            st_ = Stage()
            xt2 = [st_.sb("xt", [128, D], F32) for _ in range(2)]
            hb2 = [st_.sb("hb", [128, D], BF16) for _ in range(2)]
            junk = st_.sb("junk", [128, D], BF16)
            gffn = st_.sb("gffn", [128, D], F32)
            wr = st_.sb("wr", [128, KC, 72], BF16)
            hTt = st_.sb("hTt", [128, KC, 128], BF16)
            OH = st_.sb("OH", [128, NT, 2, NE], F32)
            RANK = st_.sb("RANK", [128, NT, NE], F32)
            cum = st_.sb("cum", [128, NE], F32)
            rt = [st_.sb("rt", [128, 128], F32) for i in range(6)]
            Mb = st_.sb("Mb", [128, NE], BF16)
            destf = st_.sb("destf", [128, NT * 2], F32)
            pcs = [st_.sb("pcs", [128, NE], F32) for i in range(3)]
            pci = st_.sb("pci", [128, NE], I32)
            thr = st_.sb("thr_sb", [128, NB], F32)
            cmpb = st_.sb("cmpb", [128, NB, NE], F32)
            bef = st_.sb("bef", [128, NB], F32)
            k.dma("sp", lambda e: e.dma_start(out=thr.t[:], in_=thr_in[:, :]), writes=[thr])
            k.dma("sp", lambda e: e.dma_start(out=gffn.t[:], in_=gffn_in[l, :, :]), writes=[gffn])
            k.dma("pool", lambda e: e.dma_start(out=wr.t[:], in_=w_rt[l].rearrange("(kc p) n -> p kc n", p=128)), writes=[wr])
            k.op("dve", lambda e: e.memset(cum.t[:], 0.0), writes=[cum])
            o_brt = _off["brt"][0]
            for tt in range(NT):
                r0 = tt * 128
                xt = nxt(xt2, "x")
                k.dma("sp", lambda e: e.dma_start(out=xt.t[:], in_=xres[r0:r0 + 128, :]), reads=[R("xres", tt)], writes=[xt])
                c0 = nxt(col, "c")
                k.op("act", lambda e: e.activation(out=junk.t[:], in_=xt.t[:], func=AF.Square, accum_out=c0.t[:, 0:1]),
                     reads=[xt], writes=[junk, c0])
                k.op("act", lambda e: e.activation(out=c0.t[:, 1:2], in_=c0.t[:, 0:1], func=AF.Sqrt, scale=1.0 / D, bias=EPS), reads=[c0], writes=[c0])
                k.op("dve", lambda e: e.reciprocal(out=c0.t[:, 2:3], in_=c0.t[:, 1:2]), reads=[c0], writes=[c0])
                hb = nxt(hb2, "hb")
                k.op("dve", lambda e: e.scalar_tensor_tensor(out=hb.t[:], in0=xt.t[:], scalar=c0.t[:, 2:3], in1=gffn.t[:],
                                                             op0=ALU.mult, op1=ALU.mult), reads=[xt, c0, gffn], writes=[hb])
                k.dma("sp", lambda e: e.dma_start(out=Hrows[r0:r0 + 128, :], in_=hb.t[:]), reads=[hb], writes=[R("hrows", tt)])
                for kc in range(KC):
                    k.op("pe", lambda e, kc=kc: e.transpose(out=pT.t[:, kc, :], in_=hb.t[:, kc * 128:(kc + 1) * 128], identity=ident_bf.t[:]),
                         reads=[hb, ident_bf], writes=[pT])
                k.op("act", lambda e: e.activation(out=hTt.t[:], in_=pT.t[:], func=AF.Copy), reads=[pT], writes=[hTt])
                pp = nxt(pA, "pa")
                for kc in range(KC):
                    k.op("pe", lambda e, kc=kc: e.matmul(pp.t[:, 0:72], lhsT=hTt.t[:, kc, :], rhs=wr.t[:, kc, :], start=(kc == 0), stop=(kc == KC - 1)),
                         reads=[hTt, wr], writes=[pp])
                lg, w1_, w2_, w3_, w4_, w5_ = rt
                k.op("dve", lambda e: e.tensor_tensor(out=lg.t[:, 0:72], in0=pp.t[:, 0:72], in1=pars.t[:, o_brt:o_brt + 72], op=ALU.add),
                     reads=[pp, pars], writes=[lg])
                k.op("dve", lambda e: e.tensor_reduce(out=w1_.t[:, 0:1], in_=lg.t[:, 0:8], axis=AX.X, op=ALU.max), reads=[lg], writes=[w1_])
                k.op("dve", lambda e: e.tensor_scalar(out=w2_.t[:, 0:8], in0=lg.t[:, 0:8], scalar1=w1_.t[:, 0:1], scalar2=None, op0=ALU.is_equal),
                     reads=[lg, w1_], writes=[w2_])
                k.op("dve", lambda e: e.tensor_scalar(out=w1_.t[:, 1:2], in0=w1_.t[:, 0:1], scalar1=-1.0, scalar2=None, op0=ALU.mult),
                     reads=[w1_], writes=[w1_])
                k.op("act", lambda e: e.activation(out=w2_.t[:, 8:16], in_=lg.t[:, 0:8], func=AF.Exp, bias=w1_.t[:, 1:2], accum_out=w1_.t[:, 2:3]),
                     reads=[lg, w1_], writes=[w2_, w1_])
                k.op("dve", lambda e: e.reciprocal(out=w1_.t[:, 3:4], in_=w1_.t[:, 2:3]), reads=[w1_], writes=[w1_])
                k.op("dve", lambda e: e.tensor_tensor(out=w3_.t[:, 0:64].rearrange("p (g j) -> p g j", j=8),
                                                      in0=lg.t[:, 8:72].rearrange("p (g j) -> p g j", j=8),
                                                      in1=w2_.t[:, 0:8].unsqueeze(2).to_broadcast([128, 8, 8]), op=ALU.mult),
                     reads=[lg, w2_], writes=[w3_])
                k.op("dve", lambda e: e.tensor_reduce(out=w4_.t[:, 0:8], in_=w3_.t[:, 0:64].rearrange("p (g j) -> p j g", j=8), axis=AX.X, op=ALU.add),
                     reads=[w3_], writes=[w4_])
                k.op("dve", lambda e: e.tensor_reduce(out=w1_.t[:, 4:5], in_=w4_.t[:, 0:8], axis=AX.X, op=ALU.max), reads=[w4_], writes=[w1_])
                k.op("dve", lambda e: e.tensor_scalar(out=w4_.t[:, 8:16], in0=w4_.t[:, 0:8], scalar1=w1_.t[:, 4:5], scalar2=None, op0=ALU.is_equal),
                     reads=[w4_, w1_], writes=[w4_])
                k.op("dve", lambda e: e.scalar_tensor_tensor(out=w4_.t[:, 16:24], in0=w4_.t[:, 8:16], scalar=-1e30, in1=w4_.t[:, 0:8],
                                                             op0=ALU.mult, op1=ALU.add), reads=[w4_], writes=[w4_])
                k.op("dve", lambda e: e.tensor_reduce(out=w1_.t[:, 5:6], in_=w4_.t[:, 16:24], axis=AX.X, op=ALU.max), reads=[w4_], writes=[w1_])
                k.op("dve", lambda e: e.tensor_scalar(out=w4_.t[:, 24:32], in0=w4_.t[:, 16:24], scalar1=w1_.t[:, 5:6], scalar2=None, op0=ALU.is_equal),
                     reads=[w4_, w1_], writes=[w4_])
                k.op("dve", lambda e: e.tensor_tensor(out=w1_.t[:, 6:7], in0=w1_.t[:, 5:6], in1=w1_.t[:, 4:5], op=ALU.subtract), reads=[w1_], writes=[w1_])
                k.op("act", lambda e: e.activation(out=w1_.t[:, 7:8], in_=w1_.t[:, 6:7], func=AF.Exp), reads=[w1_], writes=[w1_])
                k.op("dve", lambda e: e.tensor_scalar(out=w1_.t[:, 8:9], in0=w1_.t[:, 7:8], scalar1=1.0, scalar2=None, op0=ALU.add), reads=[w1_], writes=[w1_])
                k.op("dve", lambda e: e.reciprocal(out=w1_.t[:, 9:10], in_=w1_.t[:, 8:9]), reads=[w1_], writes=[w1_])
                k.op("dve", lambda e: e.tensor_tensor(out=gates.t[:, tt, 0:1], in0=w1_.t[:, 3:4], in1=w1_.t[:, 9:10], op=ALU.mult), reads=[w1_], writes=[gates])
                k.op("dve", lambda e: e.tensor_tensor(out=gates.t[:, tt, 1:2], in0=gates.t[:, tt, 0:1], in1=w1_.t[:, 7:8], op=ALU.mult), reads=[w1_], writes=[gates])
                for kk in range(2):
                    k.op("dve", lambda e, kk=kk: e.tensor_tensor(
                        out=OH.t[:, tt, kk, :].rearrange("p (g j) -> p g j", j=8),
                        in0=w2_.t[:, 0:8].unsqueeze(2).to_broadcast([128, 8, 8]),
                        in1=w4_.t[:, 8 + 16 * kk:16 + 16 * kk].unsqueeze(1).to_broadcast([128, 8, 8]), op=ALU.mult),
                         reads=[w2_, w4_], writes=[OH])
                k.op("dve", lambda e: e.tensor_tensor(out=Mb.t[:], in0=OH.t[:, tt, 0, :], in1=OH.t[:, tt, 1, :], op=ALU.add), reads=[OH], writes=[Mb])
                pr = nxt(pA, "pa")
                k.op("pe", lambda e: e.matmul(pr.t[:, 0:NE], lhsT=ltri_bf.t[:], rhs=Mb.t[:], start=True, stop=True), reads=[ltri_bf, Mb], writes=[pr])
                k.op("dve", lambda e: e.tensor_tensor(out=RANK.t[:, tt, :], in0=pr.t[:, 0:NE], in1=cum.t[:], op=ALU.add), reads=[pr, cum], writes=[RANK])
                pc_ = nxt(pA, "pa")
                k.op("pe", lambda e: e.matmul(pc_.t[:, 0:NE], lhsT=ones_bf.t[:], rhs=Mb.t[:], start=True, stop=True), reads=[ones_bf, Mb], writes=[pc_])
                k.op("dve", lambda e: e.tensor_tensor(out=cum.t[:], in0=cum.t[:], in1=pc_.t[:, 0:NE], op=ALU.add), reads=[pc_], writes=[cum])
            k.op("dve", lambda e: e.tensor_scalar(out=pcs[0].t[:], in0=cum.t[:], scalar1=float(BLK - 1), scalar2=None, op0=ALU.add), reads=[cum], writes=[pcs[0]])
            k.op("dve", lambda e: e.tensor_copy(out=pci.t[:], in_=pcs[0].t[:]), reads=[pcs[0]], writes=[pci])
            sh = int(math.log2(BLK))
            k.op("dve", lambda e: e.tensor_single_scalar(out=pci.t[:], in_=pci.t[:], scalar=sh, op=ALU.arith_shift_right), writes=[pci])
            k.op("dve", lambda e: e.tensor_single_scalar(out=pci.t[:], in_=pci.t[:], scalar=sh, op=ALU.logical_shift_left), writes=[pci])
            k.op("dve", lambda e: e.tensor_copy(out=pcs[0].t[:], in_=pci.t[:]), reads=[pci], writes=[pcs[0]])
            k.op("dve", lambda e: e.tensor_copy(out=pcs[1].t[:], in_=pcs[0].t[:]), reads=[pcs[0]], writes=[pcs[1]])
            cur, oth = pcs[1], pcs[2]
            stp = 1
            while stp < NE:
                k.op("dve", lambda e, stp=stp, cur=cur, oth=oth: e.tensor_copy(out=oth.t[:, 0:stp], in_=cur.t[:, 0:stp]), reads=[cur], writes=[oth])
                k.op("dve", lambda e, stp=stp, cur=cur, oth=oth: e.tensor_tensor(out=oth.t[:, stp:NE], in0=cur.t[:, stp:NE], in1=cur.t[:, 0:NE - stp], op=ALU.add),
                     reads=[cur], writes=[oth])
                cur, oth = oth, cur
                stp *= 2
            pend = cur
            pstart = oth
            k.op("dve", lambda e: e.tensor_tensor(out=pstart.t[:], in0=pend.t[:], in1=pcs[0].t[:], op=ALU.subtract), reads=[pend, pcs[0]], writes=[pstart])
            for tt in range(NT):
                k.op("dve", lambda e, tt=tt: e.tensor_tensor(out=RANK.t[:, tt, :], in0=RANK.t[:, tt, :], in1=pstart.t[:], op=ALU.add), reads=[pstart], writes=[RANK])
                for kk in range(2):
                    k.op("dve", lambda e, tt=tt, kk=kk: e.tensor_tensor(out=rt[2].t[:, 0:NE], in0=RANK.t[:, tt, :], in1=OH.t[:, tt, kk, :], op=ALU.mult),
                         reads=[RANK, OH], writes=[rt[2]])
                    k.op("dve", lambda e, tt=tt, kk=kk: e.tensor_reduce(out=destf.t[:, tt * 2 + kk:tt * 2 + kk + 1], in_=rt[2].t[:, 0:NE], axis=AX.X, op=ALU.add),
                         reads=[rt[2]], writes=[destf])
            k.op("dve", lambda e: e.tensor_copy(out=desti.t[:], in_=destf.t[:]), reads=[destf], writes=[desti])
            k.op("dve", lambda e: e.tensor_tensor(out=cmpb.t[:], in0=pend.t[:].unsqueeze(1).to_broadcast([128, NB, NE]),
                                                  in1=thr.t[:].unsqueeze(2).to_broadcast([128, NB, NE]), op=ALU.is_le), reads=[pend, thr], writes=[cmpb])
            k.op("dve", lambda e: e.tensor_reduce(out=bef.t[:], in_=cmpb.t[:], axis=AX.X, op=ALU.add), reads=[cmpb], writes=[bef])
            k.op("dve", lambda e: e.tensor_scalar(out=bef.t[:], in0=bef.t[:], scalar1=128.0, scalar2=None, op0=ALU.mult), writes=[bef])
            k.op("dve", lambda e: e.tensor_scalar(out=bef.t[:], in0=bef.t[:], scalar1=iota_p, scalar2=None, op0=ALU.add), reads=[cst], writes=[bef])
            k.op("dve", lambda e: e.tensor_copy(out=widx.t[:], in_=bef.t[:]), reads=[bef], writes=[widx])

            k.op("dve", lambda e: e.memset(junk.t[:], 0.0), writes=[junk])
            RPP = PR // 128
            zc = 24
            xs_v = Xs.rearrange("(p r) d -> p r d", p=128)
            for z0 in range(0, RPP, zc):
                zn = min(zc, RPP - z0)
                k.dma("sp", lambda e, z0=z0, zn=zn: e.dma_start(out=xs_v[:, z0:z0 + zn, :],
                                                                 in_=junk.t[:].unsqueeze(1).to_broadcast([128, zn, D])),
                      reads=[junk], writes=[R("xs")], accumulate_writes=True)
            k.barrier()
            for tt in range(NT):
                hb = nxt(hb2, "hb")
                k.dma("sp", lambda e: e.dma_start(out=hb.t[:], in_=Hrows[tt * 128:(tt + 1) * 128, :]), reads=[R("hrows", tt)], writes=[hb])
                for kk in range(2):
                    k.dma("pool", lambda e, kk=kk: e.indirect_dma_start(
                        out=Xs[:, :], out_offset=bass.IndirectOffsetOnAxis(ap=desti.t[:, tt * 2 + kk:tt * 2 + kk + 1], axis=0),
                        in_=hb.t[:], in_offset=None), reads=[hb, desti], writes=[R("xs")], accumulate_writes=True)

            if "moe" in dbg and l == 0:
                k.dma("sp", lambda e: e.dma_start(out=dbg["moe"][:, 0:NT * 2], in_=destf.t[:]), reads=[destf], writes=[R("dbg", "moe")])
                k.dma("sp", lambda e: e.dma_start(out=dbg["moe"][:, NT * 2:NT * 2 + NB], in_=bef.t[:]), reads=[bef], writes=[R("dbg", "moe2")])
                k.dma("sp", lambda e: e.dma_start(out=dbg["moe"][:, NT * 2 + NB:NT * 4 + NB], in_=gates.t[:].rearrange("p t k -> p (t k)")),
                      reads=[gates], writes=[R("dbg", "moe3")])
            st_.close()

            st_ = Stage()
            yo2 = [st_.sb("yo", [128, D], F32) for _ in range(2)]
            xsb = [st_.sb("xsb", [128, 2, D], BF16) for i in range(2)]
            xTb = st_.sb("xTb", [128, KC, BLK], BF16)
            hid = st_.sb("hid", [128, FC, BLK], BF16)
            w1b = [st_.sb("w1b", [128, KC, DE], BF16) for i in range(2)]
            w3b = [st_.sb("w3b", [128, KC, DE], BF16) for i in range(2)]
            w2b = [st_.sb("w2b", [128, FC, D], BF16) for i in range(2)]

            bc_reg = nc.gpsimd.to_reg(NE * 128 - 1)

            def load_block(b):
                i = b % 2
                k.dma("sp", lambda e: e.dma_start(out=xsb[i].t[:], in_=Xs[b * BLK:(b + 1) * BLK, :].rearrange("(r p) d -> p r d", p=128)),
                      reads=[R("xs")], writes=[xsb[i]])
                for wsb, wdr, nch, g in ((w1b[i], w1r, NCH, GK), (w3b[i], w3r, NCH, GK), (w2b[i], w2r, FC, 1)):
                    for c in range(nch):
                        k.dma("pool", lambda e, wsb=wsb, wdr=wdr, c=c, g=g: e.indirect_dma_start(
                            out=wsb.t[:, c * g:(c + 1) * g, :].rearrange("p a b -> p (a b)"), out_offset=None, in_=wdr[l][c][:, :],
                            in_offset=bass.IndirectOffsetOnAxis(ap=widx.t[:, b:b + 1], axis=0),
                            bounds_check=bc_reg, oob_is_err=False), reads=[widx], writes=[wsb],
                              accumulate_writes=(c > 0))

            load_block(0)
            for b in range(NB):
                i = b % 2
                if b + 1 < NB:
                    load_block(b + 1)
                for r in range(2):
                    for kc in range(KC):
                        k.op("pe", lambda e, kc=kc, r=r: e.transpose(out=pT.t[:, kc, :], in_=xsb[i].t[:, r, kc * 128:(kc + 1) * 128], identity=ident_bf.t[:]),
                             reads=[xsb[i], ident_bf], writes=[pT])
                    k.op("act", lambda e, r=r: e.activation(out=xTb.t[:, :, r * 128:(r + 1) * 128], in_=pT.t[:], func=AF.Copy), reads=[pT], writes=[xTb])
                for fc in range(FC):
                    p1 = nxt(pA, "pa")
                    p3 = nxt(pA, "pa")
                    for kc in range(KC):
                        k.op("pe", lambda e, kc=kc: e.matmul(p1.t[:, 0:BLK], lhsT=w1b[i].t[:, kc, fc * 128:(fc + 1) * 128], rhs=xTb.t[:, kc, :],
                                                             start=(kc == 0), stop=(kc == KC - 1)), reads=[w1b[i], xTb], writes=[p1])
                    for kc in range(KC):
                        k.op("pe", lambda e, kc=kc: e.matmul(p3.t[:, 0:BLK], lhsT=w3b[i].t[:, kc, fc * 128:(fc + 1) * 128], rhs=xTb.t[:, kc, :],
                                                             start=(kc == 0), stop=(kc == KC - 1)), reads=[w3b[i], xTb], writes=[p3])
                    sg = nxt(f512, "f")
                    k.op("act", lambda e: e.activation(out=sg.t[:, 0:BLK], in_=p1.t[:, 0:BLK], func=AF.Silu), reads=[p1], writes=[sg])
                    k.op("dve", lambda e: e.tensor_tensor(out=hid.t[:, fc, :], in0=p3.t[:, 0:BLK], in1=sg.t[:, 0:BLK], op=ALU.mult),
                         reads=[p3, sg], writes=[hid])
                for r in range(2):
                    yo = nxt(yo2, "yo")
                    for nb in range(4):
                        pp = nxt(pA, "pa")
                        for fc in range(FC):
                            k.op("pe", lambda e, fc=fc: e.matmul(pp.t[:], lhsT=hid.t[:, fc, r * 128:(r + 1) * 128], rhs=w2b[i].t[:, fc, nb * 512:(nb + 1) * 512],
                                                                 start=(fc == 0), stop=(fc == FC - 1)), reads=[hid, w2b[i]], writes=[pp])
                        if nb % 2 == 0:
                            k.op("act", lambda e, nb=nb: e.activation(out=yo.t[:, nb * 512:(nb + 1) * 512], in_=pp.t[:], func=AF.Copy), reads=[pp], writes=[yo])
                        else:
                            k.op("dve", lambda e, nb=nb: e.tensor_copy(out=yo.t[:, nb * 512:(nb + 1) * 512], in_=pp.t[:]), reads=[pp], writes=[yo])
                    rr0 = b * BLK + r * 128
                    k.dma("sp", lambda e: e.dma_start(out=Ys[rr0:rr0 + 128, :], in_=yo.t[:]), reads=[yo], writes=[R("ys")], accumulate_writes=True)
            st_.close()

            st_ = Stage()
            xt2 = [st_.sb("xt", [128, D], F32) for _ in range(2)]
            y02 = [st_.sb("y0", [128, D], F32) for _ in range(2)]
            y12 = [st_.sb("y1", [128, D], F32) for _ in range(2)]
            dst = out if last else xres
            for tt in range(NT):
                r0 = tt * 128
                xt = nxt(xt2, "x")
                k.dma("sp", lambda e: e.dma_start(out=xt.t[:], in_=xres[r0:r0 + 128, :]), reads=[R("xres", tt)], writes=[xt])
                ys = [nxt(y02, "y0"), nxt(y12, "y1")]
                for kk in range(2):
                    k.dma("pool", lambda e, kk=kk: e.indirect_dma_start(
                        out=ys[kk].t[:], out_offset=None, in_=Ys[:, :],
                        in_offset=bass.IndirectOffsetOnAxis(ap=desti.t[:, tt * 2 + kk:tt * 2 + kk + 1], axis=0)),
                          reads=[R("ys"), desti], writes=[ys[kk]])
                k.op("dve", lambda e: e.scalar_tensor_tensor(out=xt.t[:], in0=ys[0].t[:], scalar=gates.t[:, tt, 0:1], in1=xt.t[:],
                                                             op0=ALU.mult, op1=ALU.add), reads=[ys[0], gates], writes=[xt])
                k.op("dve", lambda e: e.scalar_tensor_tensor(out=xt.t[:], in0=ys[1].t[:], scalar=gates.t[:, tt, 1:2], in1=xt.t[:],
                                                             op0=ALU.mult, op1=ALU.add), reads=[ys[1], gates], writes=[xt])
                k.dma("sp", lambda e: e.dma_start(out=dst[r0:r0 + 128, :], in_=xt.t[:]), reads=[xt],
                      writes=[R("out" if last else "xres", tt)])
            st_.close()
        LS.close()

    k.barrier()
    return nc, k


def t5_bucket_np(dist):
    max_exact = 16
    d_f = np.maximum(dist, 1).astype(np.float32)
    large = max_exact + (np.log(d_f / np.float32(max_exact)) / np.float32(math.log(128 / max_exact)) * np.float32(16)).astype(np.int32)
    large = np.minimum(large, 31)
    return np.where(dist < max_exact, dist, large)


def host_prep(inp, L, S, DE):
    f = lambda a: np.ascontiguousarray(np.asarray(a, dtype=np.float32))
    rep = lambda v: np.broadcast_to(np.asarray(v, np.float32).reshape(1, -1), (128, np.asarray(v).size))
    pars = np.zeros((L, 128, NPAR), np.float32)

    def put(l, name, arr):
        o, w = _off[name]
        pars[l, :, o:o + w] = arr.reshape(128, w)

    tbl = np.asarray(inp["rel_bias_table"], np.float32)
    for l in range(L):
        put(l, "gmixT", np.asarray(inp["g_mix"][l]).reshape(KC, 128).T)
        put(l, "gcrossT", np.asarray(inp["g_cross"][l]).reshape(KC, 128).T)
        put(l, "gmemT", np.asarray(inp["g_mem"][l]).reshape(KC, 128).T)
        put(l, "gq2", np.tile(np.asarray(inp["g_q"][l]), 2))
        put(l, "gk2", np.tile(np.asarray(inp["g_k"][l]), 2))
        put(l, "gsub", np.asarray(inp["g_subln"][l]))
        put(l, "gqc", np.asarray(inp["g_qc"][l]))
        put(l, "gkc", np.asarray(inp["g_kc"][l]))
        cw = np.asarray(inp["conv_w"][l])
        put(l, "convw", cw.reshape(CW, 8, 128).transpose(2, 1, 0))
        put(l, "convb", np.asarray(inp["conv_b"][l]).reshape(8, 128).T)
        put(l, "lng", np.asarray(inp["conv_ln_g"][l]).reshape(8, 128).T)
        put(l, "lnb", np.asarray(inp["conv_ln_b"][l]).reshape(8, 128).T)
        put(l, "dl", rep(np.asarray(inp["diff_lambda"][l]).reshape(-1)))
        put(l, "brt", rep(np.concatenate([np.asarray(inp["b_group"][l]), np.asarray(inp["b_router"][l])])))
        put(l, "relc", rep(tbl[31, :]))
    gffnb = np.stack([rep(inp["g_ffn"][l]) for l in range(L)]).astype(np.float32)
    kk_, qq_ = np.meshgrid(np.arange(128), np.arange(128), indexing="ij")
    biasT = np.zeros((128, NH * 2, 128), np.float32)
    for dlt in range(2):
        dist = dlt * 128 + qq_ - kk_
        bk = t5_bucket_np(np.maximum(dist, 0))
        for h in range(NH):
            biasT[:, h * 2 + dlt, :] = np.where(dist >= 0, tbl[bk, h], np.float32(MASKV))
    consts = np.zeros((128, 5, 128), np.float32)
    consts[:, 0, :] = np.eye(128)
    consts[:, 1, :] = 1.0
    consts[0:64, 2, 0:64] = 1.0
    consts[64:128, 2, 64:128] = 1.0
    consts[:, 3, :] = (kk_ < qq_)
    consts[:, 4, :] = np.arange(128).reshape(128, 1)
    w_rt = np.concatenate([np.asarray(inp["w_group"], np.float32)[:L], np.asarray(inp["w_router"], np.float32)[:L]], axis=-1)
    FC = DE // 128
    shared = {
        "pars": pars, "gffnb": gffnb, "biasT": biasT, "consts": consts,
        "w_in": f(inp["w_in"][:L]), "w_out": f(inp["w_out"][:L]), "wq_c": f(inp["wq_c"][:L]), "wkv_c": f(inp["wkv_c"][:L]),
        "wo_c": f(inp["wo_c"][:L]), "w_rt": f(w_rt),
    }
    if "F" in STAGES:
        NCH = KC * DE // 2048
        GK = 2048 // DE
        for nm, src_ in (("w1r", "w1"), ("w3r", "w3")):
            for l in range(L):
                a6 = np.asarray(inp[src_][l], np.float32).reshape(NE, NCH, GK, 128, DE)
                for c in range(NCH):
                    shared[f"{nm}_{l}_{c}"] = f(a6[:, c].transpose(0, 2, 1, 3).reshape(NE * 128, GK * DE))
        for l in range(L):
            a5 = np.asarray(inp["w2"][l], np.float32).reshape(NE, FC, 128, D)
            for c in range(FC):
                shared[f"w2r_{l}_{c}"] = f(a5[:, c].reshape(NE * 128, D))
    return shared


_CACHE = {}


def run(inp, L, NSEQ, S, DE, debug=()):
    key = (L, NSEQ, S, DE, tuple(debug))
    if key not in _CACHE:
        _CACHE[key] = build_program(L, NSEQ, S, DE, debug)
    nc, kb = _CACHE[key]
    shared = host_prep(inp, L, S, DE)
    T = NSEQ * S
    NB = -(-(2 * T + NE * (BLK - 1)) // BLK)
    shared["thr"] = np.ascontiguousarray(np.broadcast_to((np.arange(NB, dtype=np.float32) * BLK).reshape(1, NB), (128, NB)))
    x = np.asarray(inp["x"], np.float32)
    mem = np.asarray(inp["mem"], np.float32)
    in_maps = []
    for c in range(NCORES):
        m = dict(shared)
        m["x"] = np.ascontiguousarray(x[c * NSEQ:(c + 1) * NSEQ].reshape(T, D))
        m["mem"] = np.ascontiguousarray(mem[c * NSEQ:(c + 1) * NSEQ].reshape(NSEQ * MEM, D))
        in_maps.append(m)
    res = run_bass_kernel_spmd(nc, in_maps, core_ids=list(range(NCORES)))
    return res


def kernel(**inputs):
    L, NSEQ, S, DE = 2, 2, 2048, 512
    res = run(inputs, L, NSEQ, S, DE)
    outs = [r["out"].reshape(NSEQ, S, D) for r in res.results]
    return np.concatenate(outs, axis=0).astype(np.float32)
```

```python
import math
from contextlib import ExitStack

import numpy as np
import concourse.bass as bass
import concourse.mybir as mybir
from concourse.bass_utils import run_bass_kernel_spmd

AF = mybir.ActivationFunctionType
ALU = mybir.AluOpType
AX = mybir.AxisListType
F32 = mybir.dt.float32
BF16 = mybir.dt.bfloat16
I32 = mybir.dt.int32

D = 2048
KC = D // 128
DQ = 64
NH = 8
D_IN = 5120
CW = 31
MEM = 256
NHC = 4
NG = 8
NE = 64
BLK = 256
EPS = 1e-6
NCORES = 8
MASKV = -30000.0
STAGES = "ABCDEF"

_off = {}
_n = 0
for _name, _w in [("gmixT", 16), ("gcrossT", 16), ("gmemT", 16), ("gq2", 1), ("gk2", 1), ("gsub", 1),
                  ("gqc", 1), ("gkc", 1), ("convw", 8 * CW), ("convb", 8), ("lng", 8), ("lnb", 8),
                  ("dl", 4 * DQ), ("brt", 72), ("relc", 8)]:
    _off[_name] = (_n, _w)
    _n += _w
NPAR = _n


class Buf:
    __slots__ = ("t", "w", "r")

    def __init__(self, t):
        self.t = t
        self.w = []
        self.r = []


class KB:
    NRING = 64

    def __init__(self, nc, es):
        self.nc = nc
        self.es = es
        self.eng = {"pe": nc.tensor, "dve": nc.vector, "act": nc.scalar, "pool": nc.gpsimd, "sp": nc.sync}
        self.chan = {}
        self.waited = {e: {} for e in self.eng}
        self.ring = [es.enter_context(nc.semaphore(f"dq{i}")) for i in range(self.NRING)]
        self.ring_cnt = [0] * self.NRING
        self.ring_i = 0
        self.nsem = 0
        self.ninstr = 0

    def _chan(self, e):
        c = self.chan.get(e)
        if c is None or c[2] >= 30000:
            self.nsem += 1
            c = [f"c{e}{self.nsem}", self.es.enter_context(self.nc.semaphore(f"c_{e}_{self.nsem}")), 0]
            self.chan[e] = c
        return c

    def _wait(self, e, toks):
        wd = self.waited[e]
        for (key, sem, v, prod) in toks:
            if prod == e and e == "pe":
                continue
            if wd.get(key, 0) >= v:
                continue
            wd[key] = v
            self.eng[e].wait_ge(sem, v)

    @staticmethod
    def _prune(lst):
        last = {}
        out = []
        for t in lst:
            if t[3] == "dma":
                out.append(t)
            else:
                last[t[3]] = t
        return out + list(last.values())

    def _deps(self, e, reads, writes):
        toks = []
        for b in reads:
            toks += b.w
        for b in writes:
            toks += b.w
            toks += b.r
        self._wait(e, toks)

    def _commit(self, tok, reads, writes):
        for b in reads:
            b.r.append(tok)
            if len(b.r) > 6:
                b.r = self._prune(b.r)
        for b in writes:
            b.w = [tok]
            b.r = []

    def op(self, e, fn, reads=(), writes=()):
        self._deps(e, reads, writes)
        ins = fn(self.eng[e])
        c = self._chan(e)
        c[2] += 1
        ins.then_inc(c[1], 1)
        tok = (c[0], c[1], c[2], e)
        self._commit(tok, reads, writes)
        self.ninstr += 1
        return tok

    def dma(self, q, fn, reads=(), writes=(), accumulate_writes=False):
        if accumulate_writes:
            toks = []
            for b in reads:
                toks += b.w
            for b in writes:
                if b.r:
                    toks += b.w
                    toks += b.r
            self._wait(q, toks)
        else:
            self._deps(q, reads, writes)
        i = self.ring_i
        self.ring_i = (i + 1) % self.NRING
        if self.ring_cnt[i] > 0:
            self._wait(q, [(f"dq{i}", self.ring[i], self.ring_cnt[i], "dma")])
        ins = fn(self.eng[q])
        self.ring_cnt[i] += 16
        ins.then_inc(self.ring[i], 16)
        tok = (f"dq{i}", self.ring[i], self.ring_cnt[i], "dma")
        for b in reads:
            b.r.append(tok)
        for b in writes:
            if accumulate_writes and not b.r:
                b.w.append(tok)
            else:
                b.w = [tok]
                b.r = []
        self.ninstr += 1
        return tok

    def barrier(self):
        toks = [(c[0], c[1], c[2], e) for e, c in self.chan.items() if c[2] > 0]
        toks += [(f"dq{i}", self.ring[i], self.ring_cnt[i], "dma") for i in range(self.NRING) if self.ring_cnt[i] > 0]
        for e in self.eng:
            self._wait(e, toks)

    def drain(self, e, bufs):
        toks = []
        for b in bufs:
            toks += b.w
            toks += b.r
        self._wait(e, toks)


def build_program(L, NSEQ, S, DE, debug=()):
    T = NSEQ * S
    NT = T // 128
    NB = -(-(2 * T + NE * (BLK - 1)) // BLK)
    PR = NB * BLK
    FC = DE // 128
    nc = bass.Bass("TRN2", target_bir_lowering=False)
    es = ExitStack()

    def dram(name, shape, dt, kind="Internal"):
        return nc.dram_tensor(name, list(shape), dt, kind=kind).ap()

    x_in = dram("x", [T, D], F32, "ExternalInput")
    mem_in = dram("mem", [NSEQ * MEM, D], F32, "ExternalInput")
    pars_in = dram("pars", [L, 128, NPAR], F32, "ExternalInput")
    gffn_in = dram("gffnb", [L, 128, D], F32, "ExternalInput")
    biasT_in = dram("biasT", [128, NH * 2, 128], F32, "ExternalInput")
    consts_in = dram("consts", [128, 5, 128], F32, "ExternalInput")
    thr_in = dram("thr", [128, NB], F32, "ExternalInput")
    w_in = dram("w_in", [L, D, D_IN], F32, "ExternalInput")
    w_out = dram("w_out", [L, D, D], F32, "ExternalInput")
    wq_c = dram("wq_c", [L, D, 512], F32, "ExternalInput")
    wkv_c = dram("wkv_c", [L, D, 1024], F32, "ExternalInput")
    wo_c = dram("wo_c", [L, 512, D], F32, "ExternalInput")
    w_rt = dram("w_rt", [L, D, 72], F32, "ExternalInput")
    if "F" in STAGES:
        NCH = KC * DE // 2048
        GK = 2048 // DE
        w1r = [[dram(f"w1r_{l_}_{c}", [NE * 128, 2048], F32, "ExternalInput") for c in range(NCH)] for l_ in range(L)]
        w3r = [[dram(f"w3r_{l_}_{c}", [NE * 128, 2048], F32, "ExternalInput") for c in range(NCH)] for l_ in range(L)]
        w2r = [[dram(f"w2r_{l_}_{c}", [NE * 128, 2048], F32, "ExternalInput") for c in range(FC)] for l_ in range(L)]
    out = dram("out", [T, D], F32, "ExternalOutput")

    xres = dram("xres", [T, D], F32)
    qTs = dram("qTs", [NSEQ, NH, 128, S], BF16)
    kTs = dram("kTs", [NSEQ, NH, 128, S], BF16)
    Vs = dram("Vs", [NSEQ, S, NH * 128], BF16)
    Us = dram("Us", [NSEQ, 8, 128, S], F32)
    mixT = dram("mixT", [16, 128, T], BF16)
    Hrows = dram("Hrows", [T, D], BF16)
    Xs = dram("Xs", [PR, D], BF16)
    Ys = dram("Ys", [PR, D], F32)
    dbg = {}
    for name, shape, dt in debug:
        dbg[name] = dram("dbg_" + name, shape, dt, "ExternalOutput")

    k = KB(nc, es)
    regions = {}
    uid = [0]

    def R(*key):
        b = regions.get(key)
        if b is None:
            b = Buf(None)
            regions[key] = b
        return b

    class Stage:
        def __init__(self):
            self.es = ExitStack()

        def sb(self, name, shape, dt):
            uid[0] += 1
            return Buf(self.es.enter_context(nc.sbuf_tensor(f"{name}_{uid[0]}", list(shape), dt)))

        def close(self):
            k.barrier()
            self.es.close()

    G = Stage()

    def ps(name, shape, dt=F32):
        return Buf(es.enter_context(nc.psum_tensor(name, list(shape), dt)))

    cst = G.sb("cst", [128, 5, 128], F32)
    ident_bf = G.sb("ident_bf", [128, 128], BF16)
    ones_bf = G.sb("ones_bf", [128, 128], BF16)
    ltri_bf = G.sb("ltri_bf", [128, 128], BF16)
    biasT = G.sb("biasT_sb", [128, NH * 2, 128], BF16)
    pars = G.sb("pars_sb", [128, NPAR], F32)
    small = G.sb("small", [128, 16], F32)
    col = [G.sb(f"col{i}", [128, 8], F32) for i in range(4)]
    f512 = [G.sb(f"f512_{i}", [128, 512], F32) for i in range(6)]
    b512 = [G.sb(f"b512_{i}", [128, 512], BF16) for i in range(4)]
    ones_f = cst.t[:, 1, :]
    bd64_f = cst.t[:, 2, :]
    iota_p = cst.t[:, 4, 0:1]
    pT = ps("pT", [128, KC, 128], BF16)
    pA = [ps(f"pA{i}", [128, 512]) for i in range(6)]

    k.dma("sp", lambda e: e.dma_start(out=cst.t[:], in_=consts_in[:, :, :]), writes=[cst])
    k.dma("pool", lambda e: e.dma_start(out=biasT.t[:], in_=biasT_in[:, :, :]), writes=[biasT])
    k.op("dve", lambda e: e.tensor_copy(out=ident_bf.t[:], in_=cst.t[:, 0, :]), reads=[cst], writes=[ident_bf])
    k.op("dve", lambda e: e.tensor_copy(out=ones_bf.t[:], in_=cst.t[:, 1, :]), reads=[cst], writes=[ones_bf])
    k.op("dve", lambda e: e.tensor_copy(out=ltri_bf.t[:], in_=cst.t[:, 3, :]), reads=[cst], writes=[ltri_bf])

    def P(name, j=0, w=1):
        o, _ = _off[name]
        return pars.t[:, o + j:o + j + w]

    cnt = {}

    def nxt(lst, key):
        i = cnt.get(key, 0)
        cnt[key] = i + 1
        return lst[i % len(lst)]

    def rms_tile(W, x_rows, xreg, gcolT, dst, dst_c0, a=1.0 / D):
        xt = nxt(W["xt"], "x")
        k.dma("sp", lambda e: e.dma_start(out=xt.t[:], in_=x_rows), reads=[xreg], writes=[xt])
        c0 = nxt(col, "c")
        junk = W["junk"]
        k.op("act", lambda e: e.activation(out=junk.t[:], in_=xt.t[:], func=AF.Square, accum_out=c0.t[:, 0:1]),
             reads=[xt], writes=[junk, c0])
        k.op("act", lambda e: e.activation(out=c0.t[:, 1:2], in_=c0.t[:, 0:1], func=AF.Sqrt, scale=a, bias=EPS),
             reads=[c0], writes=[c0])
        k.op("dve", lambda e: e.reciprocal(out=c0.t[:, 2:3], in_=c0.t[:, 1:2]), reads=[c0], writes=[c0])
        hb = nxt(W["hb"], "hb")
        k.op("act", lambda e: e.activation(out=hb.t[:], in_=xt.t[:], func=AF.Copy, scale=c0.t[:, 2:3]),
             reads=[xt, c0], writes=[hb])
        for kc in range(KC):
            k.op("pe", lambda e, kc=kc: e.transpose(out=pT.t[:, kc, :], in_=hb.t[:, kc * 128:(kc + 1) * 128],
                                                    identity=ident_bf.t[:]),
                 reads=[hb, ident_bf], writes=[pT])
        k.op("dve", lambda e: e.tensor_tensor(out=dst.t[:, :, dst_c0:dst_c0 + 128], in0=pT.t[:],
                                              in1=gcolT.unsqueeze(2).to_broadcast([128, KC, 128]), op=ALU.mult),
             reads=[pT, pars], writes=[dst])
        return xt

    def load_w(W, dram_view, cols, width=512):
        wb = nxt(W["wb"], "w")
        k.dma("pool", lambda e: e.dma_start(out=wb.t[:, :, 0:width], in_=dram_view[:, :, cols:cols + width]),
              writes=[wb])
        return wb

    def fm_norm(psrc, gcol, a, bconst, n, ones_mat):
        sq = nxt(f512, "f")
        k.op("act", lambda e: e.activation(out=sq.t[:, 0:n], in_=psrc.t[:, 0:n], func=AF.Square), reads=[psrc], writes=[sq])
        pss = nxt(pA, "pa")
        k.op("pe", lambda e: e.matmul(pss.t[:, 0:n], lhsT=ones_mat, rhs=sq.t[:, 0:n], start=True, stop=True),
             reads=[sq, cst], writes=[pss])
        rs = nxt(f512, "f")
        k.op("act", lambda e: e.activation(out=rs.t[:, 0:n], in_=pss.t[:, 0:n], func=AF.Sqrt, scale=a, bias=bconst),
             reads=[pss], writes=[rs])
        k.op("dve", lambda e: e.reciprocal(out=rs.t[:, 0:n], in_=rs.t[:, 0:n]), reads=[rs], writes=[rs])
        ob = nxt(b512, "b")
        k.op("dve", lambda e: e.scalar_tensor_tensor(out=ob.t[:, 0:n], in0=psrc.t[:, 0:n], scalar=gcol, in1=rs.t[:, 0:n],
                                                     op0=ALU.mult, op1=ALU.mult),
             reads=[psrc, rs, pars, small], writes=[ob])
        return ob

    def rmsW(st):
        return {"xt": [st.sb("xt", [128, D], F32) for _ in range(2)],
                "hb": [st.sb("hb", [128, D], BF16) for _ in range(2)],
                "junk": st.sb("junk", [128, D], BF16)}

    x_src = x_in
    x_key = "xin"
    for l in range(L):
        last = (l == L - 1)
        lam_init = 0.8 - 0.6 * math.exp(-0.3 * l)
        LS = Stage()
        k.dma("sp", lambda e: e.dma_start(out=pars.t[:], in_=pars_in[l, :, :]), writes=[pars])
        o_dl = _off["dl"][0]
        tmpc = f512[0]
        for i in range(2):
            k.op("dve", lambda e, i=i: e.tensor_tensor(out=tmpc.t[:, 0:DQ], in0=pars.t[:, o_dl + 2 * i * DQ:o_dl + (2 * i + 1) * DQ],
                                                       in1=pars.t[:, o_dl + (2 * i + 1) * DQ:o_dl + (2 * i + 2) * DQ], op=ALU.mult),
                 reads=[pars], writes=[tmpc])
            k.op("dve", lambda e, i=i: e.tensor_reduce(out=small.t[:, 2 + i:3 + i], in_=tmpc.t[:, 0:DQ], axis=AX.X, op=ALU.add),
                 reads=[tmpc], writes=[small])
        k.op("act", lambda e: e.activation(out=small.t[:, 4:6], in_=small.t[:, 2:4], func=AF.Exp), reads=[small], writes=[small])
        k.op("dve", lambda e: e.scalar_tensor_tensor(out=small.t[:, 0:1], in0=small.t[:, 5:6], scalar=-lam_init, in1=small.t[:, 4:5],
                                                     op0=ALU.add, op1=ALU.subtract), reads=[small], writes=[small])
        k.op("dve", lambda e: e.tensor_scalar(out=small.t[:, 1:2], in0=P("gsub"), scalar1=1.0 - lam_init, scalar2=None, op0=ALU.mult),
             reads=[pars], writes=[small])
        neglam = small.t[:, 0:1]
        gsubs = small.t[:, 1:2]

        if "A" in STAGES:
            st_ = Stage()
            W = rmsW(st_)
            W["wb"] = [st_.sb("wb", [128, KC, 512], BF16) for _ in range(3)]
            hT = st_.sb("hT", [128, KC, S], BF16)
            w_in_v = w_in[l].rearrange("(kc p) n -> p kc n", p=128)
            w_order = [(s_, nb_) for s_ in range(NSEQ) for nb_ in (0, 1, 2, 3, 4, 5, 6, 8, 7, 9)]
            w_loaded = {}
            w_pos = [0]

            def get_w(s_, nb_):
                target = min(w_order.index((s_, nb_)) + 1, len(w_order) - 1)
                while w_pos[0] <= target:
                    key_ = w_order[w_pos[0]]
                    w_loaded[key_] = load_w(W, w_in_v, key_[1] * 512)
                    w_pos[0] += 1
                return w_loaded[(s_, nb_)]

            for s in range(NSEQ):
                for tt in range(S // 128):
                    r0 = s * S + tt * 128
                    rms_tile(W, x_src[r0:r0 + 128, :], R(x_key, r0 // 128), P("gmixT", 0, 16), hT, tt * 128)
                for nb in range(4):
                    wb = get_w(s, nb)
                    isq = nb < 2
                    dstT = qTs if isq else kTs
                    gcol = P("gq2") if isq else P("gk2")
                    a, bc = (1.0, DQ * EPS) if isq else (1.0 / DQ, EPS)
                    for j in range(4):
                        head = (nb % 2) * 4 + j
                        for tb in range(S // 512):
                            pp = nxt(pA, "pa")
                            for kc in range(KC):
                                k.op("pe", lambda e, kc=kc: e.matmul(pp.t[:], lhsT=wb.t[:, kc, j * 128:(j + 1) * 128],
                                                                     rhs=hT.t[:, kc, tb * 512:(tb + 1) * 512],
                                                                     start=(kc == 0), stop=(kc == KC - 1)),
                                     reads=[wb, hT], writes=[pp])
                            ob = fm_norm(pp, gcol, a, bc, 512, bd64_f)
                            k.dma("sp", lambda e: e.dma_start(out=dstT[s, head, :, tb * 512:(tb + 1) * 512], in_=ob.t[:]),
                                  reads=[ob], writes=[R("qk", isq, s, head)], accumulate_writes=True)
                for nb in range(4, 6):
                    wb = get_w(s, nb)
                    for tt in range(S // 128):
                        pp = nxt(pA, "pa")
                        for kc in range(KC):
                            k.op("pe", lambda e, kc=kc: e.matmul(pp.t[:], lhsT=hT.t[:, kc, tt * 128:(tt + 1) * 128], rhs=wb.t[:, kc, :],
                                                                 start=(kc == 0), stop=(kc == KC - 1)),
                                 reads=[wb, hT], writes=[pp])
                        ob = nxt(b512, "b")
                        k.op("act", lambda e: e.activation(out=ob.t[:], in_=pp.t[:], func=AF.Copy), reads=[pp], writes=[ob])
                        k.dma("sp", lambda e: e.dma_start(out=Vs[s, tt * 128:(tt + 1) * 128, (nb - 4) * 512:(nb - 3) * 512], in_=ob.t[:]),
                              reads=[ob], writes=[R("v", s)], accumulate_writes=True)
                for i in range(2):
                    wa = get_w(s, 6 + i)
                    wg = get_w(s, 8 + i)
                    for j in range(4):
                        ch = i * 4 + j
                        for tb in range(S // 512):
                            pa_ = nxt(pA, "pa")
                            pg_ = nxt(pA, "pa")
                            for kc in range(KC):
                                k.op("pe", lambda e, kc=kc: e.matmul(pa_.t[:], lhsT=wa.t[:, kc, j * 128:(j + 1) * 128],
                                                                     rhs=hT.t[:, kc, tb * 512:(tb + 1) * 512],
                                                                     start=(kc == 0), stop=(kc == KC - 1)),
                                     reads=[wa, hT], writes=[pa_])
                            for kc in range(KC):
                                k.op("pe", lambda e, kc=kc: e.matmul(pg_.t[:], lhsT=wg.t[:, kc, j * 128:(j + 1) * 128],
                                                                     rhs=hT.t[:, kc, tb * 512:(tb + 1) * 512],
                                                                     start=(kc == 0), stop=(kc == KC - 1)),
                                     reads=[wg, hT], writes=[pg_])
                            sg = nxt(f512, "f")
                            k.op("act", lambda e: e.activation(out=sg.t[:], in_=pg_.t[:], func=AF.Sigmoid), reads=[pg_], writes=[sg])
                            ub = nxt(f512, "f")
                            k.op("dve", lambda e: e.tensor_tensor(out=ub.t[:], in0=pa_.t[:], in1=sg.t[:], op=ALU.mult),
                                 reads=[pa_, sg], writes=[ub])
                            k.dma("sp", lambda e: e.dma_start(out=Us[s, ch, :, tb * 512:(tb + 1) * 512], in_=ub.t[:]),
                                  reads=[ub], writes=[R("u", s, ch)], accumulate_writes=True)
            st_.close()

        if "qT" in dbg and l == 0:
            k.dma("sp", lambda e: e.dma_start(out=dbg["qT"][:, :, :, :], in_=qTs[:, :, :, :]), writes=[R("dbg", "qT")])
            k.dma("sp", lambda e: e.dma_start(out=dbg["kT"][:, :, :, :], in_=kTs[:, :, :, :]), writes=[R("dbg", "kT")])
            k.dma("sp", lambda e: e.dma_start(out=dbg["V"][:, :, :], in_=Vs[:, :, :]), writes=[R("dbg", "V")])
            k.dma("sp", lambda e: e.dma_start(out=dbg["U"][:, :, :, :], in_=Us[:, :, :, :]), writes=[R("dbg", "U")])
            k.barrier()

        if "B" in STAGES:
            st_ = Stage()
            qh2 = [st_.sb("qh", [128, S], BF16) for i in range(2)]
            kh2 = [st_.sb("kh", [128, S], BF16) for i in range(2)]
            vh2 = [st_.sb("vh", [128, S // 128, 128], BF16) for i in range(2)]
            attn_r = [st_.sb("attn_r", [128, 512], F32) for i in range(2)]
            attn_t = [st_.sb("attn_t", [128, 512], F32) for i in range(2)]
            Ubuf2 = [st_.sb("Ubuf", [128, 32 + S], F32) for _ in range(2)]
            ctmp = [st_.sb("ctmp", [128, S], F32) for _ in range(2)]
            Yc = [st_.sb("Yc", [128, S], F32) for j in range(8)]
            mean = st_.sb("mean", [128, 512], F32)
            msq = st_.sb("msq", [128, 512], F32)
            var = st_.sb("var", [128, 512], F32)
            for ub_ in Ubuf2:
                k.op("pool", lambda e, ub_=ub_: e.memset(ub_.t[:, 0:32], 0.0), writes=[ub_])
            o_cw = _off["convw"][0]
            itc = [0]

            POOL_CH = 5
            ubd = st_.sb("Ubuf_d", [128, 32 + S], F32)
            k.op("dve", lambda e: e.memset(ubd.t[:, 0:32], 0.0), writes=[ubd])

            def dve_conv(s):
                for j in range(POOL_CH, 8):
                    k.dma("sp", lambda e: e.dma_start(out=ubd.t[:, 32:32 + S], in_=Us[s, j, :, :]), reads=[R("u", s, j)], writes=[ubd])
                    k.op("dve", lambda e: e.tensor_scalar(out=Yc[j].t[:], in0=ubd.t[:, 2:2 + S], scalar1=pars.t[:, o_cw + j * CW:o_cw + j * CW + 1],
                                                          scalar2=P("convb", j), op0=ALU.mult, op1=ALU.add),
                         reads=[ubd, pars], writes=[Yc[j]])
                    yield
                    for tap in range(1, CW):
                        k.op("dve", lambda e, tap=tap: e.scalar_tensor_tensor(
                            out=Yc[j].t[:], in0=ubd.t[:, 2 + tap:2 + tap + S], scalar=pars.t[:, o_cw + j * CW + tap:o_cw + j * CW + tap + 1],
                            in1=Yc[j].t[:], op0=ALU.mult, op1=ALU.add),
                             reads=[ubd, pars], writes=[Yc[j]])
                        yield

            def conv_seq(s):
                for j in range(POOL_CH):
                    ub = nxt(Ubuf2, "ub")
                    k.dma("pool", lambda e: e.dma_start(out=ub.t[:, 32:32 + S], in_=Us[s, j, :, :]), reads=[R("u", s, j)], writes=[ub])
                    k.op("pool", lambda e: e.tensor_scalar(out=Yc[j].t[:], in0=ub.t[:, 2:2 + S], scalar1=pars.t[:, o_cw + j * CW:o_cw + j * CW + 1],
                                                           scalar2=P("convb", j), op0=ALU.mult, op1=ALU.add),
                         reads=[ub, pars], writes=[Yc[j]])
                    for tap in range(1, CW):
                        tmp = nxt(ctmp, "ctmp")
                        k.op("pool", lambda e, tap=tap: e.tensor_scalar(out=tmp.t[:], in0=ub.t[:, 2 + tap:2 + tap + S],
                                                                        scalar1=pars.t[:, o_cw + j * CW + tap:o_cw + j * CW + tap + 1],
                                                                        scalar2=0.0, op0=ALU.mult, op1=ALU.add),
                             reads=[ub, pars], writes=[tmp])
                        k.op("pool", lambda e: e.tensor_tensor(out=Yc[j].t[:], in0=Yc[j].t[:], in1=tmp.t[:], op=ALU.add),
                             reads=[tmp], writes=[Yc[j]])

            def attn_seq(s):
                gen = dve_conv(s)
                for h in range(NH):
                    qh, kh, vh = qh2[itc[0] % 2], kh2[itc[0] % 2], vh2[itc[0] % 2]
                    itc[0] += 1
                    k.dma("sp", lambda e: e.dma_start(out=qh.t[:], in_=qTs[s, h, :, :]), reads=[R("qk", True, s, h)], writes=[qh])
                    k.dma("sp", lambda e: e.dma_start(out=kh.t[:], in_=kTs[s, h, :, :]), reads=[R("qk", False, s, h)], writes=[kh])
                    k.dma("sp", lambda e: e.dma_start(out=vh.t[:], in_=Vs[s, :, h * 128:(h + 1) * 128].rearrange("(t p) d -> p t d", p=128)),
                          reads=[R("v", s)], writes=[vh])
                    for qc in range(S // 512):
                        tms = []
                        for m in range(2):
                            pO = pA[2 * m]
                            pL = pA[2 * m + 1]
                            nk = (qc + 1) * 4

                            def emit_qk(kj, m=m, qc=qc):
                                q_lo = max(qc * 512, kj * 128)
                                c0 = q_lo - qc * 512
                                sT = nxt(pA[4:6], "pa_s")
                                blist = [dlt for dlt in (0, 1) if qc * 4 <= kj + dlt < (qc + 1) * 4]
                                k.op("pe", lambda e: e.matmul(sT.t[:, c0:512], lhsT=kh.t[m * 64:(m + 1) * 64, kj * 128:(kj + 1) * 128],
                                                              rhs=qh.t[m * 64:(m + 1) * 64, q_lo:(qc + 1) * 512],
                                                              start=True, stop=(len(blist) == 0)),
                                     reads=[kh, qh], writes=[sT])
                                for bi, dlt in enumerate(blist):
                                    cc = (kj + dlt) * 128 - qc * 512
                                    k.op("pe", lambda e, cc=cc, dlt=dlt, bi=bi: e.matmul(
                                        sT.t[:, cc:cc + 128], lhsT=ident_bf.t[:], rhs=biasT.t[:, h * 2 + dlt, :],
                                        start=False, stop=(bi == len(blist) - 1)),
                                         reads=[ident_bf, biasT], writes=[sT])
                                return sT, c0

                            nxt_qk = emit_qk(0)
                            for kj in range(nk):
                                sT, c0 = nxt_qk
                                if kj + 1 < nk:
                                    nxt_qk = emit_qk(kj + 1)
                                near_end = min((kj + 2) * 128, (qc + 1) * 512) - qc * 512
                                pt = nxt(b512, "b")
                                if near_end > c0:
                                    k.op("act", lambda e: e.activation(out=pt.t[:, c0:near_end], in_=sT.t[:, c0:near_end], func=AF.Exp),
                                         reads=[sT], writes=[pt])
                                if near_end < 512:
                                    lo = max(near_end, c0)
                                    k.op("act", lambda e: e.activation(out=pt.t[:, lo:512], in_=sT.t[:, lo:512], func=AF.Exp,
                                                                       bias=P("relc", h)),
                                         reads=[sT, pars], writes=[pt])
                                k.op("pe", lambda e: e.matmul(pO.t[:, c0:512], lhsT=vh.t[:, kj, :], rhs=pt.t[:, c0:512],
                                                              start=(kj == 0), stop=(kj == nk - 1)),
                                     reads=[vh, pt], writes=[pO])
                                k.op("pe", lambda e: e.matmul(pL.t[:, c0:512], lhsT=ones_bf.t[:], rhs=pt.t[:, c0:512],
                                                              start=(kj == 0), stop=(kj == nk - 1)),
                                     reads=[ones_bf, pt], writes=[pL])
                            r_ = attn_r[m]
                            k.op("dve", lambda e: e.reciprocal(out=r_.t[:], in_=pL.t[:]), reads=[pL], writes=[r_])
                            t_ = attn_t[m]
                            k.op("dve", lambda e: e.tensor_tensor(out=t_.t[:], in0=pO.t[:], in1=r_.t[:], op=ALU.mult),
                                 reads=[pO, r_], writes=[t_])
                            tms.append(t_)
                        a_ = nxt(f512, "f")
                        k.op("dve", lambda e: e.scalar_tensor_tensor(out=a_.t[:], in0=tms[1].t[:], scalar=neglam, in1=tms[0].t[:],
                                                                     op0=ALU.mult, op1=ALU.add),
                             reads=[tms[0], tms[1], small], writes=[a_])
                        ob = fm_norm(a_, gsubs, 1.0 / 128, EPS, 512, ones_f)
                        c_lo = s * S + qc * 512
                        k.dma("sp", lambda e: e.dma_start(out=mixT[h, :, c_lo:c_lo + 512], in_=ob.t[:]),
                              reads=[ob], writes=[R("mix", c_lo // 512)], accumulate_writes=True)
                        for _ in range(-(-(8 - POOL_CH) * CW // (NH * (S // 512)))):
                            next(gen, None)
                for _ in gen:
                    pass

            def ln_seq(s):
                for tb in range(S // 512):
                    cs = slice(tb * 512, (tb + 1) * 512)
                    pM = nxt(pA, "pa")
                    pQ = nxt(pA, "pa")
                    for j in range(8):
                        k.op("pe", lambda e, j=j: e.matmul(pM.t[:], lhsT=ones_f, rhs=Yc[j].t[:, cs], start=(j == 0), stop=(j == 7)),
                             reads=[cst, Yc[j]], writes=[pM])
                    for j in range(8):
                        sq = nxt(f512, "f")
                        k.op("act", lambda e, j=j: e.activation(out=sq.t[:], in_=Yc[j].t[:, cs], func=AF.Square), reads=[Yc[j]], writes=[sq])
                        k.op("pe", lambda e, j=j: e.matmul(pQ.t[:], lhsT=ones_f, rhs=sq.t[:], start=(j == 0), stop=(j == 7)),
                             reads=[cst, sq], writes=[pQ])
                    k.op("act", lambda e: e.activation(out=mean.t[:], in_=pM.t[:], func=AF.Copy, scale=1.0 / 1024), reads=[pM], writes=[mean])
                    k.op("act", lambda e: e.activation(out=msq.t[:], in_=mean.t[:], func=AF.Square), reads=[mean], writes=[msq])
                    k.op("dve", lambda e: e.scalar_tensor_tensor(out=var.t[:], in0=pQ.t[:], scalar=1.0 / 1024, in1=msq.t[:],
                                                                 op0=ALU.mult, op1=ALU.subtract), reads=[pQ, msq], writes=[var])
                    k.op("act", lambda e: e.activation(out=var.t[:], in_=var.t[:], func=AF.Sqrt, scale=1.0, bias=EPS), reads=[var], writes=[var])
                    k.op("dve", lambda e: e.reciprocal(out=var.t[:], in_=var.t[:]), reads=[var], writes=[var])
                    for j in range(8):
                        t_ = nxt(f512, "f")
                        k.op("dve", lambda e, j=j: e.tensor_tensor(out=t_.t[:], in0=Yc[j].t[:, cs], in1=mean.t[:], op=ALU.subtract),
                             reads=[Yc[j], mean], writes=[t_])
                        k.op("dve", lambda e: e.tensor_tensor(out=t_.t[:], in0=t_.t[:], in1=var.t[:], op=ALU.mult),
                             reads=[var], writes=[t_])
                        ob = nxt(b512, "b")
                        k.op("act", lambda e, j=j: e.activation(out=ob.t[:], in_=t_.t[:], func=AF.Silu, scale=P("lng", j), bias=P("lnb", j)),
                             reads=[t_, pars], writes=[ob])
                        c_lo = s * S + tb * 512
                        k.dma("sp", lambda e, j=j: e.dma_start(out=mixT[8 + j, :, c_lo:c_lo + 512], in_=ob.t[:]),
                              reads=[ob], writes=[R("mix", c_lo // 512)], accumulate_writes=True)

            for s in range(NSEQ):
                conv_seq(s)
                attn_seq(s)
                ln_seq(s)
            st_.close()

        if "mixT" in dbg and l == 0:
            k.dma("sp", lambda e: e.dma_start(out=dbg["mixT"][:, :, :], in_=mixT[:, :, :]), writes=[R("dbg", "mixT")])
            k.barrier()

        if "D" in STAGES:
            st_ = Stage()
            wo_v = w_out[l].rearrange("(kc p) n -> p kc n", p=128)
            wfull = st_.sb("wfull", [128, KC, D], BF16)
            mb2 = [st_.sb("mb", [128, KC, 512], BF16) for _ in range(2)]
            xt2 = [st_.sb("xt", [128, D], F32) for _ in range(2)]
            xo2 = [st_.sb("xo", [128, D], F32) for _ in range(2)]
            for nb in range(4):
                k.dma("pool", lambda e, nb=nb: e.dma_start(out=wfull.t[:, :, nb * 512:(nb + 1) * 512], in_=wo_v[:, :, nb * 512:(nb + 1) * 512]),
                      writes=[wfull], accumulate_writes=True)
            mix_v = mixT.rearrange("c p t -> p c t")
            for tb in range(T // 512):
                mb = nxt(mb2, "mb")
                k.dma("sp", lambda e: e.dma_start(out=mb.t[:], in_=mix_v[:, :, tb * 512:(tb + 1) * 512]), reads=[R("mix", tb)], writes=[mb])
                for tt in range(4):
                    r0 = tb * 512 + tt * 128
                    xt = nxt(xt2, "x")
                    k.dma("sp", lambda e: e.dma_start(out=xt.t[:], in_=x_src[r0:r0 + 128, :]), reads=[R(x_key, r0 // 128)], writes=[xt])
                    xo = nxt(xo2, "xo")
                    for nb in range(4):
                        pp = nxt(pA, "pa")
                        for c in range(KC):
                            k.op("pe", lambda e, c=c: e.matmul(pp.t[:], lhsT=mb.t[:, c, tt * 128:(tt + 1) * 128],
                                                               rhs=wfull.t[:, c, nb * 512:(nb + 1) * 512],
                                                               start=(c == 0), stop=(c == KC - 1)),
                                 reads=[mb, wfull], writes=[pp])
                        k.op("dve", lambda e, nb=nb: e.tensor_tensor(out=xo.t[:, nb * 512:(nb + 1) * 512], in0=pp.t[:],
                                                                     in1=xt.t[:, nb * 512:(nb + 1) * 512], op=ALU.add),
                             reads=[pp, xt], writes=[xo])
                    k.dma("sp", lambda e: e.dma_start(out=xres[r0:r0 + 128, :], in_=xo.t[:]), reads=[xo], writes=[R("xres", r0 // 128)])
            st_.close()
            x_src = xres
            x_key = "xres"

        if "x1" in dbg and l == 0:
            k.dma("sp", lambda e: e.dma_start(out=dbg["x1"][:, :], in_=xres[:, :]), writes=[R("dbg", "x1")])
            k.barrier()

        if "E" in STAGES:
            st_ = Stage()
            W = rmsW(st_)
            W["wb"] = [st_.sb("wb", [128, KC, 512], BF16) for _ in range(2)]
            xo2 = [st_.sb("xo", [128, D], F32) for _ in range(2)]
            memT = st_.sb("memT", [128, KC, MEM], BF16)
            kcT = st_.sb("kcT", [128, NHC, MEM], BF16)
            vcs = st_.sb("vcs", [128, 2, 512], BF16)
            ocT = st_.sb("ocT", [128, NHC, 512], BF16)
            hTb = st_.sb("hTb", [128, KC, 512], BF16)
            wq_sb = st_.sb("wq_sb", [128, KC, 512], BF16)
            qn4 = [st_.sb("qn4", [128, 512], BF16) for _ in range(NHC)]
            wo_sb = st_.sb("wo_sb", [128, NHC, D], BF16)
            wq_v = wq_c[l].rearrange("(kc p) n -> p kc n", p=128)
            wkv_v = wkv_c[l].rearrange("(kc p) n -> p kc n", p=128)
            woc_v = wo_c[l].rearrange("(kc p) n -> p kc n", p=128)
            k.dma("pool", lambda e: e.dma_start(out=wq_sb.t[:], in_=wq_v[:, :, :]), writes=[wq_sb])
            k.dma("pool", lambda e: e.dma_start(out=wo_sb.t[:], in_=woc_v[:, :, :]), writes=[wo_sb])
            for s in range(NSEQ):
                for mt in range(2):
                    r0 = s * MEM + mt * 128
                    rms_tile(W, mem_in[r0:r0 + 128, :], R("memin"), P("gmemT", 0, 16), memT, mt * 128)
                wk_ = load_w(W, wkv_v, 0)
                for h in range(NHC):
                    pp = nxt(pA, "pa")
                    for kc in range(KC):
                        k.op("pe", lambda e, kc=kc: e.matmul(pp.t[:, 0:MEM], lhsT=wk_.t[:, kc, h * 128:(h + 1) * 128], rhs=memT.t[:, kc, :],
                                                             start=(kc == 0), stop=(kc == KC - 1)), reads=[wk_, memT], writes=[pp])
                    ob = fm_norm(pp, P("gkc"), 1.0 / 128, EPS, MEM, ones_f)
                    k.op("dve", lambda e: e.tensor_copy(out=kcT.t[:, h, :], in_=ob.t[:, 0:MEM]), reads=[ob], writes=[kcT])
                wv_ = load_w(W, wkv_v, 512)
                for mt in range(2):
                    pp = nxt(pA, "pa")
                    for kc in range(KC):
                        k.op("pe", lambda e, kc=kc: e.matmul(pp.t[:], lhsT=memT.t[:, kc, mt * 128:(mt + 1) * 128], rhs=wv_.t[:, kc, :],
                                                             start=(kc == 0), stop=(kc == KC - 1)), reads=[wv_, memT], writes=[pp])
                    k.op("act", lambda e: e.activation(out=vcs.t[:, mt, :], in_=pp.t[:], func=AF.Copy), reads=[pp], writes=[vcs])
                for tb in range(S // 512):
                    for tt in range(4):
                        r0 = s * S + tb * 512 + tt * 128
                        rms_tile(W, xres[r0:r0 + 128, :], R("xres", r0 // 128), P("gcrossT", 0, 16), hTb, tt * 128)
                    for h in range(NHC):
                        pp = pA[h]
                        for kc in range(KC):
                            k.op("pe", lambda e, kc=kc, pp=pp: e.matmul(pp.t[:], lhsT=wq_sb.t[:, kc, h * 128:(h + 1) * 128], rhs=hTb.t[:, kc, :],
                                                                        start=(kc == 0), stop=(kc == KC - 1)), reads=[wq_sb, hTb], writes=[pp])
                    for h0 in (0, 2):
                        hs = (h0, h0 + 1)
                        for h in hs:
                            k.op("act", lambda e, h=h: e.activation(out=f512[h].t[:], in_=pA[h].t[:], func=AF.Square), reads=[pA[h]], writes=[f512[h]])
                        for h in hs:
                            k.op("pe", lambda e, h=h: e.matmul(pA[4 + h % 2].t[:], lhsT=ones_f, rhs=f512[h].t[:], start=True, stop=True),
                                 reads=[f512[h], cst], writes=[pA[4 + h % 2]])
                        for h in hs:
                            k.op("act", lambda e, h=h: e.activation(out=f512[h].t[:], in_=pA[4 + h % 2].t[:], func=AF.Sqrt, scale=1.0, bias=128 * EPS),
                                 reads=[pA[4 + h % 2]], writes=[f512[h]])
                        for h in hs:
                            k.op("dve", lambda e, h=h: e.reciprocal(out=f512[h].t[:], in_=f512[h].t[:]), reads=[f512[h]], writes=[f512[h]])
                        for h in hs:
                            k.op("dve", lambda e, h=h: e.scalar_tensor_tensor(out=qn4[h].t[:], in0=pA[h].t[:], scalar=P("gqc"), in1=f512[h].t[:],
                                                                              op0=ALU.mult, op1=ALU.mult),
                                 reads=[pA[h], f512[h], pars], writes=[qn4[h]])
                    for h in range(NHC):
                        pO = pA[(h % 2) * 2]
                        pL = pA[(h % 2) * 2 + 1]
                        sTs = [pA[4], pA[5]]
                        pts = [nxt(b512, "b"), nxt(b512, "b")]
                        for mt in range(2):
                            k.op("pe", lambda e, mt=mt: e.matmul(sTs[mt].t[:], lhsT=kcT.t[:, h, mt * 128:(mt + 1) * 128], rhs=qn4[h].t[:], start=True, stop=True),
                                 reads=[kcT, qn4[h]], writes=[sTs[mt]])
                        for mt in range(2):
                            k.op("act", lambda e, mt=mt: e.activation(out=pts[mt].t[:], in_=sTs[mt].t[:], func=AF.Exp), reads=[sTs[mt]], writes=[pts[mt]])
                        for mt in range(2):
                            k.op("pe", lambda e, mt=mt: e.matmul(pO.t[:], lhsT=vcs.t[:, mt, h * 128:(h + 1) * 128], rhs=pts[mt].t[:], start=(mt == 0), stop=(mt == 1)),
                                 reads=[vcs, pts[mt]], writes=[pO])
                            k.op("pe", lambda e, mt=mt: e.matmul(pL.t[:], lhsT=ones_bf.t[:], rhs=pts[mt].t[:], start=(mt == 0), stop=(mt == 1)),
                                 reads=[ones_bf, pts[mt]], writes=[pL])
                        r_ = f512[4 + h % 2]
                        k.op("dve", lambda e: e.reciprocal(out=r_.t[:], in_=pL.t[:]), reads=[pL], writes=[r_])
                        k.op("dve", lambda e: e.tensor_tensor(out=ocT.t[:, h, :], in0=pO.t[:], in1=r_.t[:], op=ALU.mult),
                             reads=[pO, r_], writes=[ocT])
                    for tt in range(4):
                        r0 = s * S + tb * 512 + tt * 128
                        xt = nxt(W["xt"], "x")
                        k.dma("sp", lambda e: e.dma_start(out=xt.t[:], in_=xres[r0:r0 + 128, :]), reads=[R("xres", r0 // 128)], writes=[xt])
                        xo = nxt(xo2, "xo")
                        for nb in range(4):
                            pp = nxt(pA, "pa")
                            for h in range(NHC):
                                k.op("pe", lambda e, h=h: e.matmul(pp.t[:], lhsT=ocT.t[:, h, tt * 128:(tt + 1) * 128],
                                                                   rhs=wo_sb.t[:, h, nb * 512:(nb + 1) * 512],
                                                                   start=(h == 0), stop=(h == NHC - 1)), reads=[ocT, wo_sb], writes=[pp])
                            k.op("dve", lambda e, nb=nb: e.tensor_tensor(out=xo.t[:, nb * 512:(nb + 1) * 512], in0=pp.t[:],
                                                                         in1=xt.t[:, nb * 512:(nb + 1) * 512], op=ALU.add),
                                 reads=[pp, xt], writes=[xo])
                        k.dma("sp", lambda e: e.dma_start(out=xres[r0:r0 + 128, :], in_=xo.t[:]), reads=[xo], writes=[R("xres", r0 // 128)])
            st_.close()

        if "x2" in dbg and l == 0:
            k.dma("sp", lambda e: e.dma_start(out=dbg["x2"][:, :], in_=xres[:, :]), writes=[R("dbg", "x2")])
            k.barrier()

        if "F" in STAGES:
            gates = LS.sb("gates", [128, NT, 2], F32)
            desti = LS.sb("desti", [128, NT * 2], I32)
            widx = LS.sb("widx", [128, NB], I32)
            st_ = Stage()
            xt2 = [st_.sb("xt", [128, D], F32) for _ in range(2)]
            hb2 = [st_.sb("hb", [128, D], BF16) for _ in range(2)]
            junk = st_.sb("junk", [128, D], BF16)
            gffn = st_.sb("gffn", [128, D], F32)
            wr = st_.sb("wr", [128, KC, 72], BF16)
            hTt = st_.sb("hTt", [128, KC, 128], BF16)
            OH = st_.sb("OH", [128, NT, 2, NE], F32)
            RANK = st_.sb("RANK", [128, NT, NE], F32)
            cum = st_.sb("cum", [128, NE], F32)
            rt = [st_.sb("rt", [128, 128], F32) for i in range(6)]
            Mb = st_.sb("Mb", [128, NE], BF16)
            destf = st_.sb("destf", [128, NT * 2], F32)
            pcs = [st_.sb("pcs", [128, NE], F32) for i in range(3)]
            pci = st_.sb("pci", [128, NE], I32)
            thr = st_.sb("thr_sb", [128, NB], F32)
            cmpb = st_.sb("cmpb", [128, NB, NE], F32)
            bef = st_.sb("bef", [128, NB], F32)
            k.dma("sp", lambda e: e.dma_start(out=thr.t[:], in_=thr_in[:, :]), writes=[thr])
            k.dma("sp", lambda e: e.dma_start(out=gffn.t[:], in_=gffn_in[l, :, :]), writes=[gffn])
            k.dma("pool", lambda e: e.dma_start(out=wr.t[:], in_=w_rt[l].rearrange("(kc p) n -> p kc n", p=128)), writes=[wr])
            k.op("dve", lambda e: e.memset(cum.t[:], 0.0), writes=[cum])
            o_brt = _off["brt"][0]
            for tt in range(NT):
                r0 = tt * 128
                xt = nxt(xt2, "x")
                k.dma("sp", lambda e: e.dma_start(out=xt.t[:], in_=xres[r0:r0 + 128, :]), reads=[R("xres", tt)], writes=[xt])
                c0 = nxt(col, "c")
                k.op("act", lambda e: e.activation(out=junk.t[:], in_=xt.t[:], func=AF.Square, accum_out=c0.t[:, 0:1]),
                     reads=[xt], writes=[junk, c0])
                k.op("act", lambda e: e.activation(out=c0.t[:, 1:2], in_=c0.t[:, 0:1], func=AF.Sqrt, scale=1.0 / D, bias=EPS), reads=[c0], writes=[c0])
                k.op("dve", lambda e: e.reciprocal(out=c0.t[:, 2:3], in_=c0.t[:, 1:2]), reads=[c0], writes=[c0])
                hb = nxt(hb2, "hb")
                k.op("dve", lambda e: e.scalar_tensor_tensor(out=hb.t[:], in0=xt.t[:], scalar=c0.t[:, 2:3], in1=gffn.t[:],
                                                             op0=ALU.mult, op1=ALU.mult), reads=[xt, c0, gffn], writes=[hb])
                k.dma("sp", lambda e: e.dma_start(out=Hrows[r0:r0 + 128, :], in_=hb.t[:]), reads=[hb], writes=[R("hrows", tt)])
                for kc in range(KC):
                    k.op("pe", lambda e, kc=kc: e.transpose(out=pT.t[:, kc, :], in_=hb.t[:, kc * 128:(kc + 1) * 128], identity=ident_bf.t[:]),
                         reads=[hb, ident_bf], writes=[pT])
                k.op("act", lambda e: e.activation(out=hTt.t[:], in_=pT.t[:], func=AF.Copy), reads=[pT], writes=[hTt])
                pp = nxt(pA, "pa")
                for kc in range(KC):
                    k.op("pe", lambda e, kc=kc: e.matmul(pp.t[:, 0:72], lhsT=hTt.t[:, kc, :], rhs=wr.t[:, kc, :], start=(kc == 0), stop=(kc == KC - 1)),
                         reads=[hTt, wr], writes=[pp])
                lg, w1_, w2_, w3_, w4_, w5_ = rt
                k.op("dve", lambda e: e.tensor_tensor(out=lg.t[:, 0:72], in0=pp.t[:, 0:72], in1=pars.t[:, o_brt:o_brt + 72], op=ALU.add),
                     reads=[pp, pars], writes=[lg])
                k.op("dve", lambda e: e.tensor_reduce(out=w1_.t[:, 0:1], in_=lg.t[:, 0:8], axis=AX.X, op=ALU.max), reads=[lg], writes=[w1_])
                k.op("dve", lambda e: e.tensor_scalar(out=w2_.t[:, 0:8], in0=lg.t[:, 0:8], scalar1=w1_.t[:, 0:1], scalar2=None, op0=ALU.is_equal),
                     reads=[lg, w1_], writes=[w2_])
                k.op("dve", lambda e: e.tensor_scalar(out=w1_.t[:, 1:2], in0=w1_.t[:, 0:1], scalar1=-1.0, scalar2=None, op0=ALU.mult),
                     reads=[w1_], writes=[w1_])
                k.op("act", lambda e: e.activation(out=w2_.t[:, 8:16], in_=lg.t[:, 0:8], func=AF.Exp, bias=w1_.t[:, 1:2], accum_out=w1_.t[:, 2:3]),
                     reads=[lg, w1_], writes=[w2_, w1_])
                k.op("dve", lambda e: e.reciprocal(out=w1_.t[:, 3:4], in_=w1_.t[:, 2:3]), reads=[w1_], writes=[w1_])
                k.op("dve", lambda e: e.tensor_tensor(out=w3_.t[:, 0:64].rearrange("p (g j) -> p g j", j=8),
                                                      in0=lg.t[:, 8:72].rearrange("p (g j) -> p g j", j=8),
                                                      in1=w2_.t[:, 0:8].unsqueeze(2).to_broadcast([128, 8, 8]), op=ALU.mult),
                     reads=[lg, w2_], writes=[w3_])
                k.op("dve", lambda e: e.tensor_reduce(out=w4_.t[:, 0:8], in_=w3_.t[:, 0:64].rearrange("p (g j) -> p j g", j=8), axis=AX.X, op=ALU.add),
                     reads=[w3_], writes=[w4_])
                k.op("dve", lambda e: e.tensor_reduce(out=w1_.t[:, 4:5], in_=w4_.t[:, 0:8], axis=AX.X, op=ALU.max), reads=[w4_], writes=[w1_])
                k.op("dve", lambda e: e.tensor_scalar(out=w4_.t[:, 8:16], in0=w4_.t[:, 0:8], scalar1=w1_.t[:, 4:5], scalar2=None, op0=ALU.is_equal),
                     reads=[w4_, w1_], writes=[w4_])
                k.op("dve", lambda e: e.scalar_tensor_tensor(out=w4_.t[:, 16:24], in0=w4_.t[:, 8:16], scalar=-1e30, in1=w4_.t[:, 0:8],
                                                             op0=ALU.mult, op1=ALU.add), reads=[w4_], writes=[w4_])
                k.op("dve", lambda e: e.tensor_reduce(out=w1_.t[:, 5:6], in_=w4_.t[:, 16:24], axis=AX.X, op=ALU.max), reads=[w4_], writes=[w1_])
                k.op("dve", lambda e: e.tensor_scalar(out=w4_.t[:, 24:32], in0=w4_.t[:, 16:24], scalar1=w1_.t[:, 5:6], scalar2=None, op0=ALU.is_equal),
                     reads=[w4_, w1_], writes=[w4_])
                k.op("dve", lambda e: e.tensor_tensor(out=w1_.t[:, 6:7], in0=w1_.t[:, 5:6], in1=w1_.t[:, 4:5], op=ALU.subtract), reads=[w1_], writes=[w1_])
                k.op("act", lambda e: e.activation(out=w1_.t[:, 7:8], in_=w1_.t[:, 6:7], func=AF.Exp), reads=[w1_], writes=[w1_])
                k.op("dve", lambda e: e.tensor_scalar(out=w1_.t[:, 8:9], in0=w1_.t[:, 7:8], scalar1=1.0, scalar2=None, op0=ALU.add), reads=[w1_], writes=[w1_])
                k.op("dve", lambda e: e.reciprocal(out=w1_.t[:, 9:10], in_=w1_.t[:, 8:9]), reads=[w1_], writes=[w1_])
                k.op("dve", lambda e: e.tensor_tensor(out=gates.t[:, tt, 0:1], in0=w1_.t[:, 3:4], in1=w1_.t[:, 9:10], op=ALU.mult), reads=[w1_], writes=[gates])
                k.op("dve", lambda e: e.tensor_tensor(out=gates.t[:, tt, 1:2], in0=gates.t[:, tt, 0:1], in1=w1_.t[:, 7:8], op=ALU.mult), reads=[w1_], writes=[gates])
                for kk in range(2):
                    k.op("dve", lambda e, kk=kk: e.tensor_tensor(
                        out=OH.t[:, tt, kk, :].rearrange("p (g j) -> p g j", j=8),
                        in0=w2_.t[:, 0:8].unsqueeze(2).to_broadcast([128, 8, 8]),
                        in1=w4_.t[:, 8 + 16 * kk:16 + 16 * kk].unsqueeze(1).to_broadcast([128, 8, 8]), op=ALU.mult),
                         reads=[w2_, w4_], writes=[OH])
                k.op("dve", lambda e: e.tensor_tensor(out=Mb.t[:], in0=OH.t[:, tt, 0, :], in1=OH.t[:, tt, 1, :], op=ALU.add), reads=[OH], writes=[Mb])
                pr = nxt(pA, "pa")
                k.op("pe", lambda e: e.matmul(pr.t[:, 0:NE], lhsT=ltri_bf.t[:], rhs=Mb.t[:], start=True, stop=True), reads=[ltri_bf, Mb], writes=[pr])
                k.op("dve", lambda e: e.tensor_tensor(out=RANK.t[:, tt, :], in0=pr.t[:, 0:NE], in1=cum.t[:], op=ALU.add), reads=[pr, cum], writes=[RANK])
                pc_ = nxt(pA, "pa")
                k.op("pe", lambda e: e.matmul(pc_.t[:, 0:NE], lhsT=ones_bf.t[:], rhs=Mb.t[:], start=True, stop=True), reads=[ones_bf, Mb], writes=[pc_])
                k.op("dve", lambda e: e.tensor_tensor(out=cum.t[:], in0=cum.t[:], in1=pc_.t[:, 0:NE], op=ALU.add), reads=[pc_], writes=[cum])
            k.op("dve", lambda e: e.tensor_scalar(out=pcs[0].t[:], in0=cum.t[:], scalar1=float(BLK - 1), scalar2=None, op0=ALU.add), reads=[cum], writes=[pcs[0]])
            k.op("dve", lambda e: e.tensor_copy(out=pci.t[:], in_=pcs[0].t[:]), reads=[pcs[0]], writes=[pci])
            sh = int(math.log2(BLK))
            k.op("dve", lambda e: e.tensor_single_scalar(out=pci.t[:], in_=pci.t[:], scalar=sh, op=ALU.arith_shift_right), writes=[pci])
            k.op("dve", lambda e: e.tensor_single_scalar(out=pci.t[:], in_=pci.t[:], scalar=sh, op=ALU.logical_shift_left), writes=[pci])
            k.op("dve", lambda e: e.tensor_copy(out=pcs[0].t[:], in_=pci.t[:]), reads=[pci], writes=[pcs[0]])
            k.op("dve", lambda e: e.tensor_copy(out=pcs[1].t[:], in_=pcs[0].t[:]), reads=[pcs[0]], writes=[pcs[1]])
            cur, oth = pcs[1], pcs[2]
            stp = 1
            while stp < NE:
                k.op("dve", lambda e, stp=stp, cur=cur, oth=oth: e.tensor_copy(out=oth.t[:, 0:stp], in_=cur.t[:, 0:stp]), reads=[cur], writes=[oth])
                k.op("dve", lambda e, stp=stp, cur=cur, oth=oth: e.tensor_tensor(out=oth.t[:, stp:NE], in0=cur.t[:, stp:NE], in1=cur.t[:, 0:NE - stp], op=ALU.add),
                     reads=[cur], writes=[oth])
                cur, oth = oth, cur
                stp *= 2
            pend = cur
            pstart = oth
            k.op("dve", lambda e: e.tensor_tensor(out=pstart.t[:], in0=pend.t[:], in1=pcs[0].t[:], op=ALU.subtract), reads=[pend, pcs[0]], writes=[pstart])
            for tt in range(NT):
                k.op("dve", lambda e, tt=tt: e.tensor_tensor(out=RANK.t[:, tt, :], in0=RANK.t[:, tt, :], in1=pstart.t[:], op=ALU.add), reads=[pstart], writes=[RANK])
                for kk in range(2):
                    k.op("dve", lambda e, tt=tt, kk=kk: e.tensor_tensor(out=rt[2].t[:, 0:NE], in0=RANK.t[:, tt, :], in1=OH.t[:, tt, kk, :], op=ALU.mult),
                         reads=[RANK, OH], writes=[rt[2]])
                    k.op("dve", lambda e, tt=tt, kk=kk: e.tensor_reduce(out=destf.t[:, tt * 2 + kk:tt * 2 + kk + 1], in_=rt[2].t[:, 0:NE], axis=AX.X, op=ALU.add),
                         reads=[rt[2]], writes=[destf])
            k.op("dve", lambda e: e.tensor_copy(out=desti.t[:], in_=destf.t[:]), reads=[destf], writes=[desti])
            k.op("dve", lambda e: e.tensor_tensor(out=cmpb.t[:], in0=pend.t[:].unsqueeze(1).to_broadcast([128, NB, NE]),
                                                  in1=thr.t[:].unsqueeze(2).to_broadcast([128, NB, NE]), op=ALU.is_le), reads=[pend, thr], writes=[cmpb])
            k.op("dve", lambda e: e.tensor_reduce(out=bef.t[:], in_=cmpb.t[:], axis=AX.X, op=ALU.add), reads=[cmpb], writes=[bef])
            k.op("dve", lambda e: e.tensor_scalar(out=bef.t[:], in0=bef.t[:], scalar1=128.0, scalar2=None, op0=ALU.mult), writes=[bef])
            k.op("dve", lambda e: e.tensor_scalar(out=bef.t[:], in0=bef.t[:], scalar1=iota_p, scalar2=None, op0=ALU.add), reads=[cst], writes=[bef])
            k.op("dve", lambda e: e.tensor_copy(out=widx.t[:], in_=bef.t[:]), reads=[bef], writes=[widx])

            k.op("dve", lambda e: e.memset(junk.t[:], 0.0), writes=[junk])
            RPP = PR // 128
            zc = 24
            xs_v = Xs.rearrange("(p r) d -> p r d", p=128)
            for z0 in range(0, RPP, zc):
                zn = min(zc, RPP - z0)
                k.dma("sp", lambda e, z0=z0, zn=zn: e.dma_start(out=xs_v[:, z0:z0 + zn, :],
                                                                 in_=junk.t[:].unsqueeze(1).to_broadcast([128, zn, D])),
                      reads=[junk], writes=[R("xs")], accumulate_writes=True)
            k.barrier()
            for tt in range(NT):
                hb = nxt(hb2, "hb")
                k.dma("sp", lambda e: e.dma_start(out=hb.t[:], in_=Hrows[tt * 128:(tt + 1) * 128, :]), reads=[R("hrows", tt)], writes=[hb])
                for kk in range(2):
                    k.dma("pool", lambda e, kk=kk: e.indirect_dma_start(
                        out=Xs[:, :], out_offset=bass.IndirectOffsetOnAxis(ap=desti.t[:, tt * 2 + kk:tt * 2 + kk + 1], axis=0),
                        in_=hb.t[:], in_offset=None), reads=[hb, desti], writes=[R("xs")], accumulate_writes=True)

            if "moe" in dbg and l == 0:
                k.dma("sp", lambda e: e.dma_start(out=dbg["moe"][:, 0:NT * 2], in_=destf.t[:]), reads=[destf], writes=[R("dbg", "moe")])
                k.dma("sp", lambda e: e.dma_start(out=dbg["moe"][:, NT * 2:NT * 2 + NB], in_=bef.t[:]), reads=[bef], writes=[R("dbg", "moe2")])
                k.dma("sp", lambda e: e.dma_start(out=dbg["moe"][:, NT * 2 + NB:NT * 4 + NB], in_=gates.t[:].rearrange("p t k -> p (t k)")),
                      reads=[gates], writes=[R("dbg", "moe3")])
            st_.close()

            st_ = Stage()
            yo2 = [st_.sb("yo", [128, D], F32) for _ in range(2)]
            xsb = [st_.sb("xsb", [128, 2, D], BF16) for i in range(2)]
            xTb = st_.sb("xTb", [128, KC, BLK], BF16)
            hid = st_.sb("hid", [128, FC, BLK], BF16)
            w1b = [st_.sb("w1b", [128, KC, DE], BF16) for i in range(2)]
            w3b = [st_.sb("w3b", [128, KC, DE], BF16) for i in range(2)]
            w2b = [st_.sb("w2b", [128, FC, D], BF16) for i in range(2)]

            bc_reg = nc.gpsimd.to_reg(NE * 128 - 1)

            def load_block(b):
                i = b % 2
                k.dma("sp", lambda e: e.dma_start(out=xsb[i].t[:], in_=Xs[b * BLK:(b + 1) * BLK, :].rearrange("(r p) d -> p r d", p=128)),
                      reads=[R("xs")], writes=[xsb[i]])
                for wsb, wdr, nch, g in ((w1b[i], w1r, NCH, GK), (w3b[i], w3r, NCH, GK), (w2b[i], w2r, FC, 1)):
                    for c in range(nch):
                        k.dma("pool", lambda e, wsb=wsb, wdr=wdr, c=c, g=g: e.indirect_dma_start(
                            out=wsb.t[:, c * g:(c + 1) * g, :].rearrange("p a b -> p (a b)"), out_offset=None, in_=wdr[l][c][:, :],
                            in_offset=bass.IndirectOffsetOnAxis(ap=widx.t[:, b:b + 1], axis=0),
                            bounds_check=bc_reg, oob_is_err=False), reads=[widx], writes=[wsb],
                              accumulate_writes=(c > 0))

            load_block(0)
            for b in range(NB):
                i = b % 2
                if b + 1 < NB:
                    load_block(b + 1)
                for r in range(2):
                    for kc in range(KC):
                        k.op("pe", lambda e, kc=kc, r=r: e.transpose(out=pT.t[:, kc, :], in_=xsb[i].t[:, r, kc * 128:(kc + 1) * 128], identity=ident_bf.t[:]),
                             reads=[xsb[i], ident_bf], writes=[pT])
                    k.op("act", lambda e, r=r: e.activation(out=xTb.t[:, :, r * 128:(r + 1) * 128], in_=pT.t[:], func=AF.Copy), reads=[pT], writes=[xTb])
                for fc in range(FC):
                    p1 = nxt(pA, "pa")
                    p3 = nxt(pA, "pa")
                    for kc in range(KC):
                        k.op("pe", lambda e, kc=kc: e.matmul(p1.t[:, 0:BLK], lhsT=w1b[i].t[:, kc, fc * 128:(fc + 1) * 128], rhs=xTb.t[:, kc, :],
                                                             start=(kc == 0), stop=(kc == KC - 1)), reads=[w1b[i], xTb], writes=[p1])
                    for kc in range(KC):
                        k.op("pe", lambda e, kc=kc: e.matmul(p3.t[:, 0:BLK], lhsT=w3b[i].t[:, kc, fc * 128:(fc + 1) * 128], rhs=xTb.t[:, kc, :],
                                                             start=(kc == 0), stop=(kc == KC - 1)), reads=[w3b[i], xTb], writes=[p3])
                    sg = nxt(f512, "f")
                    k.op("act", lambda e: e.activation(out=sg.t[:, 0:BLK], in_=p1.t[:, 0:BLK], func=AF.Silu), reads=[p1], writes=[sg])
                    k.op("dve", lambda e: e.tensor_tensor(out=hid.t[:, fc, :], in0=p3.t[:, 0:BLK], in1=sg.t[:, 0:BLK], op=ALU.mult),
                         reads=[p3, sg], writes=[hid])
                for r in range(2):
                    yo = nxt(yo2, "yo")
                    for nb in range(4):
                        pp = nxt(pA, "pa")
                        for fc in range(FC):
                            k.op("pe", lambda e, fc=fc: e.matmul(pp.t[:], lhsT=hid.t[:, fc, r * 128:(r + 1) * 128], rhs=w2b[i].t[:, fc, nb * 512:(nb + 1) * 512],
                                                                 start=(fc == 0), stop=(fc == FC - 1)), reads=[hid, w2b[i]], writes=[pp])
                        if nb % 2 == 0:
                            k.op("act", lambda e, nb=nb: e.activation(out=yo.t[:, nb * 512:(nb + 1) * 512], in_=pp.t[:], func=AF.Copy), reads=[pp], writes=[yo])
                        else:
                            k.op("dve", lambda e, nb=nb: e.tensor_copy(out=yo.t[:, nb * 512:(nb + 1) * 512], in_=pp.t[:]), reads=[pp], writes=[yo])
                    rr0 = b * BLK + r * 128
                    k.dma("sp", lambda e: e.dma_start(out=Ys[rr0:rr0 + 128, :], in_=yo.t[:]), reads=[yo], writes=[R("ys")], accumulate_writes=True)
            st_.close()

            st_ = Stage()
            xt2 = [st_.sb("xt", [128, D], F32) for _ in range(2)]
            y02 = [st_.sb("y0", [128, D], F32) for _ in range(2)]
            y12 = [st_.sb("y1", [128, D], F32) for _ in range(2)]
            dst = out if last else xres
            for tt in range(NT):
                r0 = tt * 128
                xt = nxt(xt2, "x")
                k.dma("sp", lambda e: e.dma_start(out=xt.t[:], in_=xres[r0:r0 + 128, :]), reads=[R("xres", tt)], writes=[xt])
                ys = [nxt(y02, "y0"), nxt(y12, "y1")]
                for kk in range(2):
                    k.dma("pool", lambda e, kk=kk: e.indirect_dma_start(
                        out=ys[kk].t[:], out_offset=None, in_=Ys[:, :],
                        in_offset=bass.IndirectOffsetOnAxis(ap=desti.t[:, tt * 2 + kk:tt * 2 + kk + 1], axis=0)),
                          reads=[R("ys"), desti], writes=[ys[kk]])
                k.op("dve", lambda e: e.scalar_tensor_tensor(out=xt.t[:], in0=ys[0].t[:], scalar=gates.t[:, tt, 0:1], in1=xt.t[:],
                                                             op0=ALU.mult, op1=ALU.add), reads=[ys[0], gates], writes=[xt])
                k.op("dve", lambda e: e.scalar_tensor_tensor(out=xt.t[:], in0=ys[1].t[:], scalar=gates.t[:, tt, 1:2], in1=xt.t[:],
                                                             op0=ALU.mult, op1=ALU.add), reads=[ys[1], gates], writes=[xt])
                k.dma("sp", lambda e: e.dma_start(out=dst[r0:r0 + 128, :], in_=xt.t[:]), reads=[xt],
                      writes=[R("out" if last else "xres", tt)])
            st_.close()
        LS.close()

    k.barrier()
    return nc, k


def t5_bucket_np(dist):
    max_exact = 16
    d_f = np.maximum(dist, 1).astype(np.float32)
    large = max_exact + (np.log(d_f / np.float32(max_exact)) / np.float32(math.log(128 / max_exact)) * np.float32(16)).astype(np.int32)
    large = np.minimum(large, 31)
    return np.where(dist < max_exact, dist, large)


def host_prep(inp, L, S, DE):
    f = lambda a: np.ascontiguousarray(np.asarray(a, dtype=np.float32))
    rep = lambda v: np.broadcast_to(np.asarray(v, np.float32).reshape(1, -1), (128, np.asarray(v).size))
    pars = np.zeros((L, 128, NPAR), np.float32)

    def put(l, name, arr):
        o, w = _off[name]
        pars[l, :, o:o + w] = arr.reshape(128, w)

    tbl = np.asarray(inp["rel_bias_table"], np.float32)
    for l in range(L):
        put(l, "gmixT", np.asarray(inp["g_mix"][l]).reshape(KC, 128).T)
        put(l, "gcrossT", np.asarray(inp["g_cross"][l]).reshape(KC, 128).T)
        put(l, "gmemT", np.asarray(inp["g_mem"][l]).reshape(KC, 128).T)
        put(l, "gq2", np.tile(np.asarray(inp["g_q"][l]), 2))
        put(l, "gk2", np.tile(np.asarray(inp["g_k"][l]), 2))
        put(l, "gsub", np.asarray(inp["g_subln"][l]))
        put(l, "gqc", np.asarray(inp["g_qc"][l]))
        put(l, "gkc", np.asarray(inp["g_kc"][l]))
        cw = np.asarray(inp["conv_w"][l])
        put(l, "convw", cw.reshape(CW, 8, 128).transpose(2, 1, 0))
        put(l, "convb", np.asarray(inp["conv_b"][l]).reshape(8, 128).T)
        put(l, "lng", np.asarray(inp["conv_ln_g"][l]).reshape(8, 128).T)
        put(l, "lnb", np.asarray(inp["conv_ln_b"][l]).reshape(8, 128).T)
        put(l, "dl", rep(np.asarray(inp["diff_lambda"][l]).reshape(-1)))
        put(l, "brt", rep(np.concatenate([np.asarray(inp["b_group"][l]), np.asarray(inp["b_router"][l])])))
        put(l, "relc", rep(tbl[31, :]))
    gffnb = np.stack([rep(inp["g_ffn"][l]) for l in range(L)]).astype(np.float32)
    kk_, qq_ = np.meshgrid(np.arange(128), np.arange(128), indexing="ij")
    biasT = np.zeros((128, NH * 2, 128), np.float32)
    for dlt in range(2):
        dist = dlt * 128 + qq_ - kk_
        bk = t5_bucket_np(np.maximum(dist, 0))
        for h in range(NH):
            biasT[:, h * 2 + dlt, :] = np.where(dist >= 0, tbl[bk, h], np.float32(MASKV))
    consts = np.zeros((128, 5, 128), np.float32)
    consts[:, 0, :] = np.eye(128)
    consts[:, 1, :] = 1.0
    consts[0:64, 2, 0:64] = 1.0
    consts[64:128, 2, 64:128] = 1.0
    consts[:, 3, :] = (kk_ < qq_)
    consts[:, 4, :] = np.arange(128).reshape(128, 1)
    w_rt = np.concatenate([np.asarray(inp["w_group"], np.float32)[:L], np.asarray(inp["w_router"], np.float32)[:L]], axis=-1)
    FC = DE // 128
    shared = {
        "pars": pars, "gffnb": gffnb, "biasT": biasT, "consts": consts,
        "w_in": f(inp["w_in"][:L]), "w_out": f(inp["w_out"][:L]), "wq_c": f(inp["wq_c"][:L]), "wkv_c": f(inp["wkv_c"][:L]),
        "wo_c": f(inp["wo_c"][:L]), "w_rt": f(w_rt),
    }
    if "F" in STAGES:
        NCH = KC * DE // 2048
        GK = 2048 // DE
        for nm, src_ in (("w1r", "w1"), ("w3r", "w3")):
            for l in range(L):
                a6 = np.asarray(inp[src_][l], np.float32).reshape(NE, NCH, GK, 128, DE)
                for c in range(NCH):
                    shared[f"{nm}_{l}_{c}"] = f(a6[:, c].transpose(0, 2, 1, 3).reshape(NE * 128, GK * DE))
        for l in range(L):
            a5 = np.asarray(inp["w2"][l], np.float32).reshape(NE, FC, 128, D)
            for c in range(FC):
                shared[f"w2r_{l}_{c}"] = f(a5[:, c].reshape(NE * 128, D))
    return shared


_CACHE = {}


def run(inp, L, NSEQ, S, DE, debug=()):
    key = (L, NSEQ, S, DE, tuple(debug))
    if key not in _CACHE:
        _CACHE[key] = build_program(L, NSEQ, S, DE, debug)
    nc, kb = _CACHE[key]
    shared = host_prep(inp, L, S, DE)
    T = NSEQ * S
    NB = -(-(2 * T + NE * (BLK - 1)) // BLK)
    shared["thr"] = np.ascontiguousarray(np.broadcast_to((np.arange(NB, dtype=np.float32) * BLK).reshape(1, NB), (128, NB)))
    x = np.asarray(inp["x"], np.float32)
    mem = np.asarray(inp["mem"], np.float32)
    in_maps = []
    for c in range(NCORES):
        m = dict(shared)
        m["x"] = np.ascontiguousarray(x[c * NSEQ:(c + 1) * NSEQ].reshape(T, D))
        m["mem"] = np.ascontiguousarray(mem[c * NSEQ:(c + 1) * NSEQ].reshape(NSEQ * MEM, D))
        in_maps.append(m)
    res = run_bass_kernel_spmd(nc, in_maps, core_ids=list(range(NCORES)))
    return res


def kernel(**inputs):
    L, NSEQ, S, DE = 2, 2, 2048, 512
    res = run(inputs, L, NSEQ, S, DE)
    outs = [r["out"].reshape(NSEQ, S, D) for r in res.results]
    return np.concatenate(outs, axis=0).astype(np.float32)
```
